# Optimizing a Trainium2 kernel written in Bass

```python
import jax
import jax.numpy as jnp
from jax import lax
import numpy as np


D_MODEL = 4096
BATCH = 2
SEQ = 4096
DEPTH = 2

N_EVEN = (DEPTH + 1) // 2
N_ODD = DEPTH // 2
MLA_HEADS = D_MODEL // 256
QK_NOPE = 128
QK_ROPE = 64
QK_HEAD = QK_NOPE + QK_ROPE
V_HEAD = 128
Q_LORA = 3 * D_MODEL // 16
KV_LORA = D_MODEL // 8
ROPE_THETA = 10000.0
ATTN_BLOCK = 128
MAX_POS_OFFSET = 1024
RWKV_HEAD = 64
RWKV_HEADS = D_MODEL // 128
RWKV_WIDTH = RWKV_HEADS * RWKV_HEAD
W_LORA = 64
A_LORA = 64
G_LORA = 128
LNX_EPS = 64e-5
MLA_IN = Q_LORA + KV_LORA + QK_ROPE
RWKV_IN = 3 * RWKV_WIDTH + W_LORA + A_LORA + G_LORA
HYB_IN = MLA_IN + RWKV_IN
MIX_WIDTH = MLA_HEADS * V_HEAD + RWKV_WIDTH
CONV_WIDTH = 3
PEER_HEADS = 8
N_KEYS = 128
N_EXPERTS = N_KEYS * N_KEYS
PEER_TOPK = 16
PEER_QDIM = 256
PEER_HALF = PEER_QDIM // 2
PEER_BLOCK = 128
NORM_EPS = 1e-6

kernel_name = 'hybrid_mla_rwkv7_shortconv_peer'


def rms_norm(x, w):
    xf = x.astype(jnp.float32)
    y = xf * lax.rsqrt(jnp.mean(xf * xf, axis=-1, keepdims=True) + NORM_EPS)
    return (y * w.astype(jnp.float32)).astype(x.dtype)


def modulate(h, shift, scale):
    return h * (1.0 + scale[:, None, :]) + shift[:, None, :]


def rope(x, positions):
    half = x.shape[-1] // 2
    freqs = ROPE_THETA ** (-jnp.arange(half, dtype=jnp.float32) / half)
    ang = positions.astype(jnp.float32)[:, :, None, None] * freqs
    cos, sin = jnp.cos(ang), jnp.sin(ang)
    xf = x.astype(jnp.float32)
    x1, x2 = xf[..., :half], xf[..., half:]
    return jnp.concatenate([x1 * cos - x2 * sin, x2 * cos + x1 * sin], axis=-1).astype(x.dtype)


def token_shift(p):
    return jnp.pad(p, ((0, 0), (1, 0), (0, 0)))[:, :-1]


def causal_block_attention(q, k, v):
    b, s, h, dk = q.shape
    dv = v.shape[-1]
    nb = s // ATTN_BLOCK
    scale = dk ** -0.5
    qb = jnp.moveaxis(q.reshape(b, nb, ATTN_BLOCK, h, dk), 1, 0)
    key_pos = jnp.arange(s)

    def one_block(args):
        q_blk, blk = args
        sc = jnp.einsum('bqhd,bkhd->bhqk', q_blk, k).astype(jnp.float32) * scale
        q_pos = blk * ATTN_BLOCK + jnp.arange(ATTN_BLOCK)
        mask = key_pos[None, :] <= q_pos[:, None]
        sc = jnp.where(mask[None, None], sc, -jnp.inf)
        pr = jax.nn.softmax(sc, axis=-1).astype(v.dtype)
        return jnp.einsum('bhqk,bkhd->bqhd', pr, v)

    out = lax.map(one_block, (qb, jnp.arange(nb)))
    return jnp.moveaxis(out, 0, 1).reshape(b, s, h * dv)


def mla_mixer(cq, ckv, kpe, positions, q_norm_w, w_uq, kv_norm_w, w_ukv, qk_q_w, qk_k_w):
    b, s, _ = cq.shape
    q = (rms_norm(cq, q_norm_w) @ w_uq).reshape(b, s, MLA_HEADS, QK_HEAD)
    kv = (rms_norm(ckv, kv_norm_w) @ w_ukv).reshape(b, s, MLA_HEADS, QK_NOPE + V_HEAD)
    k_nope, v = kv[..., :QK_NOPE], kv[..., QK_NOPE:]
    k_pe = jnp.broadcast_to(kpe[:, :, None, :], (b, s, MLA_HEADS, QK_ROPE))
    k = jnp.concatenate([k_nope, k_pe], axis=-1)
    q = rms_norm(q, qk_q_w)
    k = rms_norm(k, qk_k_w)
    q = jnp.concatenate([q[..., :QK_NOPE], rope(q[..., QK_NOPE:], positions)], axis=-1)
    k = jnp.concatenate([k[..., :QK_NOPE], rope(k[..., QK_NOPE:], positions)], axis=-1)
    return causal_block_attention(q, k, v)


def rwkv7_mixer(p, mu, w0, w2, a0, a2, g2, k_k, k_a, r_k, lnx_w, lnx_b):
    b, s, _ = p.shape
    f32 = jnp.float32
    p = p + (token_shift(p) - p) * mu
    o1 = RWKV_WIDTH
    o2 = 2 * RWKV_WIDTH
    o3 = 3 * RWKV_WIDTH
    r, k, v, xw, xa, xg = jnp.split(p, [o1, o2, o3, o3 + W_LORA, o3 + W_LORA + A_LORA], axis=-1)
    w = -jax.nn.softplus(-(w0 + jnp.tanh(xw) @ w2)) - 0.5
    a = jax.nn.sigmoid(a0 + xa @ a2)
    g = jax.nn.sigmoid(xg) @ g2

    def heads(t):
        return t.reshape(b, s, RWKV_HEADS, RWKV_HEAD)

    kk = heads((k * k_k).astype(f32))
    kk = kk / jnp.maximum(jnp.sqrt(jnp.sum(kk * kk, axis=-1, keepdims=True)), 1e-12)
    k = k * (1.0 + (a - 1.0) * k_a)
    r_h, k_h, v_h, a_h = heads(r), heads(k), heads(v), heads(a)
    decay = jnp.exp(-jnp.exp(heads(w).astype(f32)))

    def step(state, inp):
        r_t, w_t, k_t, v_t, kk_t, a_t = inp
        sa = jnp.einsum('bhij,bhj->bhi', state, -kk_t)
        state = (state * w_t[:, :, None, :]
                 + sa[..., None] * (kk_t * a_t)[:, :, None, :]
                 + v_t[..., None] * k_t[:, :, None, :])
        return state, jnp.einsum('bhij,bhj->bhi', state, r_t)

    def time_major(t):
        return jnp.moveaxis(t.astype(f32), 1, 0)

    s0 = jnp.zeros((b, RWKV_HEADS, RWKV_HEAD, RWKV_HEAD), f32)
    xs = tuple(map(time_major, (r_h, decay, k_h, v_h, kk, a_h)))
    _, y = lax.scan(step, s0, xs)
    y = jnp.moveaxis(y, 0, 1)
    mean = jnp.mean(y, axis=-1, keepdims=True)
    var = jnp.mean(jnp.square(y - mean), axis=-1, keepdims=True)
    y = (y - mean) * lax.rsqrt(var + LNX_EPS)
    gn_w = lnx_w.reshape(RWKV_HEADS, RWKV_HEAD).astype(f32)
    gn_b = lnx_b.reshape(RWKV_HEADS, RWKV_HEAD).astype(f32)
    y = y * gn_w + gn_b
    bonus = jnp.sum((r_h * k_h * r_k).astype(f32), axis=-1, keepdims=True) * v_h.astype(f32)
    return (y + bonus).reshape(b, s, RWKV_WIDTH).astype(p.dtype) * g


def short_conv_mixer(h, w_in, conv_w, w_out):
    s = h.shape[1]
    gate_b, gate_c, u = jnp.split(h @ w_in, 3, axis=-1)
    u = gate_c * u
    up = jnp.pad(u, ((0, 0), (CONV_WIDTH - 1, 0), (0, 0)))
    y = sum(conv_w[j] * up[:, j:j + s] for j in range(CONV_WIDTH))
    return (gate_b * y) @ w_out


def peer_ffn(h, w_q, keys, u_tab, v_tab):
    b, s, d = h.shape
    nb = (b * s) // PEER_BLOCK
    hb = h.reshape(nb, PEER_BLOCK, d)

    def one_block(xb):
        q = (xb @ w_q).reshape(PEER_BLOCK, PEER_HEADS, 2, PEER_HALF)
        sc = jnp.einsum('thpd,hpkd->thpk', q, keys)
        sv, si = lax.top_k(sc, PEER_TOPK)
        cand = sv[:, :, 0, :, None] + sv[:, :, 1, None, :]
        cv, ci = lax.top_k(cand.reshape(PEER_BLOCK, PEER_HEADS, PEER_TOPK * PEER_TOPK), PEER_TOPK)
        i1 = jnp.take_along_axis(si[:, :, 0], ci // PEER_TOPK, axis=-1)
        i2 = jnp.take_along_axis(si[:, :, 1], ci % PEER_TOPK, axis=-1)
        e = i1 * N_KEYS + i2
        gw = jax.nn.softmax(cv.astype(jnp.float32), axis=-1).astype(xb.dtype)
        act = jax.nn.gelu(jnp.einsum('thkd,td->thk', u_tab[e], xb), approximate=False)
        return jnp.einsum('thk,thkd->td', gw * act, v_tab[e])

    return lax.map(one_block, hb).reshape(b, s, d)


def setup_inputs(seed: int = 0) -> dict:
    key = jax.random.key(seed)
    ks = iter(jax.random.split(key, 48))
    f32 = jnp.float32
    D = D_MODEL

    def nrm(shape, scale):
        return jax.random.normal(next(ks), shape, f32) * scale

    def gain(shape):
        return 1.0 + nrm(shape, 0.02)

    positions = (jax.random.randint(next(ks), (BATCH, 1), 0, MAX_POS_OFFSET, jnp.int32)
                 + jnp.arange(SEQ, dtype=jnp.int32)[None, :])
    return {
        'x': nrm((BATCH, SEQ, D), 1.0),
        'c': nrm((BATCH, D), 1.0),
        'positions': positions,
        'ada_w': nrm((DEPTH, D, 6 * D), 0.5 * D ** -0.5),
        'ada_b': nrm((DEPTH, 6 * D), 0.02),
        'norm_mix_w': gain((DEPTH, D)),
        'norm_ffn_w': gain((DEPTH, D)),
        'hyb_w_in': nrm((N_EVEN, D, HYB_IN), D ** -0.5),
        'mla_q_norm_w': gain((N_EVEN, Q_LORA)),
        'mla_w_uq': nrm((N_EVEN, Q_LORA, MLA_HEADS * QK_HEAD), Q_LORA ** -0.5),
        'mla_kv_norm_w': gain((N_EVEN, KV_LORA)),
        'mla_w_ukv': nrm((N_EVEN, KV_LORA, MLA_HEADS * (QK_NOPE + V_HEAD)), KV_LORA ** -0.5),
        'mla_qk_q_w': gain((N_EVEN, QK_HEAD)),
        'mla_qk_k_w': gain((N_EVEN, QK_HEAD)),
        'rwkv_mu': jax.random.uniform(next(ks), (N_EVEN, RWKV_IN), f32),
        'rwkv_w0': jax.random.uniform(next(ks), (N_EVEN, RWKV_WIDTH), f32, minval=-6.0, maxval=1.0),
        'rwkv_w2': nrm((N_EVEN, W_LORA, RWKV_WIDTH), 0.1),
        'rwkv_a0': nrm((N_EVEN, RWKV_WIDTH), 0.5),
        'rwkv_a2': nrm((N_EVEN, A_LORA, RWKV_WIDTH), 0.1),
        'rwkv_g2': nrm((N_EVEN, G_LORA, RWKV_WIDTH), G_LORA ** -0.5),
        'rwkv_k_k': 0.85 + nrm((N_EVEN, RWKV_WIDTH), 0.05),
        'rwkv_k_a': 1.0 + nrm((N_EVEN, RWKV_WIDTH), 0.05),
        'rwkv_r_k': nrm((N_EVEN, RWKV_HEADS, RWKV_HEAD), 0.1),
        'rwkv_lnx_w': gain((N_EVEN, RWKV_WIDTH)),
        'rwkv_lnx_b': nrm((N_EVEN, RWKV_WIDTH), 0.02),
        'hyb_w_out': nrm((N_EVEN, MIX_WIDTH, D), MIX_WIDTH ** -0.5),
        'conv_w_in': nrm((N_ODD, D, 3 * D), D ** -0.5),
        'conv_w': nrm((N_ODD, CONV_WIDTH, D), CONV_WIDTH ** -0.5),
        'conv_w_out': nrm((N_ODD, D, D), D ** -0.5),
        'peer_w_q': nrm((DEPTH, D, PEER_HEADS * PEER_QDIM), D ** -0.5),
        'peer_keys': nrm((DEPTH, PEER_HEADS, 2, N_KEYS, PEER_HALF), PEER_HALF ** -0.5),
        'peer_u': nrm((DEPTH, N_EXPERTS, D), D ** -0.5),
        'peer_v': nrm((DEPTH, N_EXPERTS, D), PEER_HEADS ** -0.5),
    }


def reference(x, c, positions, ada_w, ada_b, norm_mix_w, norm_ffn_w, hyb_w_in,
              mla_q_norm_w, mla_w_uq, mla_kv_norm_w, mla_w_ukv, mla_qk_q_w, mla_qk_k_w,
              rwkv_mu, rwkv_w0, rwkv_w2, rwkv_a0, rwkv_a2, rwkv_g2, rwkv_k_k, rwkv_k_a,
              rwkv_r_k, rwkv_lnx_w, rwkv_lnx_b, hyb_w_out, conv_w_in, conv_w, conv_w_out,
              peer_w_q, peer_keys, peer_u, peer_v):
    cond = jax.nn.silu(c)
    for layer in range(DEPTH):
        mod = cond @ ada_w[layer] + ada_b[layer]
        sh_m, sc_m, gt_m, sh_f, sc_f, gt_f = jnp.split(mod, 6, axis=-1)
        h = modulate(rms_norm(x, norm_mix_w[layer]), sh_m, sc_m)
        i = layer // 2
        if layer % 2 == 0:
            p = h @ hyb_w_in[i]
            cq, ckv, kpe, prw = jnp.split(p, [Q_LORA, Q_LORA + KV_LORA, MLA_IN], axis=-1)
            y_mla = mla_mixer(cq, ckv, kpe, positions, mla_q_norm_w[i], mla_w_uq[i],
                              mla_kv_norm_w[i], mla_w_ukv[i], mla_qk_q_w[i], mla_qk_k_w[i])
            y_rwkv = rwkv7_mixer(prw, rwkv_mu[i], rwkv_w0[i], rwkv_w2[i], rwkv_a0[i], rwkv_a2[i],
                                 rwkv_g2[i], rwkv_k_k[i], rwkv_k_a[i], rwkv_r_k[i],
                                 rwkv_lnx_w[i], rwkv_lnx_b[i])
            y = jnp.concatenate([y_mla, y_rwkv], axis=-1) @ hyb_w_out[i]
        else:
            y = short_conv_mixer(h, conv_w_in[i], conv_w[i], conv_w_out[i])
        x = x + gt_m[:, None, :] * y
        h = modulate(rms_norm(x, norm_ffn_w[layer]), sh_f, sc_f)
        x = x + gt_f[:, None, :] * peer_ffn(h, peer_w_q[layer], peer_keys[layer],
                                            peer_u[layer], peer_v[layer])
    return x
```

```python
import numpy as np
from contextlib import ExitStack
import concourse.bass as bass
import concourse.mybir as mybir
from concourse.bass_utils import run_bass_kernel_spmd

F32 = mybir.dt.float32
BF16 = mybir.dt.bfloat16
AF = mybir.ActivationFunctionType
ALU = mybir.AluOpType
AX = mybir.AxisListType

D = 4096
KC = D // 128
COMPUTE = ("pe", "act", "dve", "pool")
SEM_ROT = 60000
DMA_RING = {"sp": 16, "pool": 8}


class Buf:
    __slots__ = ("name", "w", "r")

    def __init__(self, name=""):
        self.name, self.w, self.r = name, None, []


class Prog:
    def __init__(self, nc, stack):
        self.nc, self.stack = nc, stack
        self.ops = {e: [] for e in ("pe", "act", "dve", "pool", "sp")}
        self.sem, self.cnt, self.nsem = {}, {}, 0
        for e in COMPUTE:
            self._new_sem(e)
        self.ring = {q: [self._alloc("dq%s%d" % (q, i)) for i in range(k)] for q, k in DMA_RING.items()}
        self.ring_n = {q: 0 for q in DMA_RING}
        self.waited, self.live = {}, []

    def _alloc(self, name):
        self.nsem += 1
        return self.stack.enter_context(self.nc.semaphore("%s_%d" % (name, self.nsem)))

    def _new_sem(self, e):
        self.sem[e], self.cnt[e] = self._alloc("c" + e), 0

    def buf(self, name=""):
        b = Buf(name)
        self.live.append(b)
        return b

    def _need(self, eng, ev, skip_same=False):
        if ev is None:
            return None
        sem, val, src = ev
        if skip_same and src == eng:
            return None
        key = (eng, id(sem))
        if self.waited.get(key, 0) >= val:
            return None
        self.waited[key] = val
        return (sem, val)

    def _deps(self, eng, reads, writes, skip_same):
        waits = []
        for b in reads:
            w = self._need(eng, b.w, skip_same)
            if w:
                waits.append(w)
        for b in writes:
            for ev in [b.w] + b.r:
                w = self._need(eng, ev, skip_same)
                if w:
                    waits.append(w)
        return waits

    def _commit(self, ev, reads, writes):
        for b in reads:
            b.r.append(ev)
            if len(b.r) > 48:
                last = {}
                for e in b.r:
                    if id(e[0]) not in last or last[id(e[0])][1] < e[1]:
                        last[id(e[0])] = e
                b.r = list(last.values())
        for b in writes:
            b.w, b.r = ev, []

    def op(self, eng, fn, reads=(), writes=(), skip_same=False):
        waits = self._deps(eng, reads, writes, skip_same)
        if self.cnt[eng] >= SEM_ROT:
            self._new_sem(eng)
        self.cnt[eng] += 1
        sem, val = self.sem[eng], self.cnt[eng]
        self.ops[eng].append((waits, fn, sem, 1))
        self._commit((sem, val, eng), reads, writes)

    def dma(self, q, fn, reads=(), writes=()):
        waits = self._deps(q, reads, writes, False)
        n, k = self.ring_n[q], len(self.ring[q])
        sem = self.ring[q][n % k]
        if n >= k:
            w = self._need(q, (sem, 16 * (n // k), "dma"))
            if w:
                waits.append(w)
        self.ring_n[q] = n + 1
        self.ops[q].append((waits, fn, sem, 16))
        self._commit((sem, 16 * (n // k + 1), "dma"), reads, writes)

    def barrier(self):
        for eng in ("pe", "act", "dve", "pool", "sp"):
            waits = []
            for b in self.live:
                for ev in [b.w] + b.r:
                    w = self._need(eng, ev)
                    if w:
                        waits.append(w)
            self.ops[eng].append((waits, None, None, 0))
        self.live = []

    def emit(self):
        ops = self.ops

        def run(e, lst):
            for waits, fn, sem, inc in lst:
                for (s, v) in waits:
                    e.wait_ge(s, v)
                if fn is not None:
                    fn(e).then_inc(sem, inc)

        with self.nc.Block() as block:
            block.tensor(lambda e: run(e, ops["pe"]))
            block.scalar(lambda e: run(e, ops["act"]))
            block.vector(lambda e: run(e, ops["dve"]))
            block.gpsimd(lambda e: run(e, ops["pool"]))
            block.sync(lambda e: run(e, ops["sp"]))
        self.ops = {e: [] for e in ops}


_GF = {}


class K:
    def __init__(self, nc, P, st):
        self.nc, self.P, self.st = nc, P, st
        self.n = 0

    def sb(self, st, shape, dt, name="t"):
        self.n += 1
        return st.enter_context(self.nc.sbuf_tensor("%s%d" % (name, self.n), list(shape), dt))

    def ps(self, st, shape, dt, name="p"):
        self.n += 1
        return st.enter_context(self.nc.psum_tensor("%s%d" % (name, self.n), list(shape), dt))

    def dram(self, shape, dt, name="scr"):
        self.n += 1
        return self.nc.dram_tensor("%s%d" % (name, self.n), list(shape), dt, kind="Internal").ap()

    def end_stage(self):
        self.P.barrier()
        self.P.emit()
        _GF.clear()


def stage_norm_T(k, st, ident_bf, x_dram, t0, nt, nw_b, sh_b, hT, hT32=None, ident_f=None):
    P, nc = k.P, k.nc
    (idb, b_id), (g_t, b_g), (s_t, b_s), (hT_t, b_hT) = ident_bf, nw_b, sh_b, hT
    if True:
        xt = [k.sb(st, [128, D], F32, "nx") for _ in range(2)]
        bx = [P.buf() for _ in range(2)]
        sq = k.sb(st, [128, D], F32, "nsq"); b_sq = P.buf()
        hb = k.sb(st, [128, D], BF16, "nhb"); b_hb = P.buf()
        ss = k.sb(st, [128, 4], F32, "nss"); b_ss = P.buf()
        pT = [k.ps(st, [128, 8, 128], BF16, "npT") for _ in range(2)]
        bpT = [P.buf() for _ in range(2)]
        if hT32 is not None:
            pT32 = [k.ps(st, [128, 4, 128], F32, "npT32") for _ in range(2)]
            bpT32 = [P.buf() for _ in range(2)]
        for i in range(nt // 128):
            x_t, b_x = xt[i % 2], bx[i % 2]
            r0 = t0 + i * 128
            P.dma("sp", lambda e, x_t=x_t, r0=r0: e.dma_start(out=x_t[:], in_=x_dram[r0:r0 + 128, :]), writes=[b_x])
            P.op("dve", lambda e: e.memset(ss[:], 0.0), writes=[b_ss])
            P.op("act", lambda e, x_t=x_t: e.activation(out=sq[:], in_=x_t[:], func=AF.Square, accum_out=ss[:, 0:1]),
                 reads=[b_x], writes=[b_sq, b_ss])
            P.op("dve", lambda e: e.tensor_scalar(out=ss[:, 1:2], in0=ss[:, 0:1], scalar1=1.0 / D, scalar2=1e-6,
                                                  op0=ALU.mult, op1=ALU.add), reads=[b_ss], writes=[b_ss])
            P.op("act", lambda e: e.activation(out=ss[:, 1:2], in_=ss[:, 1:2], func=AF.Sqrt), reads=[b_ss], writes=[b_ss])
            P.op("dve", lambda e: e.reciprocal(out=ss[:, 1:2], in_=ss[:, 1:2]), reads=[b_ss], writes=[b_ss])
            P.op("dve", lambda e, x_t=x_t: e.scalar_tensor_tensor(out=sq[:], in0=x_t[:], scalar=ss[:, 1:2], in1=g_t[:],
                                                                 op0=ALU.mult, op1=ALU.mult),
                 reads=[b_x, b_ss, b_g], writes=[b_sq])
            if hT32 is None:
                P.op("dve", lambda e: e.tensor_tensor(out=hb[:], in0=sq[:], in1=s_t[:], op=ALU.add),
                     reads=[b_sq, b_s], writes=[b_hb])
            else:
                P.op("dve", lambda e: e.tensor_tensor(out=sq[:], in0=sq[:], in1=s_t[:], op=ALU.add),
                     reads=[b_sq, b_s], writes=[b_sq])
                P.op("act", lambda e: e.copy(out=hb[:], in_=sq[:]), reads=[b_sq], writes=[b_hb])
                (h32_t, b_h32), (idf_t, b_idf) = hT32, ident_f
                for g in range(KC // 4):
                    p_t, b_p = pT32[g % 2], bpT32[g % 2]
                    for j in range(4):
                        c = g * 4 + j
                        P.op("pe", lambda e, p_t=p_t, j=j, c=c: e.transpose(out=p_t[:, j, :], in_=sq[:, c * 128:(c + 1) * 128],
                                                                             identity=idf_t[:]),
                             reads=[b_sq, b_idf], writes=[b_p], skip_same=True)
                    if g % 2 == 0:
                        P.op("act", lambda e, p_t=p_t, g=g, i=i: e.copy(out=h32_t[:, g * 4:(g + 1) * 4, i * 128:(i + 1) * 128], in_=p_t[:]),
                             reads=[b_p], writes=[b_h32])
                    else:
                        P.op("dve", lambda e, p_t=p_t, g=g, i=i: e.tensor_copy(out=h32_t[:, g * 4:(g + 1) * 4, i * 128:(i + 1) * 128], in_=p_t[:]),
                             reads=[b_p], writes=[b_h32])
            for g in range(KC // 8):
                p_t, b_p = pT[g % 2], bpT[g % 2]
                for j in range(8):
                    c = g * 8 + j
                    P.op("pe", lambda e, p_t=p_t, j=j, c=c: e.transpose(out=p_t[:, j, :], in_=hb[:, c * 128:(c + 1) * 128],
                                                                         identity=idb[:]),
                         reads=[b_hb, b_id], writes=[b_p], skip_same=True)
                eng = "act" if g % 2 == 0 else "dve"
                if eng == "act":
                    P.op("act", lambda e, p_t=p_t, g=g, i=i: e.copy(out=hT_t[:, g * 8:(g + 1) * 8, i * 128:(i + 1) * 128], in_=p_t[:]),
                         reads=[b_p], writes=[b_hT])
                else:
                    P.op("dve", lambda e, p_t=p_t, g=g, i=i: e.tensor_copy(out=hT_t[:, g * 8:(g + 1) * 8, i * 128:(i + 1) * 128], in_=p_t[:]),
                         reads=[b_p], writes=[b_hT])


def gemm_tok(k, st, xT, kc_n, nt, W, n_cols, evac):
    P = k.P
    xT_t, b_xT = xT
    if True:
        wt = [k.sb(st, [128, kc_n, 512], BF16, "gw") for _ in range(2)]
        bw = [P.buf() for _ in range(2)]
        pp = [k.ps(st, [128, 512], F32, "gp") for _ in range(2)]
        bp = [P.buf() for _ in range(2)]
        it = 0
        for bi, c0 in enumerate(range(0, n_cols, 512)):
            w = min(512, n_cols - c0)
            w_t, b_w = wt[bi % 2], bw[bi % 2]
            P.dma("pool", lambda e, w_t=w_t, bi=bi: e.dma_start(out=w_t[:], in_=W[bi]), writes=[b_w])
            for ti in range(nt // 128):
                p_t, b_p = pp[it % 2], bp[it % 2]
                it += 1
                for c in range(kc_n):
                    P.op("pe", lambda e, p_t=p_t, w_t=w_t, c=c, ti=ti, w=w: e.matmul(
                        p_t[:, 0:w], lhsT=xT_t[:, c, ti * 128:(ti + 1) * 128], rhs=w_t[:, c, 0:w],
                        start=(c == 0), stop=(c == kc_n - 1)), reads=[b_xT, b_w], writes=[b_p], skip_same=True)
                evac(ti, c0, w, p_t, b_p)


def stage_ada(k, st, cT_dram, ada_w, ada_b, mod_out, n_cols, Bn):
    P = k.P
    cf = k.sb(st, [128, KC, Bn], F32, "acf"); b_cf = P.buf()
    cb = k.sb(st, [128, KC, Bn], BF16, "acb"); b_cb = P.buf()
    P.dma("sp", lambda e: e.dma_start(out=cf[:], in_=cT_dram[:, :, :]), writes=[b_cf])
    P.op("act", lambda e: e.activation(out=cb[:], in_=cf[:], func=AF.Silu), reads=[b_cf], writes=[b_cb])
    wt = [k.sb(st, [128, KC, 512], BF16, "aw") for _ in range(2)]
    bw = [P.buf() for _ in range(2)]
    bt = [k.sb(st, [Bn, 512], F32, "ab") for _ in range(2)]
    bb = [P.buf() for _ in range(2)]
    ot = [k.sb(st, [Bn, 512], F32, "ao") for _ in range(2)]
    bo = [P.buf() for _ in range(2)]
    pp = [k.ps(st, [Bn, 512], F32, "ap") for _ in range(2)]
    bp = [P.buf() for _ in range(2)]
    for bi, c0 in enumerate(range(0, n_cols, 512)):
        w = min(512, n_cols - c0)
        j = bi % 2
        P.dma("pool", lambda e, j=j, bi=bi: e.dma_start(out=wt[j][:], in_=ada_w[bi]), writes=[bw[j]])
        P.dma("sp", lambda e, j=j, c0=c0, w=w: e.dma_start(
            out=bt[j][:, 0:w], in_=ada_b[c0:c0 + w].partition_broadcast(Bn)), writes=[bb[j]])
        for c in range(KC):
            P.op("pe", lambda e, j=j, c=c, w=w: e.matmul(pp[j][:, 0:w], lhsT=cb[:, c, :], rhs=wt[j][:, c, 0:w],
                                                        start=(c == 0), stop=(c == KC - 1)),
                 reads=[b_cb, bw[j]], writes=[bp[j]], skip_same=True)
        P.op("dve", lambda e, j=j, w=w: e.tensor_tensor(out=ot[j][:, 0:w], in0=pp[j][:, 0:w], in1=bt[j][:, 0:w], op=ALU.add),
             reads=[bp[j], bb[j]], writes=[bo[j]])
        P.dma("sp", lambda e, j=j, c0=c0, w=w: e.dma_start(out=mod_out[:, c0:c0 + w], in_=ot[j][:, 0:w]), reads=[bo[j]])


def gemm_feat(k, st, xT, kc_n, col_lo, col_n, W, f0, nf, evac):
    P = k.P
    xT_t, b_xT = xT
    wt = [k.sb(st, [128, kc_n, 128], BF16, "fw") for _ in range(2)]
    bw = [P.buf() for _ in range(2)]
    pp = [k.ps(st, [128, 512], F32, "fp") for _ in range(2)]
    bp = [P.buf() for _ in range(2)]
    for j in range(nf):
        i = j % 2
        c0 = f0 + j * 128
        P.dma("pool", lambda e, i=i, c0=c0: e.dma_start(
            out=wt[i][:], in_=W[:, c0:c0 + 128].rearrange("(c p) n -> p c n", p=128)), writes=[bw[i]])
        for c in range(kc_n):
            P.op("pe", lambda e, i=i, c=c: e.matmul(pp[i][:, 0:col_n], lhsT=wt[i][:, c, :],
                                                   rhs=xT_t[:, c, col_lo:col_lo + col_n],
                                                   start=(c == 0), stop=(c == kc_n - 1)),
                 reads=[b_xT, bw[i]], writes=[bp[i]], skip_same=True)
        evac(j, pp[i], bp[i])


def stage_conv_core(k, st, hT, nt, halo, w_in, conv_w3, nf, gT):
    P = k.P
    assert halo == 2 and halo + nt <= 512
    gT_t, b_gT = gT
    n = halo + nt
    cw = k.sb(st, [128, KC, 3], F32, "ccw"); b_cw = P.buf()
    P.dma("sp", lambda e: e.dma_start(out=cw[:], in_=conv_w3[:, :, :]), writes=[b_cw])
    gb = k.sb(st, [128, 512], F32, "cgb"); b_gb = P.buf()
    gc = k.sb(st, [128, 512], F32, "cgc"); b_gc = P.buf()
    uu = k.sb(st, [128, 512], F32, "cuu"); b_uu = P.buf()
    yy = k.sb(st, [128, 512], F32, "cyy"); b_yy = P.buf()
    for j in range(nf):
        def ev_to(dst, b_dst):
            def ev(_j, p_t, b_p):
                P.op("act", lambda e: e.copy(out=dst[:, 0:n], in_=p_t[:, 0:n]), reads=[b_p], writes=[b_dst])
            return ev
        gemm_feat_one(k, st, hT, n, w_in[0 * KC + j], ev_to(gb, b_gb))
        gemm_feat_one(k, st, hT, n, w_in[1 * KC + j], ev_to(gc, b_gc))
        gemm_feat_one(k, st, hT, n, w_in[2 * KC + j], ev_to(uu, b_uu))
        P.op("dve", lambda e: e.tensor_tensor(out=uu[:, 0:n], in0=uu[:, 0:n], in1=gc[:, 0:n], op=ALU.mult),
             reads=[b_uu, b_gc], writes=[b_uu])
        P.op("dve", lambda e, j=j: e.tensor_scalar(out=yy[:, 0:nt], in0=uu[:, 0:nt], scalar1=cw[:, j, 0:1], scalar2=None,
                                                  op0=ALU.mult), reads=[b_uu, b_cw], writes=[b_yy])
        P.op("dve", lambda e, j=j: e.scalar_tensor_tensor(out=yy[:, 0:nt], in0=uu[:, 1:1 + nt], scalar=cw[:, j, 1:2],
                                                         in1=yy[:, 0:nt], op0=ALU.mult, op1=ALU.add),
             reads=[b_uu, b_cw, b_yy], writes=[b_yy])
        P.op("dve", lambda e, j=j: e.scalar_tensor_tensor(out=yy[:, 0:nt], in0=uu[:, 2:2 + nt], scalar=cw[:, j, 2:3],
                                                         in1=yy[:, 0:nt], op0=ALU.mult, op1=ALU.add),
             reads=[b_uu, b_cw, b_yy], writes=[b_yy])
        P.op("dve", lambda e, j=j: e.tensor_tensor(out=gT_t[:, j, 0:nt], in0=yy[:, 0:nt], in1=gb[:, 2:2 + nt], op=ALU.mult),
             reads=[b_yy, b_gb], writes=[b_gT])


def gemm_feat_one(k, st, hT, n, Wc, evac, wdt=BF16):
    P = k.P
    key = (id(st), wdt == BF16)
    if key not in _GF:
        _GF[key] = dict(wt=[k.sb(st, [128, KC, 128], wdt, "fw") for _ in range(2)], bw=[P.buf() for _ in range(2)],
                        pp=[k.ps(st, [128, 512], F32, "fp") for _ in range(2)], bp=[P.buf() for _ in range(2)], n=0)
    g = _GF[key]
    i = g["n"] % 2
    g["n"] += 1
    xT_t, b_xT = hT
    P.dma("pool" if wdt == BF16 else "sp",
          lambda e: e.dma_start(out=g["wt"][i][:], in_=Wc),
          writes=[g["bw"][i]])
    for c in range(KC):
        P.op("pe", lambda e, c=c: e.matmul(g["pp"][i][:, 0:n], lhsT=g["wt"][i][:, c, :], rhs=xT_t[:, c, 0:n],
                                          start=(c == 0), stop=(c == KC - 1)),
             reads=[b_xT, g["bw"][i]], writes=[g["bp"][i]], skip_same=True)
    evac(0, g["pp"][i], g["bp"][i])


def peer_route(k, st, S_t, b_S, rt, b_rt, heads=8):
    P = k.P
    sv = k.sb(st, [128, heads, 2, 16], F32, "rsv"); b_sv = P.buf()
    scr = k.sb(st, [128, 128], F32, "rscr"); b_scr = P.buf()
    cand = k.sb(st, [128, 16, 16], F32, "rcand"); b_cand = P.buf()
    cscr = k.sb(st, [128, 256], F32, "rcscr"); b_cscr = P.buf()
    cv = k.sb(st, [128, 16], F32, "rcv"); b_cv = P.buf()
    ex = k.sb(st, [128, 16], F32, "rex"); b_ex = P.buf()
    zz = k.sb(st, [128, 4], F32, "rzz"); b_zz = P.buf()
    for h in range(heads):
        for p in range(2):
            P.op("dve", lambda e, h=h, p=p: e.max(out=sv[:, h, p, 0:8], in_=S_t[:, h, p, :]), reads=[b_S], writes=[b_sv])
            P.op("dve", lambda e, h=h, p=p: e.match_replace(out=scr[:], in_to_replace=sv[:, h, p, 0:8], in_values=S_t[:, h, p, :],
                                                           imm_value=-1e30), reads=[b_S, b_sv], writes=[b_scr])
            P.op("dve", lambda e, h=h, p=p: e.max(out=sv[:, h, p, 8:16], in_=scr[:]), reads=[b_scr], writes=[b_sv])
        for a in range(16):
            P.op("dve", lambda e, h=h, a=a: e.tensor_scalar(out=cand[:, a, :], in0=sv[:, h, 1, :], scalar1=sv[:, h, 0, a:a + 1],
                                                           scalar2=None, op0=ALU.add), reads=[b_sv], writes=[b_cand])
        cflat = cand[:].rearrange("p a b -> p (a b)")
        P.op("dve", lambda e: e.max(out=cv[:, 0:8], in_=cflat), reads=[b_cand], writes=[b_cv])
        P.op("dve", lambda e: e.match_replace(out=cscr[:], in_to_replace=cv[:, 0:8], in_values=cflat, imm_value=-1e30),
             reads=[b_cand, b_cv], writes=[b_cscr])
        P.op("dve", lambda e: e.max(out=cv[:, 8:16], in_=cscr[:]), reads=[b_cscr], writes=[b_cv])
        P.op("dve", lambda e, h=h: e.tensor_copy(out=rt[:, h, 0:1], in_=cv[:, 15:16]), reads=[b_cv], writes=[b_rt])
        P.op("dve", lambda e, h=h: e.tensor_copy(out=rt[:, h, 1:2], in_=cv[:, 0:1]), reads=[b_cv], writes=[b_rt])
        P.op("dve", lambda e: e.tensor_scalar(out=zz[:, 0:1], in0=cv[:, 0:1], scalar1=-1.0, scalar2=None, op0=ALU.mult),
             reads=[b_cv], writes=[b_zz])
        P.op("act", lambda e: e.activation(out=ex[:], in_=cv[:], func=AF.Exp, bias=zz[:, 0:1], scale=1.0),
             reads=[b_cv, b_zz], writes=[b_ex])
        P.op("dve", lambda e, h=h: e.reduce_sum(out=rt[:, h, 2:3], in_=ex[:], axis=AX.X), reads=[b_ex], writes=[b_rt])
    return sv, b_sv


def make_residual_evac(k, st, x_src, x_dst, t0, gate_b, nt):
    P = k.P
    g_t, b_g = gate_b
    xt = [k.sb(st, [128, 512], F32, "rx") for _ in range(3)]
    bx = [P.buf() for _ in range(3)]
    cnt = [0]
    base = [t0]

    def evac(ti, c0, w, p_t, b_p):
        i = cnt[0] % 3
        cnt[0] += 1
        r0 = base[0] + ti * 128
        P.dma("sp", lambda e: e.dma_start(out=xt[i][:, 0:w], in_=x_src[r0:r0 + 128, c0:c0 + w]), writes=[bx[i]])
        P.op("dve", lambda e: e.tensor_tensor(out=p_sb[i][:, 0:w], in0=p_t[:, 0:w], in1=g_t[:, c0:c0 + w], op=ALU.mult),
             reads=[b_p, b_g], writes=[bp[i]])
        P.op("pool", lambda e: e.tensor_tensor(out=xt[i][:, 0:w], in0=xt[i][:, 0:w], in1=p_sb[i][:, 0:w], op=ALU.add),
             reads=[bx[i], bp[i]], writes=[bx[i]])
        P.dma("sp", lambda e: e.dma_start(out=x_dst[r0:r0 + 128, c0:c0 + w], in_=xt[i][:, 0:w]), reads=[bx[i]])

    p_sb = [k.sb(st, [128, 512], F32, "rp") for _ in range(3)]
    bp = [P.buf() for _ in range(3)]
    evac.base = base
    return evac


NE = 16384
NCH = NE // 128


def peer_prep_UT(k, idb_b, U, UT):
    P = k.P
    idb, b_id = idb_b
    with ExitStack() as st:
        ub = [k.sb(st, [128, D], BF16, "uub") for _ in range(2)]; bub = [P.buf() for _ in range(2)]
        ut = [k.sb(st, [128, KC, 512], BF16, "uut") for _ in range(2)]; but = [P.buf() for _ in range(2)]
        pT = [k.ps(st, [128, 8, 128], BF16, "upT") for _ in range(2)]; bpT = [P.buf() for _ in range(2)]
        n = 0
        for eb in range(NE // 512):
            u_t, b_u = ut[eb % 2], but[eb % 2]
            for cc in range(4):
                ch = eb * 4 + cc
                r_t, b_r = ub[ch % 2], bub[ch % 2]
                P.dma("pool", lambda e, r_t=r_t, ch=ch: e.dma_start(out=r_t[:], in_=U[ch * 128:(ch + 1) * 128, :]), writes=[b_r])
                for g in range(KC // 8):
                    p_t, b_p = pT[n % 2], bpT[n % 2]
                    for j in range(8):
                        c = g * 8 + j
                        P.op("pe", lambda e, p_t=p_t, r_t=r_t, j=j, c=c: e.transpose(out=p_t[:, j, :], in_=r_t[:, c * 128:(c + 1) * 128],
                                                                                      identity=idb[:]),
                             reads=[b_r, b_id], writes=[b_p], skip_same=True)
                    if n % 2 == 0:
                        P.op("act", lambda e, p_t=p_t, u_t=u_t, g=g, cc=cc: e.copy(out=u_t[:, g * 8:(g + 1) * 8, cc * 128:(cc + 1) * 128], in_=p_t[:]),
                             reads=[b_p], writes=[b_u])
                    else:
                        P.op("dve", lambda e, p_t=p_t, u_t=u_t, g=g, cc=cc: e.tensor_copy(out=u_t[:, g * 8:(g + 1) * 8, cc * 128:(cc + 1) * 128], in_=p_t[:]),
                             reads=[b_p], writes=[b_u])
                    n += 1
            P.dma("sp", lambda e, u_t=u_t, eb=eb: e.dma_start(out=UT[eb], in_=u_t[:]), reads=[b_u])
        k.end_stage()


def peer_group_scores(k, st, h32_b, n, tg, w_q, kT32_b, S_all_b):
    P = k.P
    kT, b_kT = kT32_b
    S_all, b_S = S_all_b
    qT = k.sb(st, [128, 16, n], F32, "pq"); b_qT = P.buf()
    for hp in range(16):
        def ev(_j, p_t, b_p, hp=hp):
            P.op("act", lambda e: e.copy(out=qT[:, hp, :], in_=p_t[:, 0:n]), reads=[b_p], writes=[b_qT])
        gemm_feat_one(k, st, h32_b, n, w_q[hp], ev, wdt=F32)
    pS = [k.ps(st, [128, 16, 128], F32, "pS") for _ in range(1)]; b_pS = [P.buf()]
    for ti in range(tg):
        for hp in range(16):
            P.op("pe", lambda e, hp=hp, ti=ti: e.matmul(pS[0][:, hp, :], lhsT=qT[:, hp, ti * 128:(ti + 1) * 128], rhs=kT[:, hp, :],
                                                       start=True, stop=True), reads=[b_qT, b_kT], writes=[b_pS[0]], skip_same=True)
        P.op("act", lambda e, ti=ti: e.copy(out=S_all[:, ti, :, :, :].rearrange("p h t k -> p (h t) k"), in_=pS[0][:]),
             reads=[b_pS[0]], writes=[b_S])


def peer_group_A(k, idb_b, hT_b, tg, tok0, S_all_b, UT, GAT):
    P = k.P
    idb, b_id = idb_b
    hT, b_hT = hT_b
    S_all, b_S = S_all_b
    n = tg * 128
    with ExitStack() as keep:
        G = [k.sb(keep, [128, NE], BF16, "pG") for _ in range(tg)]; bG = [P.buf() for _ in range(tg)]
        with ExitStack() as st:
            rt = k.sb(st, [128, 8, 4], F32, "prt"); b_rt = P.buf()
            nb = k.sb(st, [128, 8, 4], F32, "pnb"); b_nb = P.buf()
            ex = [k.sb(st, [128, 16, 128], F32, "pex") for _ in range(2)]; bex = [P.buf() for _ in range(2)]
            acc = [k.sb(st, [128, 16, 128], F32, "pacc") for _ in range(2)]; bacc = [P.buf() for _ in range(2)]
            tmp = [k.sb(st, [128, 16, 128], F32, "ptmp") for _ in range(2)]; btmp = [P.buf() for _ in range(2)]
            for ti in range(tg):
                S_t = S_all[:, ti, :, :, :]
                sv, b_sv = peer_route(k, st, S_t, b_S, rt, b_rt, 8)
                P.op("dve", lambda e: e.tensor_scalar(out=nb[:, :, 0:1], in0=rt[:, :, 1:2], scalar1=-1.0, scalar2=None, op0=ALU.mult),
                     reads=[b_rt], writes=[b_nb])
                P.op("dve", lambda e: e.reciprocal(out=nb[:, :, 2:3], in_=rt[:, :, 2:3]), reads=[b_rt], writes=[b_nb])
                for q in range(8):
                    a_t, b_a, t_t, b_t, x_t, b_x = acc[q % 2], bacc[q % 2], tmp[q % 2], btmp[q % 2], ex[q % 2], bex[q % 2]
                    for h in range(8):
                        s2b = S_t[:, h, 1, :].unsqueeze(1).to_broadcast([128, 16, 128])
                        s1b = S_t[:, h, 0, q * 16:(q + 1) * 16].unsqueeze(2).to_broadcast([128, 16, 128])
                        P.op("pool", lambda e, t_t=t_t, s2b=s2b, s1b=s1b: e.tensor_tensor(out=t_t[:], in0=s2b, in1=s1b, op=ALU.add),
                             reads=[b_S], writes=[b_t])
                        P.op("act", lambda e, t_t=t_t, x_t=x_t, h=h: e.activation(out=x_t[:], in_=t_t[:], func=AF.Exp, bias=nb[:, h, 0:1], scale=1.0),
                             reads=[b_t, b_nb], writes=[b_x])
                        P.op("dve", lambda e, t_t=t_t, x_t=x_t, h=h: e.scalar_tensor_tensor(out=x_t[:], in0=t_t[:], scalar=rt[:, h, 0:1], in1=x_t[:],
                                                                                           op0=ALU.is_ge, op1=ALU.mult),
                             reads=[b_t, b_x, b_rt], writes=[b_x])
                        if h == 0:
                            P.op("dve", lambda e, a_t=a_t, x_t=x_t, h=h: e.tensor_scalar(out=a_t[:], in0=x_t[:], scalar1=nb[:, h, 2:3], scalar2=None,
                                                                                        op0=ALU.mult), reads=[b_x, b_nb], writes=[b_a])
                        else:
                            P.op("dve", lambda e, a_t=a_t, x_t=x_t, h=h: e.scalar_tensor_tensor(out=a_t[:], in0=x_t[:], scalar=nb[:, h, 2:3], in1=a_t[:],
                                                                                               op0=ALU.mult, op1=ALU.add),
                                 reads=[b_x, b_nb, b_a], writes=[b_a])
                    P.op("pool", lambda e, a_t=a_t, ti=ti, q=q: e.tensor_copy(out=G[ti][:, q * 2048:(q + 1) * 2048],
                                                                             in_=a_t[:].rearrange("p a b -> p (a b)")),
                         reads=[b_a], writes=[bG[ti]])
            k.end_stage()
        with ExitStack() as st:
            ut = [k.sb(st, [128, KC, 512], BF16, "aut") for _ in range(2)]; but = [P.buf() for _ in range(2)]
            pA = [k.ps(st, [128, 512], F32, "apA") for _ in range(2)]; bpA = [P.buf() for _ in range(2)]
            pT = [k.ps(st, [128, 4, 128], BF16, "apT") for _ in range(2)]; bpT = [P.buf() for _ in range(2)]
            act = [k.sb(st, [128, 512], F32, "aact") for _ in range(2)]; bact = [P.buf() for _ in range(2)]
            ga = [k.sb(st, [128, 512], BF16, "aga") for _ in range(2)]; bga = [P.buf() for _ in range(2)]
            gaT = [k.sb(st, [128, 4, n], BF16, "agaT") for _ in range(2)]; bgaT = [P.buf() for _ in range(2)]
            it = 0
            for eb in range(NE // 512):
                u_t, b_u = ut[eb % 2], but[eb % 2]
                g_t, b_g = gaT[eb % 2], bgaT[eb % 2]
                P.dma("sp", lambda e, u_t=u_t, eb=eb: e.dma_start(out=u_t[:], in_=UT[eb]), writes=[b_u])
                for ti in range(tg):
                    i = it % 2
                    it += 1
                    for c in range(KC):
                        P.op("pe", lambda e, i=i, c=c, ti=ti, u_t=u_t: e.matmul(pA[i][:], lhsT=hT[:, c, ti * 128:(ti + 1) * 128], rhs=u_t[:, c, :],
                                                                              start=(c == 0), stop=(c == KC - 1)),
                             reads=[b_hT, b_u], writes=[bpA[i]], skip_same=True)
                    P.op("act", lambda e, i=i: e.activation(out=act[i][:], in_=pA[i][:], func=AF.Gelu), reads=[bpA[i]], writes=[bact[i]])
                    P.op("dve", lambda e, i=i, ti=ti, eb=eb: e.tensor_tensor(out=ga[i][:], in0=act[i][:], in1=G[ti][:, eb * 512:(eb + 1) * 512],
                                                                            op=ALU.mult), reads=[bact[i], bG[ti]], writes=[bga[i]])
                    for cc in range(4):
                        P.op("pe", lambda e, i=i, cc=cc: e.transpose(out=pT[i][:, cc, :], in_=ga[i][:, cc * 128:(cc + 1) * 128], identity=idb[:]),
                             reads=[bga[i], b_id], writes=[bpT[i]], skip_same=True)
                    P.op("act", lambda e, i=i, ti=ti, g_t=g_t: e.copy(out=g_t[:, :, ti * 128:(ti + 1) * 128], in_=pT[i][:]),
                         reads=[bpT[i]], writes=[b_g])
                P.dma("sp", lambda e, g_t=g_t, eb=eb: e.dma_start(out=GAT[eb * 4:(eb + 1) * 4, :, tok0:tok0 + n].rearrange("c p t -> p c t"), in_=g_t[:]),
                      reads=[b_g])
            k.end_stage()


def peer_out(k, V, GAT, T, x_src, x_dst, gate_vec):
    P = k.P
    SB = min(8, T // 128)
    with ExitStack() as st:
        gate_t, b_gate = load_bcast(k, st, gate_vec, "ogate")
        vt = [k.sb(st, [128, 512], BF16, "ovt") for _ in range(4)]; bvt = [P.buf() for _ in range(4)]
        gt = [k.sb(st, [128, SB * 128], BF16, "ogt") for _ in range(4)]; bgt = [P.buf() for _ in range(4)]
        pO = [k.ps(st, [128, 512], F32, "opO") for _ in range(SB)]; bpO = [P.buf() for _ in range(SB)]
        it = 0
        evac = make_residual_evac(k, st, x_src, x_dst, 0, (gate_t, b_gate), SB * 128)
        for s0 in range(0, T, SB * 128):
            evac.base[0] = s0
            for db in range(D // 512):
                for ch in range(NCH):
                    i = it % 4
                    it += 1
                    P.dma("pool", lambda e, i=i, ch=ch, db=db: e.dma_start(out=vt[i][:], in_=V[ch * 128:(ch + 1) * 128, db * 512:(db + 1) * 512]),
                          writes=[bvt[i]])
                    P.dma("sp", lambda e, i=i, ch=ch, s0=s0: e.dma_start(out=gt[i][:], in_=GAT[ch, :, s0:s0 + SB * 128]), writes=[bgt[i]])
                    for ti in range(SB):
                        P.op("pe", lambda e, i=i, ti=ti, ch=ch: e.matmul(pO[ti][:], lhsT=gt[i][:, ti * 128:(ti + 1) * 128], rhs=vt[i][:],
                                                                        start=(ch == 0), stop=(ch == NCH - 1)),
                             reads=[bgt[i], bvt[i]], writes=[bpO[ti]], skip_same=True)
                for ti in range(SB):
                    evac(ti, db * 512, 512, pO[ti], bpO[ti])
        k.end_stage()


def peer_layer(k, idb_b, idf_b, x_src, x_dst, T, tg, nw, sc, sh, gate, w_q, keysT, U, V, UT, GAT, do_prep=True):
    P = k.P
    if do_prep:
        peer_prep_UT(k, idb_b, U, UT)
    n = tg * 128
    with ExitStack() as lay:
        kT = k.sb(lay, [128, 16, 128], F32, "lkT"); b_kT = P.buf()
        P.dma("sp", lambda e: e.dma_start(out=kT[:], in_=keysT[:, :, :]), writes=[b_kT])
        for g0 in range(0, T, n):
            with ExitStack() as keep:
                hT = k.sb(keep, [128, KC, n], BF16, "lhT"); b_hT = P.buf()
                S_all = k.sb(keep, [128, tg, 8, 2, 128], F32, "lS"); b_S = P.buf()
                with ExitStack() as keep2:
                    h32 = k.sb(keep2, [128, KC, n], F32, "lh32"); b_h32 = P.buf()
                    with ExitStack() as st:
                        g_t, b_g = load_bcast(k, st, nw, "lg"); s_t, b_s = load_bcast(k, st, sc, "ls"); h_t, b_h = load_bcast(k, st, sh, "lh")
                        P.op("dve", lambda e: e.scalar_tensor_tensor(out=g_t[:], in0=s_t[:], scalar=1.0, in1=g_t[:], op0=ALU.add, op1=ALU.mult),
                             reads=[b_s, b_g], writes=[b_g])
                        stage_norm_T(k, st, idb_b, x_src, g0, n, (g_t, b_g), (h_t, b_h), (hT, b_hT), hT32=(h32, b_h32), ident_f=idf_b)
                        k.end_stage()
                    with ExitStack() as st:
                        peer_group_scores(k, st, (h32, b_h32), n, tg, w_q, (kT, b_kT), (S_all, b_S))
                        k.end_stage()
                peer_group_A(k, idb_b, (hT, b_hT), tg, g0, (S_all, b_S), UT, GAT)
        k.end_stage()
    peer_out(k, V, GAT, T, x_src, x_dst, gate)


MLA_H, QK_NOPE, QK_ROPE, QK_HEAD, V_HEAD, Q_LORA, KV_LORA = 16, 128, 64, 192, 128, 768, 512
MLA_IN = Q_LORA + KV_LORA + QK_ROPE
PI = 3.141592653589793


def _rstd(P, eng_tile, b, src_col, dst_col, inv_n, eps):
    t = eng_tile
    P.op("dve", lambda e: e.tensor_scalar(out=dst_col, in0=src_col, scalar1=inv_n, scalar2=eps, op0=ALU.mult, op1=ALU.add),
         reads=[b], writes=[b])
    P.op("act", lambda e: e.activation(out=dst_col, in_=dst_col, func=AF.Sqrt), reads=[b], writes=[b])
    P.op("dve", lambda e: e.reciprocal(out=dst_col, in_=dst_col), reads=[b], writes=[b])


def mla_prep(k, idb_b, consts, Pm, col0, pos, S, t_base, q_norm_w, w_uq, kv_norm_w, w_ukv, qk_q_w, qk_k_w, QTn, QTr, KTn, KTr, Vh):
    P = k.P
    idb, b_id = idb_b
    with ExitStack() as st:
        wq = k.sb(st, [128, 6, 3072], BF16, "mwq"); b_wq = P.buf()
        wkv = k.sb(st, [128, 4, 4096], BF16, "mwkv"); b_wkv = P.buf()
        P.dma("pool", lambda e: e.dma_start(out=wq[:], in_=w_uq.rearrange("(c p) n -> p c n", p=128)), writes=[b_wq])
        P.dma("pool", lambda e: e.dma_start(out=wkv[:], in_=w_ukv.rearrange("(c p) n -> p c n", p=128)), writes=[b_wkv])
        qnw, b_qnw = load_bcast(k, st, q_norm_w, "mqnw")
        kvnw, b_kvnw = load_bcast(k, st, kv_norm_w, "mkvnw")
        gq, b_gq = load_bcast(k, st, qk_q_w, "mgq")
        gk, b_gk = load_bcast(k, st, qk_k_w, "mgk")
        fr = k.sb(st, [128, 32], F32, "mfr"); b_fr = P.buf()
        P.dma("sp", lambda e: e.dma_start(out=fr[:], in_=consts[:, 256:288]), writes=[b_fr])
        P.op("dve", lambda e: e.tensor_scalar(out=gq[:], in0=gq[:], scalar1=float(QK_HEAD) ** -0.5, scalar2=None, op0=ALU.mult),
             reads=[b_gq], writes=[b_gq])
        ca = k.sb(st, [128, MLA_IN], F32, "mca"); b_ca = P.buf()
        scr = k.sb(st, [128, 4096], F32, "mscr"); b_scr = P.buf()
        cb = k.sb(st, [128, Q_LORA + KV_LORA], BF16, "mcb"); b_cb = P.buf()
        cT = k.sb(st, [128, 10, 128], BF16, "mcT"); b_cT = P.buf()
        stt = k.sb(st, [128, 64], F32, "mst"); b_st = P.buf()
        pi = k.sb(st, [128, 2], mybir.dt.int32, "mpi"); b_pi = P.buf()
        cs = k.sb(st, [128, 4, 32], F32, "mcs"); b_cs = P.buf()
        ni = k.sb(st, [128, 2, 32], mybir.dt.int32, "mni"); b_ni = P.buf()
        q_sb = k.sb(st, [128, 16, 192], F32, "mq"); b_q = P.buf()
        kv_sb = k.sb(st, [128, 16, 256], F32, "mkv"); b_kv = P.buf()
        kp = k.sb(st, [128, 16, 64], F32, "mkp"); b_kp = P.buf()
        rtmp = k.sb(st, [128, 16, 64], F32, "mrt"); b_rtmp = P.buf()
        qb = k.sb(st, [128, 16, 192], BF16, "mqb"); b_qb = P.buf()
        kb = k.sb(st, [128, 16, 192], BF16, "mkb"); b_kb = P.buf()
        vb = k.sb(st, [128, 16, 128], BF16, "mvb"); b_vb = P.buf()
        oTn = [k.sb(st, [128, 16, 128], BF16, "moTn") for _ in range(2)]; b_oTn = [P.buf() for _ in range(2)]
        oTr = [k.sb(st, [64, 16, 128], BF16, "moTr") for _ in range(2)]; b_oTr = [P.buf() for _ in range(2)]
        pT = [k.ps(st, [128, 8, 128], BF16, "mpT") for _ in range(2)]; bpT = [P.buf() for _ in range(2)]
        pM = [k.ps(st, [128, 512], F32, "mpM") for _ in range(2)]; bpM = [P.buf() for _ in range(2)]
        npt = [0]

        def transpose_to(src_ap_fn, nblk, rows, dst_fn, b_src, b_dst):
            for g0 in range(0, nblk, 8):
                i = npt[0] % 2
                npt[0] += 1
                nb_ = min(8, nblk - g0)
                for j in range(nb_):
                    P.op("pe", lambda e, i=i, j=j, g0=g0: e.transpose(out=pT[i][0:rows, j, :], in_=src_ap_fn(g0 + j), identity=idb[:]),
                         reads=[b_src, b_id], writes=[bpT[i]], skip_same=True)
                if i == 0:
                    P.op("act", lambda e, i=i, g0=g0, nb_=nb_: e.copy(out=dst_fn(g0, nb_), in_=pT[i][0:rows, 0:nb_, :]), reads=[bpT[i]], writes=[b_dst])
                else:
                    P.op("dve", lambda e, i=i, g0=g0, nb_=nb_: e.tensor_copy(out=dst_fn(g0, nb_), in_=pT[i][0:rows, 0:nb_, :]), reads=[bpT[i]], writes=[b_dst])

        for ti in range(S // 128):
            r0 = t_base + ti * 128
            P.dma("sp", lambda e, r0=r0: e.dma_start(out=ca[:], in_=Pm[r0:r0 + 128, col0:col0 + MLA_IN]), writes=[b_ca])
            P.dma("sp", lambda e, r0=r0: e.dma_start(out=pi[:, 0:1], in_=pos[r0:r0 + 128, :]), writes=[b_pi])
            P.op("dve", lambda e: e.memset(stt[:], 0.0), writes=[b_st])
            P.op("act", lambda e: e.activation(out=scr[:, 0:Q_LORA], in_=ca[:, 0:Q_LORA], func=AF.Square, accum_out=stt[:, 0:1]),
                 reads=[b_ca], writes=[b_scr, b_st])
            P.op("act", lambda e: e.activation(out=scr[:, 0:KV_LORA], in_=ca[:, Q_LORA:Q_LORA + KV_LORA], func=AF.Square, accum_out=stt[:, 1:2]),
                 reads=[b_ca], writes=[b_scr, b_st])
            P.op("act", lambda e: e.activation(out=scr[:, 0:QK_ROPE], in_=ca[:, Q_LORA + KV_LORA:MLA_IN], func=AF.Square, accum_out=stt[:, 2:3]),
                 reads=[b_ca], writes=[b_scr, b_st])
            _rstd(P, stt, b_st, stt[:, 0:1], stt[:, 4:5], 1.0 / Q_LORA, 1e-6)
            _rstd(P, stt, b_st, stt[:, 1:2], stt[:, 5:6], 1.0 / KV_LORA, 1e-6)
            P.op("dve", lambda e: e.scalar_tensor_tensor(out=cb[:, 0:Q_LORA], in0=ca[:, 0:Q_LORA], scalar=stt[:, 4:5], in1=qnw[:],
                                                         op0=ALU.mult, op1=ALU.mult), reads=[b_ca, b_st, b_qnw], writes=[b_cb])
            P.op("dve", lambda e: e.scalar_tensor_tensor(out=cb[:, Q_LORA:], in0=ca[:, Q_LORA:Q_LORA + KV_LORA], scalar=stt[:, 5:6], in1=kvnw[:],
                                                         op0=ALU.mult, op1=ALU.mult), reads=[b_ca, b_st, b_kvnw], writes=[b_cb])
            transpose_to(lambda c: cb[:, c * 128:(c + 1) * 128], 10, 128, lambda g0, nb_: cT[:, g0:g0 + nb_, :], b_cb, b_cT)
            nm = 0
            for cbk in range(6):
                i = nm % 2; nm += 1
                for c in range(6):
                    P.op("pe", lambda e, i=i, c=c, cbk=cbk: e.matmul(pM[i][:], lhsT=cT[:, c, :], rhs=wq[:, c, cbk * 512:(cbk + 1) * 512],
                                                                    start=(c == 0), stop=(c == 5)), reads=[b_cT, b_wq], writes=[bpM[i]], skip_same=True)
                P.op("act", lambda e, i=i, cbk=cbk: e.copy(out=q_sb[:].rearrange("p h d -> p (h d)")[:, cbk * 512:(cbk + 1) * 512], in_=pM[i][:]),
                     reads=[bpM[i]], writes=[b_q])
            for cbk in range(8):
                i = nm % 2; nm += 1
                for c in range(4):
                    P.op("pe", lambda e, i=i, c=c, cbk=cbk: e.matmul(pM[i][:], lhsT=cT[:, 6 + c, :], rhs=wkv[:, c, cbk * 512:(cbk + 1) * 512],
                                                                    start=(c == 0), stop=(c == 3)), reads=[b_cT, b_wkv], writes=[bpM[i]], skip_same=True)
                P.op("dve", lambda e, i=i, cbk=cbk: e.tensor_copy(out=kv_sb[:].rearrange("p h d -> p (h d)")[:, cbk * 512:(cbk + 1) * 512], in_=pM[i][:]),
                     reads=[bpM[i]], writes=[b_kv])
            P.op("act", lambda e: e.activation(out=scr[:, 0:3072], in_=q_sb[:].rearrange("p h d -> p (h d)"), func=AF.Square),
                 reads=[b_q], writes=[b_scr])
            P.op("dve", lambda e: e.reduce_sum(out=stt[:, 8:24], in_=scr[:, 0:3072].rearrange("p (h d) -> p h d", d=192), axis=AX.X),
                 reads=[b_scr], writes=[b_st])
            P.op("act", lambda e: e.activation(out=scr[:, 0:2048].rearrange("p (h d) -> p h d", d=128), in_=kv_sb[:, :, 0:128], func=AF.Square),
                 reads=[b_kv], writes=[b_scr])
            P.op("dve", lambda e: e.reduce_sum(out=stt[:, 24:40], in_=scr[:, 0:2048].rearrange("p (h d) -> p h d", d=128), axis=AX.X),
                 reads=[b_scr], writes=[b_st])
            P.op("dve", lambda e: e.tensor_scalar(out=stt[:, 24:40], in0=stt[:, 24:40], scalar1=stt[:, 2:3], scalar2=None, op0=ALU.add),
                 reads=[b_st], writes=[b_st])
            _rstd(P, stt, b_st, stt[:, 8:24], stt[:, 8:24], 1.0 / QK_HEAD, 1e-6)
            _rstd(P, stt, b_st, stt[:, 24:40], stt[:, 24:40], 1.0 / QK_HEAD, 1e-6)
            rq = stt[:, 8:24].unsqueeze(2)
            rk = stt[:, 24:40].unsqueeze(2)
            P.op("dve", lambda e: e.tensor_tensor(out=q_sb[:], in0=q_sb[:], in1=rq.to_broadcast([128, 16, 192]), op=ALU.mult),
                 reads=[b_q, b_st], writes=[b_q])
            P.op("dve", lambda e: e.tensor_tensor(out=q_sb[:], in0=q_sb[:], in1=gq[:].unsqueeze(1).to_broadcast([128, 16, 192]), op=ALU.mult),
                 reads=[b_q, b_gq], writes=[b_q])
            P.op("pool", lambda e: e.tensor_tensor(out=kv_sb[:, :, 0:128], in0=kv_sb[:, :, 0:128], in1=rk.to_broadcast([128, 16, 128]), op=ALU.mult),
                 reads=[b_kv, b_st], writes=[b_kv])
            P.op("pool", lambda e: e.tensor_tensor(out=kv_sb[:, :, 0:128], in0=kv_sb[:, :, 0:128],
                                                   in1=gk[:, 0:128].unsqueeze(1).to_broadcast([128, 16, 128]), op=ALU.mult),
                 reads=[b_kv, b_gk], writes=[b_kv])
            P.op("pool", lambda e: e.tensor_tensor(out=kp[:], in0=ca[:, Q_LORA + KV_LORA:MLA_IN].unsqueeze(1).to_broadcast([128, 16, 64]),
                                                   in1=rk.to_broadcast([128, 16, 64]), op=ALU.mult), reads=[b_ca, b_st], writes=[b_kp])
            P.op("pool", lambda e: e.tensor_tensor(out=kp[:], in0=kp[:], in1=gk[:, 128:192].unsqueeze(1).to_broadcast([128, 16, 64]), op=ALU.mult),
                 reads=[b_kp, b_gk], writes=[b_kp])
            P.op("dve", lambda e: e.tensor_copy(out=stt[:, 40:41], in_=pi[:, 0:1]), reads=[b_pi], writes=[b_st])
            P.op("dve", lambda e: e.tensor_scalar(out=cs[:, 0, :], in0=fr[:], scalar1=stt[:, 40:41], scalar2=None, op0=ALU.mult),
                 reads=[b_fr, b_st], writes=[b_cs])
            P.op("dve", lambda e: e.tensor_scalar(out=cs[:, 1, :], in0=cs[:, 0, :], scalar1=0.5 * PI, scalar2=None, op0=ALU.add),
                 reads=[b_cs], writes=[b_cs])
            ph, wk = cs[:, 0:2, :], cs[:, 2:4, :]
            P.op("dve", lambda e: e.tensor_scalar(out=wk, in0=ph, scalar1=1.0 / (2 * PI), scalar2=None, op0=ALU.mult), reads=[b_cs], writes=[b_cs])
            P.op("dve", lambda e: e.tensor_copy(out=ni[:], in_=wk), reads=[b_cs], writes=[b_ni])
            P.op("dve", lambda e: e.tensor_copy(out=wk, in_=ni[:]), reads=[b_ni], writes=[b_cs])
            P.op("dve", lambda e: e.scalar_tensor_tensor(out=ph, in0=wk, scalar=-2 * PI, in1=ph, op0=ALU.mult, op1=ALU.add), reads=[b_cs], writes=[b_cs])
            P.op("dve", lambda e: e.tensor_scalar(out=wk, in0=ph, scalar1=PI, scalar2=-2 * PI, op0=ALU.is_gt, op1=ALU.mult), reads=[b_cs], writes=[b_cs])
            P.op("dve", lambda e: e.tensor_tensor(out=ph, in0=ph, in1=wk, op=ALU.add), reads=[b_cs], writes=[b_cs])
            P.op("dve", lambda e: e.tensor_scalar(out=wk, in0=ph, scalar1=-PI, scalar2=2 * PI, op0=ALU.is_lt, op1=ALU.mult), reads=[b_cs], writes=[b_cs])
            P.op("dve", lambda e: e.tensor_tensor(out=ph, in0=ph, in1=wk, op=ALU.add), reads=[b_cs], writes=[b_cs])
            P.op("act", lambda e: e.activation(out=cs[:, 0:2, :], in_=cs[:, 0:2, :], func=AF.Sin), reads=[b_cs], writes=[b_cs])
            sinb = cs[:, 0, :].unsqueeze(1).to_broadcast([128, 16, 32])
            cosb = cs[:, 1, :].unsqueeze(1).to_broadcast([128, 16, 32])
            for (src, b_src, lo, dstb, b_dstb) in ((q_sb, b_q, 128, qb, b_qb), (kp, b_kp, 0, kb, b_kb)):
                x1, x2 = src[:, :, lo:lo + 32], src[:, :, lo + 32:lo + 64]
                P.op("dve", lambda e, x1=x1: e.tensor_tensor(out=rtmp[:, :, 0:32], in0=x1, in1=cosb, op=ALU.mult), reads=[b_src, b_cs], writes=[b_rtmp])
                P.op("dve", lambda e, x2=x2: e.tensor_tensor(out=rtmp[:, :, 32:64], in0=x2, in1=sinb, op=ALU.mult), reads=[b_src, b_cs], writes=[b_rtmp])
                P.op("dve", lambda e, dstb=dstb: e.tensor_tensor(out=dstb[:, :, 128:160], in0=rtmp[:, :, 0:32], in1=rtmp[:, :, 32:64], op=ALU.subtract),
                     reads=[b_rtmp], writes=[b_dstb])
                P.op("dve", lambda e, x2=x2: e.tensor_tensor(out=rtmp[:, :, 0:32], in0=x2, in1=cosb, op=ALU.mult), reads=[b_src, b_cs, b_dstb], writes=[b_rtmp])
                P.op("dve", lambda e, x1=x1: e.tensor_tensor(out=rtmp[:, :, 32:64], in0=x1, in1=sinb, op=ALU.mult), reads=[b_src, b_cs], writes=[b_rtmp])
                P.op("dve", lambda e, dstb=dstb: e.tensor_tensor(out=dstb[:, :, 160:192], in0=rtmp[:, :, 0:32], in1=rtmp[:, :, 32:64], op=ALU.add),
                     reads=[b_rtmp], writes=[b_dstb])
            P.op("act", lambda e: e.copy(out=qb[:, :, 0:128], in_=q_sb[:, :, 0:128]), reads=[b_q], writes=[b_qb])
            P.op("act", lambda e: e.copy(out=kb[:, :, 0:128], in_=kv_sb[:, :, 0:128]), reads=[b_kv], writes=[b_kb])
            P.op("pool", lambda e: e.tensor_copy(out=vb[:], in_=kv_sb[:, :, 128:256]), reads=[b_kv], writes=[b_vb])
            P.dma("sp", lambda e, ti=ti: e.dma_start(out=Vh[:, ti * 128:(ti + 1) * 128, :].rearrange("h t d -> t h d"), in_=vb[:]), reads=[b_vb])
            for (srcb, b_srcb, Dn, Dr) in ((qb, b_qb, QTn, QTr), (kb, b_kb, KTn, KTr)):
                j = ti % 2
                transpose_to(lambda h, srcb=srcb: srcb[:, h, 0:128], 16, 128, lambda g0, nb_, j=j: oTn[j][:, g0:g0 + nb_, :], b_srcb, b_oTn[j])
                transpose_to(lambda h, srcb=srcb: srcb[:, h, 128:192], 16, 64, lambda g0, nb_, j=j: oTr[j][:, g0:g0 + nb_, :], b_srcb, b_oTr[j])
                P.dma("sp", lambda e, j=j, Dn=Dn, ti=ti: e.dma_start(out=Dn[:, :, ti * 128:(ti + 1) * 128].rearrange("h d t -> d h t"), in_=oTn[j][:]),
                      reads=[b_oTn[j]])
                P.dma("sp", lambda e, j=j, Dr=Dr, ti=ti: e.dma_start(out=Dr[:, :, ti * 128:(ti + 1) * 128].rearrange("h d t -> d h t"), in_=oTr[j][:]),
                      reads=[b_oTr[j]])
        k.end_stage()


def mla_attn(k, consts, qk_q_w, qk_k_w, QTn, QTr, KTn, KTr, Vh, S, Y, y_row0, y_col0):
    P = k.P
    nb = S // 128
    with ExitStack() as st:
        mk = k.sb(st, [128, 128], F32, "amk"); b_mk = P.buf()
        P.dma("sp", lambda e: e.dma_start(out=mk[:], in_=consts[:, 128:256]), writes=[b_mk])
        mkb = k.sb(st, [128, 128], BF16, "amkb"); b_mkb = P.buf()
        P.op("dve", lambda e: e.tensor_copy(out=mkb[:], in_=mk[:]), reads=[b_mk], writes=[b_mkb])
        gq, b_gq = load_bcast(k, st, qk_q_w, "agq"); gk, b_gk = load_bcast(k, st, qk_k_w, "agk")
        bd = k.sb(st, [128, 4], F32, "abd"); b_bd = P.buf()
        P.op("dve", lambda e: e.reduce_max(out=bd[:, 0:1], in_=gq[:], axis=AX.X, apply_absolute_value=True), reads=[b_gq], writes=[b_bd])
        P.op("dve", lambda e: e.reduce_max(out=bd[:, 1:2], in_=gk[:], axis=AX.X, apply_absolute_value=True), reads=[b_gk], writes=[b_bd])
        P.op("dve", lambda e: e.tensor_tensor(out=bd[:, 2:3], in0=bd[:, 0:1], in1=bd[:, 1:2], op=ALU.mult), reads=[b_bd], writes=[b_bd])
        P.op("dve", lambda e: e.tensor_scalar(out=bd[:, 2:3], in0=bd[:, 2:3], scalar1=-(float(QK_HEAD) ** 0.5), scalar2=None, op0=ALU.mult),
             reads=[b_bd], writes=[b_bd])
        kn = [k.sb(st, [128, S], BF16, "akn") for _ in range(2)]; kr = [k.sb(st, [64, S], BF16, "akr") for _ in range(2)]
        qn = [k.sb(st, [128, S], BF16, "aqn") for _ in range(2)]; qr = [k.sb(st, [64, S], BF16, "aqr") for _ in range(2)]
        vv = [k.sb(st, [128, nb, 132], BF16, "avv") for _ in range(2)]
        bh = [P.buf() for _ in range(2)]
        pS = [k.ps(st, [128, 512], F32, "apS") for _ in range(2)]; bpS = [P.buf() for _ in range(2)]
        pO = [k.ps(st, [128, 512], F32, "apO") for _ in range(4)]; bpO = [P.buf() for _ in range(4)]
        pt = [k.sb(st, [128, 512], BF16, "apt") for _ in range(3)]; bpt = [P.buf() for _ in range(3)]
        ob = [k.sb(st, [128, 132], F32, "aob") for _ in range(2)]; bob = [P.buf() for _ in range(2)]
        nS = nP = nO = 0
        for h in range(MLA_H):
            j = h % 2
            for (dst, src) in ((kn[j], KTn[h]), (kr[j], KTr[h]), (qn[j], QTn[h]), (qr[j], QTr[h])):
                P.dma("sp", lambda e, dst=dst, src=src: e.dma_start(out=dst[:], in_=src), writes=[bh[j]])
            P.dma("sp", lambda e, j=j, h=h: e.dma_start(out=vv[j][:, :, 0:128], in_=Vh[h].rearrange("(b p) d -> p b d", p=128)), writes=[bh[j]])
            P.op("pool", lambda e, j=j: e.memset(vv[j][:, :, 128:129], 1.0), writes=[bh[j]])
            for g0 in range(0, nb, 4):
                ng = min(4, nb - g0)
                for kb_ in range(g0 + ng):
                    lo = max(kb_, g0)
                    c0, c1 = (lo - g0) * 128, ng * 128
                    si = nS % 2; nS += 1
                    P.op("pe", lambda e, si=si, j=j, kb_=kb_, g0=g0, c0=c0, c1=c1: e.matmul(
                        pS[si][:, c0:c1], lhsT=kn[j][:, kb_ * 128:(kb_ + 1) * 128], rhs=qn[j][:, g0 * 128 + c0:g0 * 128 + c1], start=True, stop=False),
                        reads=[bh[j]], writes=[bpS[si]], skip_same=True)
                    P.op("pe", lambda e, si=si, j=j, kb_=kb_, g0=g0, c0=c0, c1=c1: e.matmul(
                        pS[si][:, c0:c1], lhsT=kr[j][:, kb_ * 128:(kb_ + 1) * 128], rhs=qr[j][:, g0 * 128 + c0:g0 * 128 + c1], start=False, stop=True),
                        reads=[bh[j]], writes=[bpS[si]], skip_same=True)
                    pi_ = nP % 3; nP += 1
                    P.op("act", lambda e, si=si, pi_=pi_, c0=c0, c1=c1: e.activation(out=pt[pi_][:, c0:c1], in_=pS[si][:, c0:c1], func=AF.Exp,
                                                                                    bias=bd[:, 2:3], scale=1.0),
                         reads=[bpS[si], b_bd], writes=[bpt[pi_]])
                    if kb_ >= g0:
                        P.op("dve", lambda e, pi_=pi_, c0=c0: e.tensor_tensor(out=pt[pi_][:, c0:c0 + 128], in0=pt[pi_][:, c0:c0 + 128], in1=mkb[:], op=ALU.mult),
                             reads=[bpt[pi_], b_mkb], writes=[bpt[pi_]])
                    for qi in range(lo, g0 + ng):
                        a = qi - g0
                        P.op("pe", lambda e, pi_=pi_, a=a, j=j, kb_=kb_, qi=qi: e.matmul(
                            pO[a][:, 0:129], lhsT=pt[pi_][:, a * 128:(a + 1) * 128], rhs=vv[j][:, kb_, 0:129], start=(kb_ == 0), stop=(kb_ == qi)),
                            reads=[bpt[pi_], bh[j]], writes=[bpO[a]], skip_same=True)
                for a in range(ng):
                    oi = nO % 2; nO += 1
                    qi = g0 + a
                    P.op("dve", lambda e, oi=oi, a=a: e.reciprocal(out=ob[oi][:, 129:130], in_=pO[a][:, 128:129]), reads=[bpO[a]], writes=[bob[oi]])
                    P.op("dve", lambda e, oi=oi, a=a: e.tensor_scalar(out=ob[oi][:, 0:128], in0=pO[a][:, 0:128], scalar1=ob[oi][:, 129:130], scalar2=None,
                                                                     op0=ALU.mult), reads=[bpO[a], bob[oi]], writes=[bob[oi]])
                    P.dma("sp", lambda e, oi=oi, qi=qi, h=h: e.dma_start(out=Y[y_row0 + qi * 128:y_row0 + (qi + 1) * 128, y_col0 + h * 128:y_col0 + (h + 1) * 128],
                                                                        in_=ob[oi][:, 0:128]), reads=[bob[oi]])
        k.end_stage()


RW_H, RW_N, RW_C = 32, 64, 64
RW_W = RW_H * RW_N


def rwkv_scan(k, consts, AT, BT, KT_, RT, GC, Vtm, Btm, Ktm, bonus, Gt, lnw, lnb, S, Y, y_row0, y_col0, HB=4, CB=8):
    P = k.P
    C, N = RW_C, RW_N
    nch = S // C
    with ExitStack() as st:
        ms = k.sb(st, [64, 4, 64], F32, "smask"); b_ms = P.buf()
        P.dma("sp", lambda e: e.dma_start(out=ms[:, 0, :], in_=consts[0:64, 0:64]), writes=[b_ms])
        P.dma("sp", lambda e: e.dma_start(out=ms[:, 1, :], in_=consts[0:64, 128:192]), writes=[b_ms])
        P.dma("sp", lambda e: e.dma_start(out=ms[:, 2, :], in_=consts[0:64, 288:352]), writes=[b_ms])
        P.dma("sp", lambda e: e.dma_start(out=ms[:, 3, :], in_=consts[0:64, 352:416]), writes=[b_ms])
        mI = ms[:, 0, :].unsqueeze(1).to_broadcast([64, HB, 64]); mIU = ms[:, 1, :].unsqueeze(1).to_broadcast([64, HB, 64])
        mSU = ms[:, 2, :].unsqueeze(1).to_broadcast([64, HB, 64]); mSL = ms[:, 3, :].unsqueeze(1).to_broadcast([64, HB, 64])
        fm = [[k.sb(st, [64, HB, CB * C], F32, "sfm") for _ in range(4)] for _ in range(2)]; b_fm = [P.buf() for _ in range(2)]
        gc = [k.sb(st, [64, HB, CB], F32, "sgc") for _ in range(2)]
        tm = [[k.sb(st, [64, CB, HB * N], F32, "stm") for _ in range(4)] for _ in range(2)]; b_tm = [P.buf() for _ in range(2)]
        bn = [k.sb(st, [64, CB, HB], F32, "sbn") for _ in range(2)]
        lw_t = k.sb(st, [64, HB * N], F32, "slw"); lb_t = k.sb(st, [64, HB * N], F32, "slb"); b_ln = P.buf()
        Z = k.sb(st, [64, HB, N], F32, "sZ"); b_Z = P.buf()
        names = ("Nm", "Lm", "LkT", "MbT", "MkT", "Pw", "Pt", "X", "Xt", "RHS", "U", "yt", "yc", "sq")
        W = {n: k.sb(st, [64, HB, 64], F32, "s" + n) for n in names}; bW = {n: P.buf() for n in names}
        stt = k.sb(st, [64, HB, 4], F32, "sst"); b_st = P.buf()
        ot = [k.sb(st, [64, HB, 64], F32, "sot") for _ in range(2)]; b_ot = [P.buf() for _ in range(2)]
        pp = [k.ps(st, [64, HB, 64], F32, "spp") for _ in range(7)]; bpp = [P.buf() for _ in range(7)]

        def mm_heads(pi, lhs_fn, rhs_fn, reads):
            mm_acc(pi, [(lhs_fn, rhs_fn)], reads)

        def mm_acc(pi, terms, reads):
            nt_ = len(terms)
            for h in range(HB):
                for ti_, (lhs_fn, rhs_fn) in enumerate(terms):
                    l_ap, r_ap = lhs_fn(h), rhs_fn(h)
                    P.op("pe", lambda e, h=h, l_ap=l_ap, r_ap=r_ap, ti_=ti_: e.matmul(
                        pp[pi][:, h, :], lhsT=l_ap, rhs=r_ap, start=(ti_ == 0), stop=(ti_ == nt_ - 1)),
                        reads=reads, writes=[bpp[pi]], skip_same=True)

        for h0 in range(0, RW_H, HB):
            r0, r1 = h0 * N, (h0 + HB) * N
            P.dma("sp", lambda e, r0=r0, r1=r1: e.dma_start(out=lw_t[:], in_=lnw[r0:r1].partition_broadcast(64)), writes=[b_ln])
            P.dma("sp", lambda e, r0=r0, r1=r1: e.dma_start(out=lb_t[:], in_=lnb[r0:r1].partition_broadcast(64)), writes=[b_ln])
            P.op("dve", lambda e: e.memset(Z[:], 0.0), writes=[b_Z])
            for cb in range(nch // CB):
                j = cb % 2
                t0 = cb * CB * C
                for i, src in enumerate((AT, BT, KT_, RT)):
                    P.dma("sp", lambda e, i=i, src=src, j=j, t0=t0, r0=r0, r1=r1: e.dma_start(
                        out=fm[j][i][:], in_=src[r0:r1, t0:t0 + CB * C].rearrange("(h j) t -> j h t", j=64)), writes=[b_fm[j]])
                P.dma("sp", lambda e, j=j, cb=cb, r0=r0, r1=r1: e.dma_start(out=gc[j][:], in_=GC[r0:r1, cb * CB:(cb + 1) * CB].rearrange("(h j) c -> j h c", j=64)),
                      writes=[b_fm[j]])
                for i, src in enumerate((Vtm, Btm, Ktm, Gt)):
                    P.dma("sp", lambda e, i=i, src=src, j=j, t0=t0, r0=r0, r1=r1: e.dma_start(
                        out=tm[j][i][:], in_=src[t0:t0 + CB * C, r0:r1].rearrange("(c t) f -> t c f", t=64)), writes=[b_tm[j]])
                P.dma("sp", lambda e, j=j, t0=t0, h0=h0: e.dma_start(out=bn[j][:], in_=bonus[t0:t0 + CB * C, h0:h0 + HB].rearrange("(c t) h -> t c h", t=64)),
                      writes=[b_tm[j]])
                A_, B_, K_, R_ = fm[j]
                V_, Bm_, Km_, G_ = tm[j]
                for ci in range(CB):
                    cs_ = slice(ci * C, (ci + 1) * C)
                    fr_ = [b_fm[j]]
                    mm_heads(0, lambda h: B_[:, h, cs_], lambda h: A_[:, h, cs_], fr_)
                    mm_heads(1, lambda h: A_[:, h, cs_], lambda h: B_[:, h, cs_], fr_)
                    mm_heads(2, lambda h: K_[:, h, cs_], lambda h: A_[:, h, cs_], fr_)
                    mm_heads(3, lambda h: B_[:, h, cs_], lambda h: R_[:, h, cs_], fr_)
                    mm_heads(4, lambda h: K_[:, h, cs_], lambda h: R_[:, h, cs_], fr_)
                    for (pi, nm, mk) in ((0, "Nm", mSU), (1, "Lm", mSL), (2, "LkT", mSU), (3, "MbT", mIU), (4, "MkT", mIU)):
                        P.op("dve", lambda e, pi=pi, nm=nm, mk=mk: e.tensor_tensor(out=W[nm][:], in0=pp[pi][:], in1=mk, op=ALU.mult),
                             reads=[bpp[pi], b_ms], writes=[bW[nm]])
                    P.op("pool", lambda e: e.tensor_copy(out=W["Pw"][:], in_=W["Nm"][:]), reads=[bW["Nm"]], writes=[bW["Pw"]])
                    P.op("pool", lambda e: e.tensor_copy(out=W["Pt"][:], in_=W["Lm"][:]), reads=[bW["Lm"]], writes=[bW["Pt"]])
                    P.op("dve", lambda e: e.tensor_tensor(out=W["X"][:], in0=W["Nm"][:], in1=mI, op=ALU.add), reads=[bW["Nm"], b_ms], writes=[bW["X"]])
                    P.op("dve", lambda e: e.tensor_tensor(out=W["Xt"][:], in0=W["Lm"][:], in1=mI, op=ALU.add), reads=[bW["Lm"], b_ms], writes=[bW["Xt"]])
                    for rd in range(5):
                        last = rd == 4
                        mm_heads(5, lambda h: W["Pt"][:, h, :], lambda h: W["Pw"][:, h, :], [bW["Pt"], bW["Pw"]])
                        if not last:
                            mm_heads(6, lambda h: W["Pw"][:, h, :], lambda h: W["Pt"][:, h, :], [bW["Pt"], bW["Pw"]])
                        P.op("act", lambda e: e.copy(out=W["Pw"][:], in_=pp[5][:]), reads=[bpp[5]], writes=[bW["Pw"]])
                        if not last:
                            P.op("act", lambda e: e.copy(out=W["Pt"][:], in_=pp[6][:]), reads=[bpp[6]], writes=[bW["Pt"]])
                        mm_heads(5, lambda h: W["Xt"][:, h, :], lambda h: W["Pw"][:, h, :], [bW["Xt"], bW["Pw"]])
                        if not last:
                            mm_heads(6, lambda h: W["Pw"][:, h, :], lambda h: W["Xt"][:, h, :], [bW["Xt"], bW["Pw"]])
                        P.op("dve", lambda e: e.tensor_tensor(out=W["X"][:], in0=W["X"][:], in1=pp[5][:], op=ALU.add), reads=[bW["X"], bpp[5]], writes=[bW["X"]])
                        if not last:
                            P.op("dve", lambda e: e.tensor_tensor(out=W["Xt"][:], in0=W["Xt"][:], in1=pp[6][:], op=ALU.add), reads=[bW["Xt"], bpp[6]], writes=[bW["Xt"]])
                    Vc = lambda h: V_[:, ci, h * N:(h + 1) * N]
                    mm_acc(0, [(lambda h: A_[:, h, cs_], lambda h: Z[:, h, :]), (lambda h: W["LkT"][:, h, :], Vc)], fr_ + [b_Z, bW["LkT"], b_tm[j]])
                    P.op("act", lambda e: e.copy(out=W["RHS"][:], in_=pp[0][:]), reads=[bpp[0]], writes=[bW["RHS"]])
                    mm_heads(1, lambda h: W["X"][:, h, :], lambda h: W["RHS"][:, h, :], [bW["X"], bW["RHS"]])
                    P.op("act", lambda e: e.copy(out=W["U"][:], in_=pp[1][:]), reads=[bpp[1]], writes=[bW["U"]])
                    mm_acc(2, [(lambda h: R_[:, h, cs_], lambda h: Z[:, h, :]), (lambda h: W["MbT"][:, h, :], lambda h: W["U"][:, h, :]),
                               (lambda h: W["MkT"][:, h, :], Vc)], fr_ + [b_Z, bW["MbT"], bW["U"], bW["MkT"], b_tm[j]])
                    P.op("act", lambda e: e.copy(out=W["yt"][:], in_=pp[2][:]), reads=[bpp[2]], writes=[bW["yt"]])
                    mm_acc(3, [(lambda h: Bm_[:, ci, h * N:(h + 1) * N], lambda h: W["U"][:, h, :]), (lambda h: Km_[:, ci, h * N:(h + 1) * N], Vc)],
                           [b_tm[j], bW["U"]])
                    P.op("dve", lambda e: e.tensor_tensor(out=Z[:], in0=Z[:], in1=pp[3][:], op=ALU.add), reads=[b_Z, bpp[3]], writes=[b_Z])
                    gcb = gc[j][:, :, ci:ci + 1].to_broadcast([64, HB, N])
                    P.op("dve", lambda e, gcb=gcb: e.tensor_tensor(out=Z[:], in0=Z[:], in1=gcb, op=ALU.mult),
                         reads=[b_Z, b_fm[j]], writes=[b_Z])
                    y_, yc, sq = W["yt"], W["yc"], W["sq"]
                    P.op("dve", lambda e: e.reduce_sum(out=stt[:, :, 0:1], in_=y_[:], axis=AX.X), reads=[bW["yt"]], writes=[b_st])
                    P.op("dve", lambda e: e.tensor_scalar(out=stt[:, :, 0:1], in0=stt[:, :, 0:1], scalar1=-1.0 / N, scalar2=None, op0=ALU.mult),
                         reads=[b_st], writes=[b_st])
                    P.op("dve", lambda e: e.tensor_tensor(out=yc[:], in0=y_[:], in1=stt[:, :, 0:1].to_broadcast([64, HB, N]), op=ALU.add),
                         reads=[bW["yt"], b_st], writes=[bW["yc"]])
                    P.op("act", lambda e: e.activation(out=sq[:], in_=yc[:], func=AF.Square), reads=[bW["yc"]], writes=[bW["sq"]])
                    P.op("dve", lambda e: e.reduce_sum(out=stt[:, :, 1:2], in_=sq[:], axis=AX.X), reads=[bW["sq"]], writes=[b_st])
                    P.op("dve", lambda e: e.tensor_scalar(out=stt[:, :, 1:2], in0=stt[:, :, 1:2], scalar1=1.0 / N, scalar2=64e-5, op0=ALU.mult, op1=ALU.add),
                         reads=[b_st], writes=[b_st])
                    P.op("act", lambda e: e.activation(out=stt[:, :, 1:2], in_=stt[:, :, 1:2], func=AF.Sqrt), reads=[b_st], writes=[b_st])
                    P.op("dve", lambda e: e.reciprocal(out=stt[:, :, 1:2], in_=stt[:, :, 1:2]), reads=[b_st], writes=[b_st])
                    P.op("dve", lambda e: e.tensor_tensor(out=yc[:], in0=yc[:], in1=stt[:, :, 1:2].to_broadcast([64, HB, N]), op=ALU.mult),
                         reads=[bW["yc"], b_st], writes=[bW["yc"]])
                    P.op("dve", lambda e: e.tensor_tensor(out=yc[:], in0=yc[:], in1=lw_t[:].rearrange("p (h n) -> p h n", n=N), op=ALU.mult),
                         reads=[bW["yc"], b_ln], writes=[bW["yc"]])
                    P.op("pool", lambda e: e.tensor_tensor(out=yc[:], in0=yc[:], in1=lb_t[:].rearrange("p (h n) -> p h n", n=N), op=ALU.add),
                         reads=[bW["yc"], b_ln], writes=[bW["yc"]])
                    Vc3 = V_[:, ci, :].rearrange("p (h n) -> p h n", n=N)
                    bnb = bn[j][:, ci, :].unsqueeze(2).to_broadcast([64, HB, N])
                    P.op("pool", lambda e, bnb=bnb, Vc3=Vc3: e.tensor_tensor(out=sq[:], in0=Vc3, in1=bnb, op=ALU.mult),
                         reads=[b_tm[j], bW["sq"]], writes=[bW["sq"]])
                    P.op("pool", lambda e: e.tensor_tensor(out=yc[:], in0=yc[:], in1=sq[:], op=ALU.add), reads=[bW["yc"], bW["sq"]], writes=[bW["yc"]])
                    oi = ci % 2
                    g3 = G_[:, ci, :].rearrange("p (h n) -> p h n", n=N)
                    P.op("dve", lambda e, oi=oi, g3=g3: e.tensor_tensor(out=ot[oi][:], in0=yc[:], in1=g3, op=ALU.mult),
                         reads=[bW["yc"], b_tm[j]], writes=[b_ot[oi]])
                    tr = y_row0 + t0 + ci * C
                    P.dma("sp", lambda e, oi=oi, tr=tr, r0=r0, r1=r1: e.dma_start(out=Y[tr:tr + C, y_col0 + r0:y_col0 + r1], in_=ot[oi][:].rearrange("p h n -> p (h n)")),
                          reads=[b_ot[oi]])
        k.end_stage()


RW_IN = 3 * RW_W + 64 + 64 + 128
C_BONES, C_SEL, C_RMASK, C_W = 512, 640, 768, 1280


def rwkv_prep(k, consts, PT, t_base, S, rwp, w2, a2, g2, AT, BT, KT_, RT, GC, Vtm, Btm, Ktm, bonus, Gt, TB=512):
    P = k.P
    n = TB
    nsl = n // 128
    with ExitStack() as st:
        def tile(shape, name, dt=F32):
            return k.sb(st, shape, dt, name), P.buf()
        idf, b_idf = tile([128, 128], "ridf"); bones, b_bones = tile([128, 128], "rbo"); sel, b_sel = tile([128, 2], "rsel")
        rmask, b_rm = tile([128, n], "rrm"); prm, b_prm = tile([128, 132], "rprm"); omk, b_omk = tile([128, 50 + 16], "romk")
        wl, b_wl = tile([128, RW_W], "rwl"); g2s, b_g2 = tile([128, RW_W], "rg2")
        P.dma("sp", lambda e: e.dma_start(out=idf[:], in_=consts[:, 0:128]), writes=[b_idf])
        P.dma("sp", lambda e: e.dma_start(out=bones[:], in_=consts[:, C_BONES:C_BONES + 128]), writes=[b_bones])
        P.dma("sp", lambda e: e.dma_start(out=sel[:], in_=consts[:, C_SEL:C_SEL + 2]), writes=[b_sel])
        P.dma("sp", lambda e: e.dma_start(out=rmask[:], in_=consts[:, C_RMASK:C_RMASK + n]), writes=[b_rm])
        P.dma("sp", lambda e: e.dma_start(out=prm[:, 0:130], in_=rwp[:, :]), writes=[b_prm])
        P.dma("sp", lambda e: e.dma_start(out=wl[0:64, :], in_=w2[:, :]), writes=[b_wl])
        P.dma("sp", lambda e: e.dma_start(out=wl[64:128, :], in_=a2[:, :]), writes=[b_wl])
        P.dma("sp", lambda e: e.dma_start(out=g2s[:], in_=g2[:, :]), writes=[b_g2])
        MU, W0, A0, KK_, KA, RK = 0, 50, 66, 82, 98, 114
        P.op("dve", lambda e: e.tensor_scalar(out=omk[:, 0:50], in0=prm[:, MU:MU + 50], scalar1=-1.0, scalar2=1.0, op0=ALU.mult, op1=ALU.add),
             reads=[b_prm], writes=[b_omk])
        P.op("dve", lambda e: e.tensor_scalar(out=omk[:, 50:66], in0=prm[:, KA:KA + 16], scalar1=-1.0, scalar2=1.0, op0=ALU.mult, op1=ALU.add),
             reads=[b_prm], writes=[b_omk])
        raw = [tile([128, 1 + n], "rraw") for _ in range(4)]
        la, b_la = tile([128, n], "rla"); sg, b_sg = tile([128, n], "rsg")
        rm, b_r = tile([128, n], "rr"); km, b_k = tile([128, n], "rk"); vm, b_v = tile([128, n], "rv")
        lw, b_lw = tile([128, n], "rlw"); aa, b_a = tile([128, n], "ra"); kk, b_kk = tile([128, n], "rkk"); kp, b_kp = tile([128, n], "rkp")
        cl, b_cl = tile([128, n], "rcl"); ep, b_ep = tile([128, n], "rep"); en, b_en = tile([128, n], "ren"); ev, b_ev = tile([128, n], "rev")
        t1, b_t1 = tile([128, n], "rt1"); t2, b_t2 = tile([128, n], "rt2")
        oA, b_oA = tile([128, n], "roA"); oB, b_oB = tile([128, n], "roB"); oK, b_oK = tile([128, n], "roK"); oR, b_oR = tile([128, n], "roR")
        gcs, b_gcs = tile([128, n // 64], "rgcs"); bns, b_bns = tile([128, nsl, 2], "rbns")
        tmo = [tile([128, 3, 128], "rtmo") for _ in range(2)]
        gto = [tile([128, 512], "rgto") for _ in range(2)]
        pM = [k.ps(st, [128, 512], F32, "rpM") for _ in range(3)]; bpM = [P.buf() for _ in range(3)]
        pT = [k.ps(st, [128, 3, 128], F32, "rpT") for _ in range(2)]; bpT = [P.buf() for _ in range(2)]
        pB = k.ps(st, [128, 8], F32, "rpB"); b_pB = P.buf()
        nraw = [0]

        def load_mixed(row0, mu_col, dst, b_dst, t0, parts=slice(0, 128)):
            (rw_, b_rw) = raw[nraw[0] % 4]; nraw[0] += 1
            c0 = t_base + t0
            if t0 == 0:
                P.op("pool", lambda e, rw_=rw_: e.memset(rw_[:, 0:1], 0.0), writes=[b_rw])
                P.dma("sp", lambda e, rw_=rw_, c0=c0: e.dma_start(out=rw_[:, 1:1 + n], in_=PT[row0:row0 + 128, c0:c0 + n]), writes=[b_rw])
            else:
                P.dma("sp", lambda e, rw_=rw_, c0=c0: e.dma_start(out=rw_[:, 0:1 + n], in_=PT[row0:row0 + 128, c0 - 1:c0 + n]), writes=[b_rw])
            P.op("dve", lambda e, rw_=rw_: e.tensor_scalar(out=dst[:], in0=rw_[:, 1:1 + n], scalar1=omk[:, mu_col:mu_col + 1], scalar2=None, op0=ALU.mult),
                 reads=[b_rw, b_omk], writes=[b_dst])
            P.op("dve", lambda e, rw_=rw_: e.scalar_tensor_tensor(out=dst[:], in0=rw_[:, 0:n], scalar=prm[:, MU + mu_col:MU + mu_col + 1], in1=dst[:],
                                                                 op0=ALU.mult, op1=ALU.add), reads=[b_rw, b_prm, b_dst], writes=[b_dst])

        ntm = ngt = 0
        for t0 in range(0, S, n):
            load_mixed(6144, 48, la, b_la, t0)
            P.op("act", lambda e: e.activation(out=la[0:64, :], in_=la[0:64, :], func=AF.Tanh), reads=[b_la], writes=[b_la])
            load_mixed(6272, 49, sg, b_sg, t0)
            P.op("act", lambda e: e.activation(out=sg[:], in_=sg[:], func=AF.Sigmoid), reads=[b_sg], writes=[b_sg])
            for sl_ in range(nsl):
                for cbk in range(4):
                    i = ngt % 2; ngt += 1
                    (go, b_go) = gto[i]
                    pi = 2
                    P.op("pe", lambda e, sl_=sl_, cbk=cbk: e.matmul(pM[2][:], lhsT=sg[:, sl_ * 128:(sl_ + 1) * 128], rhs=g2s[:, cbk * 512:(cbk + 1) * 512],
                                                                   start=True, stop=True), reads=[b_sg, b_g2], writes=[bpM[2]], skip_same=True)
                    P.op("act", lambda e, go=go: e.copy(out=go[:], in_=pM[2][:]), reads=[bpM[2]], writes=[b_go])
                    tr = t_base * 0 + t0 + sl_ * 128
                    P.dma("sp", lambda e, go=go, tr=tr, cbk=cbk: e.dma_start(out=Gt[tr:tr + 128, cbk * 512:(cbk + 1) * 512], in_=go[:]), reads=[b_go])
            for c in range(16):
                load_mixed(c * 128, c, rm, b_r, t0)
                load_mixed(RW_W + c * 128, 16 + c, km, b_k, t0)
                load_mixed(2 * RW_W + c * 128, 32 + c, vm, b_v, t0)
                fc = slice(c * 128, (c + 1) * 128)
                P.op("pe", lambda e, fc=fc: e.matmul(pM[0][:, 0:n], lhsT=wl[0:64, fc], rhs=la[0:64, :], start=True, stop=True),
                     reads=[b_wl, b_la], writes=[bpM[0]], skip_same=True)
                P.op("act", lambda e, c=c: e.activation(out=lw[:], in_=pM[0][:, 0:n], func=AF.Sigmoid, bias=prm[:, W0 + c:W0 + c + 1], scale=1.0),
                     reads=[bpM[0], b_prm], writes=[b_lw])
                P.op("dve", lambda e: e.tensor_scalar(out=lw[:], in0=lw[:], scalar1=-0.6065306597126334, scalar2=None, op0=ALU.mult), reads=[b_lw], writes=[b_lw])
                P.op("pe", lambda e, fc=fc: e.matmul(pM[1][:, 0:n], lhsT=wl[64:128, fc], rhs=la[64:128, :], start=True, stop=True),
                     reads=[b_wl, b_la], writes=[bpM[1]], skip_same=True)
                P.op("act", lambda e, c=c: e.activation(out=aa[:], in_=pM[1][:, 0:n], func=AF.Sigmoid, bias=prm[:, A0 + c:A0 + c + 1], scale=1.0),
                     reads=[bpM[1], b_prm], writes=[b_a])
                P.op("dve", lambda e, c=c: e.tensor_scalar(out=kk[:], in0=km[:], scalar1=prm[:, KK_ + c:KK_ + c + 1], scalar2=None, op0=ALU.mult),
                     reads=[b_k, b_prm], writes=[b_kk])
                P.op("act", lambda e: e.activation(out=t1[:], in_=kk[:], func=AF.Square), reads=[b_kk], writes=[b_t1])
                P.op("pe", lambda e: e.matmul(pM[0][:, 0:n], lhsT=bones[:], rhs=t1[:], start=True, stop=True), reads=[b_bones, b_t1], writes=[bpM[0]], skip_same=True)
                P.op("act", lambda e: e.activation(out=t2[:], in_=pM[0][:, 0:n], func=AF.Sqrt), reads=[bpM[0]], writes=[b_t2])
                P.op("dve", lambda e: e.tensor_scalar(out=t2[:], in0=t2[:], scalar1=1e-12, scalar2=None, op0=ALU.max), reads=[b_t2], writes=[b_t2])
                P.op("dve", lambda e: e.reciprocal(out=t2[:], in_=t2[:]), reads=[b_t2], writes=[b_t2])
                P.op("dve", lambda e: e.tensor_tensor(out=kk[:], in0=kk[:], in1=t2[:], op=ALU.mult), reads=[b_kk, b_t2], writes=[b_kk])
                P.op("dve", lambda e, c=c: e.tensor_scalar(out=kp[:], in0=aa[:], scalar1=prm[:, KA + c:KA + c + 1], scalar2=omk[:, 50 + c:51 + c],
                                                          op0=ALU.mult, op1=ALU.add), reads=[b_a, b_prm, b_omk], writes=[b_kp])
                P.op("dve", lambda e: e.tensor_tensor(out=kp[:], in0=kp[:], in1=km[:], op=ALU.mult), reads=[b_kp, b_k], writes=[b_kp])
                P.op("dve", lambda e: e.tensor_tensor_scan(out=cl[:], data0=rmask[:], data1=lw[:], initial=0.0, op0=ALU.mult, op1=ALU.add),
                     reads=[b_rm, b_lw], writes=[b_cl])
                P.op("act", lambda e: e.activation(out=ep[:], in_=cl[:], func=AF.Exp), reads=[b_cl], writes=[b_ep])
                P.op("act", lambda e: e.activation(out=en[:], in_=cl[:], func=AF.Exp, scale=-1.0), reads=[b_cl], writes=[b_en])
                P.op("pool", lambda e: e.tensor_tensor(out=t1[:], in0=cl[:], in1=lw[:], op=ALU.subtract), reads=[b_cl, b_lw, b_t1], writes=[b_t1])
                P.op("act", lambda e: e.activation(out=ev[:], in_=t1[:], func=AF.Exp), reads=[b_t1], writes=[b_ev])
                P.op("dve", lambda e: e.scalar_tensor_tensor(out=oA[:], in0=kk[:], scalar=-1.0, in1=ev[:], op0=ALU.mult, op1=ALU.mult),
                     reads=[b_kk, b_ev], writes=[b_oA])
                P.op("pool", lambda e: e.tensor_tensor(out=oB[:], in0=kk[:], in1=aa[:], op=ALU.mult), reads=[b_kk, b_a], writes=[b_oB])
                P.op("pool", lambda e: e.tensor_tensor(out=oB[:], in0=oB[:], in1=en[:], op=ALU.mult), reads=[b_oB, b_en], writes=[b_oB])
                P.op("dve", lambda e: e.tensor_tensor(out=oK[:], in0=kp[:], in1=en[:], op=ALU.mult), reads=[b_kp, b_en], writes=[b_oK])
                P.op("pool", lambda e: e.tensor_tensor(out=oR[:], in0=rm[:], in1=ep[:], op=ALU.mult), reads=[b_r, b_ep], writes=[b_oR])
                P.op("dve", lambda e: e.tensor_copy(out=gcs[:], in_=ep[:, 63::64]), reads=[b_ep], writes=[b_gcs])
                for (src, dstD) in ((oA, AT), (oB, BT), (oK, KT_), (oR, RT)):
                    bsrc = {id(oA): b_oA, id(oB): b_oB, id(oK): b_oK, id(oR): b_oR}[id(src)]
                    P.dma("sp", lambda e, src=src, dstD=dstD, fc=fc, t0=t0: e.dma_start(out=dstD[fc, t0:t0 + n], in_=src[:]), reads=[bsrc])
                P.dma("sp", lambda e, fc=fc, t0=t0: e.dma_start(out=GC[fc, t0 // 64:(t0 + n) // 64], in_=gcs[:]), reads=[b_gcs])
                P.op("dve", lambda e, c=c: e.scalar_tensor_tensor(out=t2[:], in0=rm[:], scalar=prm[:, RK + c:RK + c + 1], in1=kp[:], op0=ALU.mult, op1=ALU.mult),
                     reads=[b_r, b_prm, b_kp, b_t2], writes=[b_t2])
                for sl_ in range(nsl):
                    ts_ = slice(sl_ * 128, (sl_ + 1) * 128)
                    P.op("pe", lambda e, ts_=ts_, sl_=sl_: e.matmul(pB[:, sl_ * 2:sl_ * 2 + 2], lhsT=t2[:, ts_], rhs=sel[:], start=True, stop=True),
                         reads=[b_t2, b_sel], writes=[b_pB], skip_same=True)
                P.op("act", lambda e: e.copy(out=bns[:].rearrange("p s h -> p (s h)"), in_=pB[:, 0:2 * nsl]), reads=[b_pB], writes=[b_bns])
                P.dma("sp", lambda e, c=c, t0=t0: e.dma_start(out=bonus[t0:t0 + n, 2 * c:2 * c + 2].rearrange("(s p) h -> p s h", p=128), in_=bns[:]), reads=[b_bns])
                for sl_ in range(nsl):
                    ts_ = slice(sl_ * 128, (sl_ + 1) * 128)
                    i = ntm % 2; ntm += 1
                    (to, b_to) = tmo[i]
                    for q_, (src, bsrc) in enumerate(((vm, b_v), (oB, b_oB), (oK, b_oK))):
                        P.op("pe", lambda e, i=i, q_=q_, src=src, ts_=ts_: e.transpose(out=pT[i][:, q_, :], in_=src[:, ts_], identity=idf[:]),
                             reads=[bsrc, b_idf], writes=[bpT[i]], skip_same=True)
                    P.op("act", lambda e, i=i, to=to: e.copy(out=to[:], in_=pT[i][:]), reads=[bpT[i]], writes=[b_to])
                    tr = t0 + sl_ * 128
                    for q_, dstD in enumerate((Vtm, Btm, Ktm)):
                        P.dma("sp", lambda e, to=to, q_=q_, dstD=dstD, tr=tr, fc=fc: e.dma_start(out=dstD[tr:tr + 128, fc], in_=to[:, q_, :]), reads=[b_to])
        k.end_stage()


def load_bcast(k, st, vec_ap, name):
    P = k.P
    t = k.sb(st, [128, vec_ap.shape[0]], F32, name)
    b = P.buf()
    P.dma("sp", lambda e: e.dma_start(out=t[:], in_=vec_ap.partition_broadcast(128)), writes=[b])
    return t, b


def stage_plain_T(k, st, ident_bf, src, r0, nt, yT):
    P = k.P
    (idb, b_id), (yT_t, b_yT) = ident_bf, yT
    xt = [k.sb(st, [128, D], F32, "tx") for _ in range(2)]; bx = [P.buf() for _ in range(2)]
    xb = k.sb(st, [128, D], BF16, "txb"); b_xb = P.buf()
    pT = [k.ps(st, [128, 8, 128], BF16, "tpT") for _ in range(2)]; bpT = [P.buf() for _ in range(2)]
    for i in range(nt // 128):
        x_t, b_x = xt[i % 2], bx[i % 2]
        rr = r0 + i * 128
        P.dma("sp", lambda e, x_t=x_t, rr=rr: e.dma_start(out=x_t[:], in_=src[rr:rr + 128, :]), writes=[b_x])
        P.op("act", lambda e, x_t=x_t: e.copy(out=xb[:], in_=x_t[:]), reads=[b_x], writes=[b_xb])
        for g in range(KC // 8):
            p_t, b_p = pT[g % 2], bpT[g % 2]
            for j in range(8):
                c = g * 8 + j
                P.op("pe", lambda e, p_t=p_t, j=j, c=c: e.transpose(out=p_t[:, j, :], in_=xb[:, c * 128:(c + 1) * 128], identity=idb[:]),
                     reads=[b_xb, b_id], writes=[b_p], skip_same=True)
            if g % 2 == 0:
                P.op("act", lambda e, p_t=p_t, g=g, i=i: e.copy(out=yT_t[:, g * 8:(g + 1) * 8, i * 128:(i + 1) * 128], in_=p_t[:]), reads=[b_p], writes=[b_yT])
            else:
                P.op("dve", lambda e, p_t=p_t, g=g, i=i: e.tensor_copy(out=yT_t[:, g * 8:(g + 1) * 8, i * 128:(i + 1) * 128], in_=p_t[:]), reads=[b_p], writes=[b_yT])


def load_norm_bcast(k, st, nw, sc, sh):
    P = k.P
    g_t, b_g = load_bcast(k, st, nw, "nbg"); s_t, b_s = load_bcast(k, st, sc, "nbs"); h_t, b_h = load_bcast(k, st, sh, "nbh")
    P.op("dve", lambda e: e.scalar_tensor_tensor(out=g_t[:], in0=s_t[:], scalar=1.0, in1=g_t[:], op0=ALU.add, op1=ALU.mult),
         reads=[b_s, b_g], writes=[b_g])
    return (g_t, b_g), (h_t, b_h)


INPUT_SHAPES = lambda B, T: dict(
    x=[T, D], cT=[128, KC, B], pos=[T, 1], consts=[128, C_W], ada_w=[2, 6 * D // 512, 128, KC, 512], ada_b=[2, 6 * D], norm_mix_w=[2, D], norm_ffn_w=[2, D],
    hyb_w_mla=[3, 128, KC, 512], hyb_w_rw=[RW_IN // 128, 128, KC, 128], mla_q_norm_w=[Q_LORA], mla_w_uq=[Q_LORA, MLA_H * QK_HEAD], mla_kv_norm_w=[KV_LORA],
    mla_w_ukv=[KV_LORA, MLA_H * (QK_NOPE + V_HEAD)], mla_qk_q_w=[QK_HEAD], mla_qk_k_w=[QK_HEAD],
    rwp=[128, 130], rwkv_w2=[64, RW_W], rwkv_a2=[64, RW_W], rwkv_g2=[128, RW_W], rwkv_lnx_w=[RW_W], rwkv_lnx_b=[RW_W], hyb_w_out=[D // 512, 128, KC, 512],
    conv_w_in=[3 * KC, 128, KC, 128], conv_w3=[128, KC, 3], conv_w_out=[D // 512, 128, KC, 512], peer_w_q=[2, 16, 128, KC, 128], keysT=[2, 128, 16, 128], peer_u=[2, NE, D], peer_v=[2, NE, D])


def build(B, S):
    T = B * S
    nc = bass.Bass("TRN2", target_bir_lowering=False)
    A = {}
    for name, shp in INPUT_SHAPES(B, T).items():
        A[name] = nc.dram_tensor(name, list(shp), mybir.dt.int32 if name == "pos" else F32, kind="ExternalInput").ap()
    out = nc.dram_tensor("out", [T, D], F32, kind="ExternalOutput").ap()
    NB = min(512, S)
    with ExitStack() as st0:
        P = Prog(nc, st0)
        k = K(nc, P, st0)
        mod = k.dram([2, B, 6 * D], F32, "mod")
        Pm = k.dram([T, MLA_IN], F32, "Pm"); PT = k.dram([RW_IN, T], F32, "PT"); Ycat = k.dram([T, D], F32, "Ycat")
        x1 = k.dram([T, D], F32, "x1"); x2 = k.dram([T, D], F32, "x2"); x3 = k.dram([T, D], F32, "x3")
        QTn = k.dram([16, 128, S], BF16, "QTn"); QTr = k.dram([16, 64, S], BF16, "QTr")
        KTn = k.dram([16, 128, S], BF16, "KTn"); KTr = k.dram([16, 64, S], BF16, "KTr"); Vh = k.dram([16, S, 128], BF16, "Vh")
        rs = {n: k.dram([RW_W, S], F32, "r" + n) for n in ("AT", "BT", "KT", "RT")}
        rs["GC"] = k.dram([RW_W, S // 64], F32, "rGC")
        for n in ("Vtm", "Btm", "Ktm", "Gt"):
            rs[n] = k.dram([S, RW_W], F32, "r" + n)
        rs["bonus"] = k.dram([S, 32], F32, "rbonus")
        UT = k.dram([NE // 512, 128, KC, 512], BF16, "UT"); GAT = k.dram([NCH, 128, T], BF16, "GAT")
        with ExitStack() as keep0:
            idb = k.sb(keep0, [128, 128], BF16, "idb"); b_idb = P.buf()
            idf = k.sb(keep0, [128, 128], F32, "idf"); b_idf = P.buf()
            ID, IDF = (idb, b_idb), (idf, b_idf)
            with ExitStack() as st:
                P.dma("sp", lambda e: e.dma_start(out=idf[:], in_=A["consts"][:, 0:128]), writes=[b_idf])
                P.op("dve", lambda e: e.tensor_copy(out=idb[:], in_=idf[:]), reads=[b_idf], writes=[b_idb])
                k.end_stage()
            for l in range(2):
                with ExitStack() as st:
                    stage_ada(k, st, A["cT"], A["ada_w"][l], A["ada_b"][l], mod[l], 6 * D, B)
                    k.end_stage()
            mv = lambda l, b, i: mod[l, b, i * D:(i + 1) * D]
            for b in range(B):
                for t0 in range(0, S, NB):
                    row0 = b * S + t0
                    with ExitStack() as keep:
                        hT = k.sb(keep, [128, KC, NB], BF16, "ihT"); b_hT = P.buf()
                        with ExitStack() as st:
                            gb_, sb_ = load_norm_bcast(k, st, A["norm_mix_w"][0], mv(0, b, 1), mv(0, b, 0))
                            stage_norm_T(k, st, ID, A["x"], row0, NB, gb_, sb_, (hT, b_hT))
                            k.end_stage()
                        with ExitStack() as st:
                            ot = [k.sb(st, [128, 512], F32, "iot") for _ in range(2)]; bo = [P.buf() for _ in range(2)]
                            cnt = [0]

                            def evac(ti, c0, w, p_t, b_p, row0=row0, ot=ot, bo=bo, cnt=cnt):
                                i = cnt[0] % 2; cnt[0] += 1
                                P.op("act", lambda e: e.copy(out=ot[i][:, 0:w], in_=p_t[:, 0:w]), reads=[b_p], writes=[bo[i]])
                                rr = row0 + ti * 128
                                P.dma("sp", lambda e: e.dma_start(out=Pm[rr:rr + 128, c0:c0 + w], in_=ot[i][:, 0:w]), reads=[bo[i]])
                            gemm_tok(k, st, (hT, b_hT), KC, NB, A["hyb_w_mla"], MLA_IN, evac)
                            k.end_stage()
                        with ExitStack() as st:
                            ft = [k.sb(st, [128, 512], F32, "ift") for _ in range(2)]; bf_ = [P.buf() for _ in range(2)]
                            for j in range(RW_IN // 128):
                                def ev(_j, p_t, b_p, j=j, row0=row0, ft=ft, bf_=bf_):
                                    i = j % 2
                                    P.op("act", lambda e: e.copy(out=ft[i][:, 0:NB], in_=p_t[:, 0:NB]), reads=[b_p], writes=[bf_[i]])
                                    P.dma("sp", lambda e: e.dma_start(out=PT[j * 128:(j + 1) * 128, row0:row0 + NB], in_=ft[i][:, 0:NB]), reads=[bf_[i]])
                                gemm_feat_one(k, st, (hT, b_hT), NB, A["hyb_w_rw"][j], ev)
                            k.end_stage()
            for b in range(B):
                mla_prep(k, ID, A["consts"], Pm, 0, A["pos"], S, b * S, A["mla_q_norm_w"], A["mla_w_uq"], A["mla_kv_norm_w"], A["mla_w_ukv"],
                         A["mla_qk_q_w"], A["mla_qk_k_w"], QTn, QTr, KTn, KTr, Vh)
                mla_attn(k, A["consts"], A["mla_qk_q_w"], A["mla_qk_k_w"], QTn, QTr, KTn, KTr, Vh, S, Ycat, b * S, 0)
                rwkv_prep(k, A["consts"], PT, b * S, S, A["rwp"], A["rwkv_w2"], A["rwkv_a2"], A["rwkv_g2"], rs["AT"], rs["BT"], rs["KT"], rs["RT"], rs["GC"],
                          rs["Vtm"], rs["Btm"], rs["Ktm"], rs["bonus"], rs["Gt"], TB=NB)
                rwkv_scan(k, A["consts"], rs["AT"], rs["BT"], rs["KT"], rs["RT"], rs["GC"], rs["Vtm"], rs["Btm"], rs["Ktm"], rs["bonus"], rs["Gt"],
                          A["rwkv_lnx_w"], A["rwkv_lnx_b"], S, Ycat, b * S, MLA_H * V_HEAD, HB=4, CB=min(8, S // 64))
            for b in range(B):
                for t0 in range(0, S, NB):
                    row0 = b * S + t0
                    with ExitStack() as keep:
                        yT = k.sb(keep, [128, KC, NB], BF16, "oyT"); b_yT = P.buf()
                        with ExitStack() as st:
                            stage_plain_T(k, st, ID, Ycat, row0, NB, (yT, b_yT))
                            k.end_stage()
                        with ExitStack() as st:
                            gate_b = load_bcast(k, st, mv(0, b, 2), "ogt")
                            evac = make_residual_evac(k, st, A["x"], x1, row0, gate_b, NB)
                            gemm_tok(k, st, (yT, b_yT), KC, NB, A["hyb_w_out"], D, evac)
                            k.end_stage()
            for b in range(B):
                rsl = slice(b * S, (b + 1) * S)
                peer_layer(k, ID, IDF, x1[rsl, :], x2[rsl, :], S, 2, A["norm_ffn_w"][0], mv(0, b, 4), mv(0, b, 3), mv(0, b, 5),
                           A["peer_w_q"][0], A["keysT"][0], A["peer_u"][0], A["peer_v"][0], UT, GAT[:, :, rsl], do_prep=(b == 0))
            NTc = 256
            for b in range(B):
                with ExitStack() as keepb:
                    hp = k.sb(keepb, [128, KC, 2], BF16, "chp"); b_hp = P.buf()
                    for t0 in range(0, S, NTc):
                        row0 = b * S + t0
                        with ExitStack() as keep:
                            hT = k.sb(keep, [128, KC, 2 + NTc], BF16, "chT"); b_hT = P.buf()
                            gT = k.sb(keep, [128, KC, NTc], BF16, "cgT"); b_gT = P.buf()
                            with ExitStack() as st:
                                gb_, sb_ = load_norm_bcast(k, st, A["norm_mix_w"][1], mv(1, b, 1), mv(1, b, 0))
                                if t0 == 0:
                                    P.op("pool", lambda e, hT=hT: e.memset(hT[:, :, 0:2], 0.0), writes=[b_hT])
                                else:
                                    P.op("pool", lambda e, hT=hT: e.tensor_copy(out=hT[:, :, 0:2], in_=hp[:]), reads=[b_hp], writes=[b_hT])
                                stage_norm_T(k, st, ID, x2, row0, NTc, gb_, sb_, (hT[:, :, 2:2 + NTc], b_hT))
                                P.op("pool", lambda e, hT=hT: e.tensor_copy(out=hp[:], in_=hT[:, :, NTc:NTc + 2]), reads=[b_hT], writes=[b_hp])
                                k.end_stage()
                            with ExitStack() as st:
                                stage_conv_core(k, st, (hT, b_hT), NTc, 2, A["conv_w_in"], A["conv_w3"], KC, (gT, b_gT))
                                k.end_stage()
                            with ExitStack() as st:
                                gate_b = load_bcast(k, st, mv(1, b, 2), "cgt")
                                evac = make_residual_evac(k, st, x2, x3, row0, gate_b, NTc)
                                gemm_tok(k, st, (gT, b_gT), KC, NTc, A["conv_w_out"], D, evac)
                                k.end_stage()
                    k.end_stage()
            for b in range(B):
                rsl = slice(b * S, (b + 1) * S)
                peer_layer(k, ID, IDF, x3[rsl, :], out[rsl, :], S, 2, A["norm_ffn_w"][1], mv(1, b, 4), mv(1, b, 3), mv(1, b, 5),
                           A["peer_w_q"][1], A["keysT"][1], A["peer_u"][1], A["peer_v"][1], UT, GAT[:, :, rsl], do_prep=(b == 0))
            k.end_stage()
    return nc


def make_consts():
    cv = np.zeros((128, C_W), np.float32)
    cv[:, 0:128] = np.eye(128)
    cv[:, 128:256] = (np.arange(128)[:, None] <= np.arange(128)[None, :])
    cv[:, 256:288] = (10000.0 ** (-np.arange(32, dtype=np.float32) / 32)).astype(np.float32)[None, :]
    cv[:64, 288:352] = (np.arange(64)[:, None] < np.arange(64)[None, :])
    cv[:64, 352:416] = (np.arange(64)[:, None] > np.arange(64)[None, :])
    cv[:64, C_BONES:C_BONES + 64] = 1
    cv[64:, C_BONES + 64:C_BONES + 128] = 1
    cv[:64, C_SEL] = 1
    cv[64:, C_SEL + 1] = 1
    cv[:, C_RMASK:C_RMASK + 512] = 1
    cv[:, C_RMASK:C_RMASK + 512:64] = 0
    return cv


def host_layout(inp):
    f = lambda a: np.ascontiguousarray(np.asarray(a), dtype=np.float32)
    B, S, _ = inp["x"].shape
    pc = lambda v: np.asarray(v, dtype=np.float32).reshape(-1, 128).T

    def blk(W, width):
        W = np.asarray(W, dtype=np.float32)
        K_, n_ = W.shape
        nb_ = -(-n_ // width)
        if nb_ * width != n_:
            W = np.concatenate([W, np.zeros((K_, nb_ * width - n_), np.float32)], 1)
        return np.ascontiguousarray(W.reshape(K_ // 128, 128, nb_, width).transpose(2, 1, 0, 3))

    m = dict(
        x=f(inp["x"]).reshape(B * S, D), cT=f(np.asarray(inp["c"]).reshape(B, KC, 128).transpose(2, 1, 0)),
        pos=np.ascontiguousarray(np.asarray(inp["positions"]).reshape(B * S, 1).astype(np.int32)), consts=make_consts(),
        ada_w=np.stack([blk(inp["ada_w"][l], 512) for l in range(2)]), ada_b=f(inp["ada_b"]), norm_mix_w=f(inp["norm_mix_w"]), norm_ffn_w=f(inp["norm_ffn_w"]),
        hyb_w_mla=blk(np.asarray(inp["hyb_w_in"][0])[:, :MLA_IN], 512), hyb_w_rw=blk(np.asarray(inp["hyb_w_in"][0])[:, MLA_IN:], 128), mla_q_norm_w=f(inp["mla_q_norm_w"][0]), mla_w_uq=f(inp["mla_w_uq"][0]),
        mla_kv_norm_w=f(inp["mla_kv_norm_w"][0]), mla_w_ukv=f(inp["mla_w_ukv"][0]), mla_qk_q_w=f(inp["mla_qk_q_w"][0]),
        mla_qk_k_w=f(inp["mla_qk_k_w"][0]),
        rwp=f(np.concatenate([pc(inp["rwkv_mu"][0]), pc(inp["rwkv_w0"][0]), pc(inp["rwkv_a0"][0]), pc(inp["rwkv_k_k"][0]), pc(inp["rwkv_k_a"][0]),
                              pc(np.asarray(inp["rwkv_r_k"][0]).reshape(-1))], 1)),
        rwkv_w2=f(inp["rwkv_w2"][0]), rwkv_a2=f(inp["rwkv_a2"][0]), rwkv_g2=f(inp["rwkv_g2"][0]), rwkv_lnx_w=f(inp["rwkv_lnx_w"][0]),
        rwkv_lnx_b=f(inp["rwkv_lnx_b"][0]), hyb_w_out=blk(inp["hyb_w_out"][0], 512),
        conv_w_in=blk(inp["conv_w_in"][0], 128), conv_w3=f(np.asarray(inp["conv_w"][0]).reshape(3, KC, 128).transpose(2, 1, 0)), conv_w_out=blk(inp["conv_w_out"][0], 512),
        peer_w_q=np.stack([blk(inp["peer_w_q"][l], 128) for l in range(2)]), keysT=f(np.asarray(inp["peer_keys"]).reshape(2, 16, 128, 128).transpose(0, 3, 1, 2)),
        peer_u=f(inp["peer_u"]), peer_v=f(inp["peer_v"]))
    return m, B, S


def kernel(**inputs):
    m, B, S = host_layout(inputs)
    nc = build(B, S)
    res = run_bass_kernel_spmd(nc, [m], core_ids=[0])
    return np.asarray(res.results[0]["out"], dtype=np.float32).reshape(B, S, D)
```

```python
import numpy as np
from contextlib import ExitStack
import concourse.bass as bass
import concourse.mybir as mybir
from concourse.bass_utils import run_bass_kernel_spmd

F32 = mybir.dt.float32
BF16 = mybir.dt.bfloat16
AF = mybir.ActivationFunctionType
ALU = mybir.AluOpType
AX = mybir.AxisListType

D = 4096
KC = D // 128
COMPUTE = ("pe", "act", "dve", "pool")
SEM_ROT = 60000
DMA_RING = {"sp": 16, "pool": 8}


class Buf:
    __slots__ = ("name", "w", "r")

    def __init__(self, name=""):
        self.name, self.w, self.r = name, None, []


class Prog:
    def __init__(self, nc, stack):
        self.nc, self.stack = nc, stack
        self.ops = {e: [] for e in ("pe", "act", "dve", "pool", "sp")}
        self.sem, self.cnt, self.nsem = {}, {}, 0
        for e in COMPUTE:
            self._new_sem(e)
        self.ring = {q: [self._alloc("dq%s%d" % (q, i)) for i in range(k)] for q, k in DMA_RING.items()}
        self.ring_n = {q: 0 for q in DMA_RING}
        self.waited, self.live = {}, []

    def _alloc(self, name):
        self.nsem += 1
        return self.stack.enter_context(self.nc.semaphore("%s_%d" % (name, self.nsem)))

    def _new_sem(self, e):
        self.sem[e], self.cnt[e] = self._alloc("c" + e), 0

    def buf(self, name=""):
        b = Buf(name)
        self.live.append(b)
        return b

    def _need(self, eng, ev, skip_same=False):
        if ev is None:
            return None
        sem, val, src = ev
        if skip_same and src == eng:
            return None
        key = (eng, id(sem))
        if self.waited.get(key, 0) >= val:
            return None
        self.waited[key] = val
        return (sem, val)

    def _deps(self, eng, reads, writes, skip_same):
        waits = []
        for b in reads:
            w = self._need(eng, b.w, skip_same)
            if w:
                waits.append(w)
        for b in writes:
            for ev in [b.w] + b.r:
                w = self._need(eng, ev, skip_same)
                if w:
                    waits.append(w)
        return waits

    def _commit(self, ev, reads, writes):
        for b in reads:
            b.r.append(ev)
            if len(b.r) > 48:
                last = {}
                for e in b.r:
                    if id(e[0]) not in last or last[id(e[0])][1] < e[1]:
                        last[id(e[0])] = e
                b.r = list(last.values())
        for b in writes:
            b.w, b.r = ev, []

    def op(self, eng, fn, reads=(), writes=(), skip_same=False):
        waits = self._deps(eng, reads, writes, skip_same)
        if self.cnt[eng] >= SEM_ROT:
            self._new_sem(eng)
        self.cnt[eng] += 1
        sem, val = self.sem[eng], self.cnt[eng]
        self.ops[eng].append((waits, fn, sem, 1))
        self._commit((sem, val, eng), reads, writes)

    def dma(self, q, fn, reads=(), writes=()):
        waits = self._deps(q, reads, writes, False)
        n, k = self.ring_n[q], len(self.ring[q])
        sem = self.ring[q][n % k]
        if n >= k:
            w = self._need(q, (sem, 16 * (n // k), "dma"))
            if w:
                waits.append(w)
        self.ring_n[q] = n + 1
        self.ops[q].append((waits, fn, sem, 16))
        self._commit((sem, 16 * (n // k + 1), "dma"), reads, writes)

    def barrier(self):
        for eng in ("pe", "act", "dve", "pool", "sp"):
            waits = []
            for b in self.live:
                for ev in [b.w] + b.r:
                    w = self._need(eng, ev)
                    if w:
                        waits.append(w)
            self.ops[eng].append((waits, None, None, 0))
        self.live = []

    def emit(self):
        ops = self.ops

        def run(e, lst):
            for waits, fn, sem, inc in lst:
                for (s, v) in waits:
                    e.wait_ge(s, v)
                if fn is not None:
                    fn(e).then_inc(sem, inc)

        with self.nc.Block() as block:
            block.tensor(lambda e: run(e, ops["pe"]))
            block.scalar(lambda e: run(e, ops["act"]))
            block.vector(lambda e: run(e, ops["dve"]))
            block.gpsimd(lambda e: run(e, ops["pool"]))
            block.sync(lambda e: run(e, ops["sp"]))
        self.ops = {e: [] for e in ops}


_GF = {}


class K:
    def __init__(self, nc, P, st):
        self.nc, self.P, self.st = nc, P, st
        self.n = 0

    def sb(self, st, shape, dt, name="t"):
        self.n += 1
        return st.enter_context(self.nc.sbuf_tensor("%s%d" % (name, self.n), list(shape), dt))

    def ps(self, st, shape, dt, name="p"):
        self.n += 1
        return st.enter_context(self.nc.psum_tensor("%s%d" % (name, self.n), list(shape), dt))

    def dram(self, shape, dt, name="scr"):
        self.n += 1
        return self.nc.dram_tensor("%s%d" % (name, self.n), list(shape), dt, kind="Internal").ap()

    def end_stage(self):
        self.P.barrier()
        self.P.emit()
        _GF.clear()


def stage_norm_T(k, st, ident_bf, x_dram, t0, nt, nw_b, sh_b, hT, hT32=None, ident_f=None):
    P, nc = k.P, k.nc
    (idb, b_id), (g_t, b_g), (s_t, b_s), (hT_t, b_hT) = ident_bf, nw_b, sh_b, hT
    if True:
        xt = [k.sb(st, [128, D], F32, "nx") for _ in range(2)]
        bx = [P.buf() for _ in range(2)]
        sq = k.sb(st, [128, D], F32, "nsq"); b_sq = P.buf()
        hb = k.sb(st, [128, D], BF16, "nhb"); b_hb = P.buf()
        ss = k.sb(st, [128, 4], F32, "nss"); b_ss = P.buf()
        pT = [k.ps(st, [128, 8, 128], BF16, "npT") for _ in range(2)]
        bpT = [P.buf() for _ in range(2)]
        if hT32 is not None:
            pT32 = [k.ps(st, [128, 4, 128], F32, "npT32") for _ in range(2)]
            bpT32 = [P.buf() for _ in range(2)]
        for i in range(nt // 128):
            x_t, b_x = xt[i % 2], bx[i % 2]
            r0 = t0 + i * 128
            P.dma("sp", lambda e, x_t=x_t, r0=r0: e.dma_start(out=x_t[:], in_=x_dram[r0:r0 + 128, :]), writes=[b_x])
            P.op("dve", lambda e: e.memset(ss[:], 0.0), writes=[b_ss])
            P.op("act", lambda e, x_t=x_t: e.activation(out=sq[:], in_=x_t[:], func=AF.Square, accum_out=ss[:, 0:1]),
                 reads=[b_x], writes=[b_sq, b_ss])
            P.op("dve", lambda e: e.tensor_scalar(out=ss[:, 1:2], in0=ss[:, 0:1], scalar1=1.0 / D, scalar2=1e-6,
                                                  op0=ALU.mult, op1=ALU.add), reads=[b_ss], writes=[b_ss])
            P.op("act", lambda e: e.activation(out=ss[:, 1:2], in_=ss[:, 1:2], func=AF.Sqrt), reads=[b_ss], writes=[b_ss])
            P.op("dve", lambda e: e.reciprocal(out=ss[:, 1:2], in_=ss[:, 1:2]), reads=[b_ss], writes=[b_ss])
            P.op("dve", lambda e, x_t=x_t: e.scalar_tensor_tensor(out=sq[:], in0=x_t[:], scalar=ss[:, 1:2], in1=g_t[:],
                                                                 op0=ALU.mult, op1=ALU.mult),
                 reads=[b_x, b_ss, b_g], writes=[b_sq])
            if hT32 is None:
                P.op("dve", lambda e: e.tensor_tensor(out=hb[:], in0=sq[:], in1=s_t[:], op=ALU.add),
                     reads=[b_sq, b_s], writes=[b_hb])
            else:
                P.op("dve", lambda e: e.tensor_tensor(out=sq[:], in0=sq[:], in1=s_t[:], op=ALU.add),
                     reads=[b_sq, b_s], writes=[b_sq])
                P.op("act", lambda e: e.copy(out=hb[:], in_=sq[:]), reads=[b_sq], writes=[b_hb])
                (h32_t, b_h32), (idf_t, b_idf) = hT32, ident_f
                for g in range(KC // 4):
                    p_t, b_p = pT32[g % 2], bpT32[g % 2]
                    for j in range(4):
                        c = g * 4 + j
                        P.op("pe", lambda e, p_t=p_t, j=j, c=c: e.transpose(out=p_t[:, j, :], in_=sq[:, c * 128:(c + 1) * 128],
                                                                             identity=idf_t[:]),
                             reads=[b_sq, b_idf], writes=[b_p], skip_same=True)
                    if g % 2 == 0:
                        P.op("act", lambda e, p_t=p_t, g=g, i=i: e.copy(out=h32_t[:, g * 4:(g + 1) * 4, i * 128:(i + 1) * 128], in_=p_t[:]),
                             reads=[b_p], writes=[b_h32])
                    else:
                        P.op("dve", lambda e, p_t=p_t, g=g, i=i: e.tensor_copy(out=h32_t[:, g * 4:(g + 1) * 4, i * 128:(i + 1) * 128], in_=p_t[:]),
                             reads=[b_p], writes=[b_h32])
            for g in range(KC // 8):
                p_t, b_p = pT[g % 2], bpT[g % 2]
                for j in range(8):
                    c = g * 8 + j
                    P.op("pe", lambda e, p_t=p_t, j=j, c=c: e.transpose(out=p_t[:, j, :], in_=hb[:, c * 128:(c + 1) * 128],
                                                                         identity=idb[:]),
                         reads=[b_hb, b_id], writes=[b_p], skip_same=True)
                eng = "act" if g % 2 == 0 else "dve"
                if eng == "act":
                    P.op("act", lambda e, p_t=p_t, g=g, i=i: e.copy(out=hT_t[:, g * 8:(g + 1) * 8, i * 128:(i + 1) * 128], in_=p_t[:]),
                         reads=[b_p], writes=[b_hT])
                else:
                    P.op("dve", lambda e, p_t=p_t, g=g, i=i: e.tensor_copy(out=hT_t[:, g * 8:(g + 1) * 8, i * 128:(i + 1) * 128], in_=p_t[:]),
                         reads=[b_p], writes=[b_hT])


def gemm_tok(k, st, xT, kc_n, nt, W, n_cols, evac):
    P = k.P
    xT_t, b_xT = xT
    if True:
        wt = [k.sb(st, [128, kc_n, 512], BF16, "gw") for _ in range(2)]
        bw = [P.buf() for _ in range(2)]
        pp = [k.ps(st, [128, 512], F32, "gp") for _ in range(2)]
        bp = [P.buf() for _ in range(2)]
        it = 0
        for bi, c0 in enumerate(range(0, n_cols, 512)):
            w = min(512, n_cols - c0)
            w_t, b_w = wt[bi % 2], bw[bi % 2]
            P.dma("pool", lambda e, w_t=w_t, bi=bi: e.dma_start(out=w_t[:], in_=W[bi]), writes=[b_w])
            for ti in range(nt // 128):
                p_t, b_p = pp[it % 2], bp[it % 2]
                it += 1
                for c in range(kc_n):
                    P.op("pe", lambda e, p_t=p_t, w_t=w_t, c=c, ti=ti, w=w: e.matmul(
                        p_t[:, 0:w], lhsT=xT_t[:, c, ti * 128:(ti + 1) * 128], rhs=w_t[:, c, 0:w],
                        start=(c == 0), stop=(c == kc_n - 1)), reads=[b_xT, b_w], writes=[b_p], skip_same=True)
                evac(ti, c0, w, p_t, b_p)


def stage_ada(k, st, cT_dram, ada_w, ada_b, mod_out, n_cols, Bn):
    P = k.P
    cf = k.sb(st, [128, KC, Bn], F32, "acf"); b_cf = P.buf()
    cb = k.sb(st, [128, KC, Bn], BF16, "acb"); b_cb = P.buf()
    P.dma("sp", lambda e: e.dma_start(out=cf[:], in_=cT_dram[:, :, :]), writes=[b_cf])
    P.op("act", lambda e: e.activation(out=cb[:], in_=cf[:], func=AF.Silu), reads=[b_cf], writes=[b_cb])
    wt = [k.sb(st, [128, KC, 512], BF16, "aw") for _ in range(2)]
    bw = [P.buf() for _ in range(2)]
    bt = [k.sb(st, [Bn, 512], F32, "ab") for _ in range(2)]
    bb = [P.buf() for _ in range(2)]
    ot = [k.sb(st, [Bn, 512], F32, "ao") for _ in range(2)]
    bo = [P.buf() for _ in range(2)]
    pp = [k.ps(st, [Bn, 512], F32, "ap") for _ in range(2)]
    bp = [P.buf() for _ in range(2)]
    for bi, c0 in enumerate(range(0, n_cols, 512)):
        w = min(512, n_cols - c0)
        j = bi % 2
        P.dma("pool", lambda e, j=j, bi=bi: e.dma_start(out=wt[j][:], in_=ada_w[bi]), writes=[bw[j]])
        P.dma("sp", lambda e, j=j, c0=c0, w=w: e.dma_start(
            out=bt[j][:, 0:w], in_=ada_b[c0:c0 + w].partition_broadcast(Bn)), writes=[bb[j]])
        for c in range(KC):
            P.op("pe", lambda e, j=j, c=c, w=w: e.matmul(pp[j][:, 0:w], lhsT=cb[:, c, :], rhs=wt[j][:, c, 0:w],
                                                        start=(c == 0), stop=(c == KC - 1)),
                 reads=[b_cb, bw[j]], writes=[bp[j]], skip_same=True)
        P.op("dve", lambda e, j=j, w=w: e.tensor_tensor(out=ot[j][:, 0:w], in0=pp[j][:, 0:w], in1=bt[j][:, 0:w], op=ALU.add),
             reads=[bp[j], bb[j]], writes=[bo[j]])
        P.dma("sp", lambda e, j=j, c0=c0, w=w: e.dma_start(out=mod_out[:, c0:c0 + w], in_=ot[j][:, 0:w]), reads=[bo[j]])


def gemm_feat(k, st, xT, kc_n, col_lo, col_n, W, f0, nf, evac):
    P = k.P
    xT_t, b_xT = xT
    wt = [k.sb(st, [128, kc_n, 128], BF16, "fw") for _ in range(2)]
    bw = [P.buf() for _ in range(2)]
    pp = [k.ps(st, [128, 512], F32, "fp") for _ in range(2)]
    bp = [P.buf() for _ in range(2)]
    for j in range(nf):
        i = j % 2
        c0 = f0 + j * 128
        P.dma("pool", lambda e, i=i, c0=c0: e.dma_start(
            out=wt[i][:], in_=W[:, c0:c0 + 128].rearrange("(c p) n -> p c n", p=128)), writes=[bw[i]])
        for c in range(kc_n):
            P.op("pe", lambda e, i=i, c=c: e.matmul(pp[i][:, 0:col_n], lhsT=wt[i][:, c, :],
                                                   rhs=xT_t[:, c, col_lo:col_lo + col_n],
                                                   start=(c == 0), stop=(c == kc_n - 1)),
                 reads=[b_xT, bw[i]], writes=[bp[i]], skip_same=True)
        evac(j, pp[i], bp[i])


def stage_conv_core(k, st, hT, nt, halo, w_in, conv_w3, nf, gT):
    P = k.P
    assert halo == 2 and halo + nt <= 512
    gT_t, b_gT = gT
    n = halo + nt
    cw = k.sb(st, [128, KC, 3], F32, "ccw"); b_cw = P.buf()
    P.dma("sp", lambda e: e.dma_start(out=cw[:], in_=conv_w3[:, :, :]), writes=[b_cw])
    gb = k.sb(st, [128, 512], F32, "cgb"); b_gb = P.buf()
    gc = k.sb(st, [128, 512], F32, "cgc"); b_gc = P.buf()
    uu = k.sb(st, [128, 512], F32, "cuu"); b_uu = P.buf()
    yy = k.sb(st, [128, 512], F32, "cyy"); b_yy = P.buf()
    for j in range(nf):
        def ev_to(dst, b_dst):
            def ev(_j, p_t, b_p):
                P.op("act", lambda e: e.copy(out=dst[:, 0:n], in_=p_t[:, 0:n]), reads=[b_p], writes=[b_dst])
            return ev
        gemm_feat_one(k, st, hT, n, w_in[0 * KC + j], ev_to(gb, b_gb))
        gemm_feat_one(k, st, hT, n, w_in[1 * KC + j], ev_to(gc, b_gc))
        gemm_feat_one(k, st, hT, n, w_in[2 * KC + j], ev_to(uu, b_uu))
        P.op("dve", lambda e: e.tensor_tensor(out=uu[:, 0:n], in0=uu[:, 0:n], in1=gc[:, 0:n], op=ALU.mult),
             reads=[b_uu, b_gc], writes=[b_uu])
        P.op("dve", lambda e, j=j: e.tensor_scalar(out=yy[:, 0:nt], in0=uu[:, 0:nt], scalar1=cw[:, j, 0:1], scalar2=None,
                                                  op0=ALU.mult), reads=[b_uu, b_cw], writes=[b_yy])
        P.op("dve", lambda e, j=j: e.scalar_tensor_tensor(out=yy[:, 0:nt], in0=uu[:, 1:1 + nt], scalar=cw[:, j, 1:2],
                                                         in1=yy[:, 0:nt], op0=ALU.mult, op1=ALU.add),
             reads=[b_uu, b_cw, b_yy], writes=[b_yy])
        P.op("dve", lambda e, j=j: e.scalar_tensor_tensor(out=yy[:, 0:nt], in0=uu[:, 2:2 + nt], scalar=cw[:, j, 2:3],
                                                         in1=yy[:, 0:nt], op0=ALU.mult, op1=ALU.add),
             reads=[b_uu, b_cw, b_yy], writes=[b_yy])
        P.op("dve", lambda e, j=j: e.tensor_tensor(out=gT_t[:, j, 0:nt], in0=yy[:, 0:nt], in1=gb[:, 2:2 + nt], op=ALU.mult),
             reads=[b_yy, b_gb], writes=[b_gT])


def gemm_feat_one(k, st, hT, n, Wc, evac, wdt=BF16):
    P = k.P
    key = (id(st), wdt == BF16)
    if key not in _GF:
        _GF[key] = dict(wt=[k.sb(st, [128, KC, 128], wdt, "fw") for _ in range(2)], bw=[P.buf() for _ in range(2)],
                        pp=[k.ps(st, [128, 512], F32, "fp") for _ in range(2)], bp=[P.buf() for _ in range(2)], n=0)
    g = _GF[key]
    i = g["n"] % 2
    g["n"] += 1
    xT_t, b_xT = hT
    P.dma("pool" if wdt == BF16 else "sp",
          lambda e: e.dma_start(out=g["wt"][i][:], in_=Wc),
          writes=[g["bw"][i]])
    for c in range(KC):
        P.op("pe", lambda e, c=c: e.matmul(g["pp"][i][:, 0:n], lhsT=g["wt"][i][:, c, :], rhs=xT_t[:, c, 0:n],
                                          start=(c == 0), stop=(c == KC - 1)),
             reads=[b_xT, g["bw"][i]], writes=[g["bp"][i]], skip_same=True)
    evac(0, g["pp"][i], g["bp"][i])


def peer_route(k, st, S_t, b_S, rt, b_rt, heads=8):
    P = k.P
    sv = k.sb(st, [128, heads, 2, 16], F32, "rsv"); b_sv = P.buf()
    scr = k.sb(st, [128, 128], F32, "rscr"); b_scr = P.buf()
    cand = k.sb(st, [128, 16, 16], F32, "rcand"); b_cand = P.buf()
    cscr = k.sb(st, [128, 256], F32, "rcscr"); b_cscr = P.buf()
    cv = k.sb(st, [128, 16], F32, "rcv"); b_cv = P.buf()
    ex = k.sb(st, [128, 16], F32, "rex"); b_ex = P.buf()
    zz = k.sb(st, [128, 4], F32, "rzz"); b_zz = P.buf()
    for h in range(heads):
        for p in range(2):
            P.op("dve", lambda e, h=h, p=p: e.max(out=sv[:, h, p, 0:8], in_=S_t[:, h, p, :]), reads=[b_S], writes=[b_sv])
            P.op("dve", lambda e, h=h, p=p: e.match_replace(out=scr[:], in_to_replace=sv[:, h, p, 0:8], in_values=S_t[:, h, p, :],
                                                           imm_value=-1e30), reads=[b_S, b_sv], writes=[b_scr])
            P.op("dve", lambda e, h=h, p=p: e.max(out=sv[:, h, p, 8:16], in_=scr[:]), reads=[b_scr], writes=[b_sv])
        for a in range(16):
            P.op("dve", lambda e, h=h, a=a: e.tensor_scalar(out=cand[:, a, :], in0=sv[:, h, 1, :], scalar1=sv[:, h, 0, a:a + 1],
                                                           scalar2=None, op0=ALU.add), reads=[b_sv], writes=[b_cand])
        cflat = cand[:].rearrange("p a b -> p (a b)")
        P.op("dve", lambda e: e.max(out=cv[:, 0:8], in_=cflat), reads=[b_cand], writes=[b_cv])
        P.op("dve", lambda e: e.match_replace(out=cscr[:], in_to_replace=cv[:, 0:8], in_values=cflat, imm_value=-1e30),
             reads=[b_cand, b_cv], writes=[b_cscr])
        P.op("dve", lambda e: e.max(out=cv[:, 8:16], in_=cscr[:]), reads=[b_cscr], writes=[b_cv])
        P.op("dve", lambda e, h=h: e.tensor_copy(out=rt[:, h, 0:1], in_=cv[:, 15:16]), reads=[b_cv], writes=[b_rt])
        P.op("dve", lambda e, h=h: e.tensor_copy(out=rt[:, h, 1:2], in_=cv[:, 0:1]), reads=[b_cv], writes=[b_rt])
        P.op("dve", lambda e: e.tensor_scalar(out=zz[:, 0:1], in0=cv[:, 0:1], scalar1=-1.0, scalar2=None, op0=ALU.mult),
             reads=[b_cv], writes=[b_zz])
        P.op("act", lambda e: e.activation(out=ex[:], in_=cv[:], func=AF.Exp, bias=zz[:, 0:1], scale=1.0),
             reads=[b_cv, b_zz], writes=[b_ex])
        P.op("dve", lambda e, h=h: e.reduce_sum(out=rt[:, h, 2:3], in_=ex[:], axis=AX.X), reads=[b_ex], writes=[b_rt])
    return sv, b_sv


def make_residual_evac(k, st, x_src, x_dst, t0, gate_b, nt):
    P = k.P
    g_t, b_g = gate_b
    xt = [k.sb(st, [128, 512], F32, "rx") for _ in range(3)]
    bx = [P.buf() for _ in range(3)]
    cnt = [0]
    base = [t0]

    def evac(ti, c0, w, p_t, b_p):
        i = cnt[0] % 3
        cnt[0] += 1
        r0 = base[0] + ti * 128
        P.dma("sp", lambda e: e.dma_start(out=xt[i][:, 0:w], in_=x_src[r0:r0 + 128, c0:c0 + w]), writes=[bx[i]])
        P.op("dve", lambda e: e.tensor_tensor(out=p_sb[i][:, 0:w], in0=p_t[:, 0:w], in1=g_t[:, c0:c0 + w], op=ALU.mult),
             reads=[b_p, b_g], writes=[bp[i]])
        P.op("pool", lambda e: e.tensor_tensor(out=xt[i][:, 0:w], in0=xt[i][:, 0:w], in1=p_sb[i][:, 0:w], op=ALU.add),
             reads=[bx[i], bp[i]], writes=[bx[i]])
        P.dma("sp", lambda e: e.dma_start(out=x_dst[r0:r0 + 128, c0:c0 + w], in_=xt[i][:, 0:w]), reads=[bx[i]])

    p_sb = [k.sb(st, [128, 512], F32, "rp") for _ in range(3)]
    bp = [P.buf() for _ in range(3)]
    evac.base = base
    return evac


NE = 16384
NCH = NE // 128


def peer_prep_UT(k, idb_b, U, UT):
    P = k.P
    idb, b_id = idb_b
    with ExitStack() as st:
        ub = [k.sb(st, [128, D], BF16, "uub") for _ in range(2)]; bub = [P.buf() for _ in range(2)]
        ut = [k.sb(st, [128, KC, 512], BF16, "uut") for _ in range(2)]; but = [P.buf() for _ in range(2)]
        pT = [k.ps(st, [128, 8, 128], BF16, "upT") for _ in range(2)]; bpT = [P.buf() for _ in range(2)]
        n = 0
        for eb in range(NE // 512):
            u_t, b_u = ut[eb % 2], but[eb % 2]
            for cc in range(4):
                ch = eb * 4 + cc
                r_t, b_r = ub[ch % 2], bub[ch % 2]
                P.dma("pool", lambda e, r_t=r_t, ch=ch: e.dma_start(out=r_t[:], in_=U[ch * 128:(ch + 1) * 128, :]), writes=[b_r])
                for g in range(KC // 8):
                    p_t, b_p = pT[n % 2], bpT[n % 2]
                    for j in range(8):
                        c = g * 8 + j
                        P.op("pe", lambda e, p_t=p_t, r_t=r_t, j=j, c=c: e.transpose(out=p_t[:, j, :], in_=r_t[:, c * 128:(c + 1) * 128],
                                                                                      identity=idb[:]),
                             reads=[b_r, b_id], writes=[b_p], skip_same=True)
                    if n % 2 == 0:
                        P.op("act", lambda e, p_t=p_t, u_t=u_t, g=g, cc=cc: e.copy(out=u_t[:, g * 8:(g + 1) * 8, cc * 128:(cc + 1) * 128], in_=p_t[:]),
                             reads=[b_p], writes=[b_u])
                    else:
                        P.op("dve", lambda e, p_t=p_t, u_t=u_t, g=g, cc=cc: e.tensor_copy(out=u_t[:, g * 8:(g + 1) * 8, cc * 128:(cc + 1) * 128], in_=p_t[:]),
                             reads=[b_p], writes=[b_u])
                    n += 1
            P.dma("sp", lambda e, u_t=u_t, eb=eb: e.dma_start(out=UT[eb], in_=u_t[:]), reads=[b_u])
        k.end_stage()


def peer_group_scores(k, st, h32_b, n, tg, w_q, kT32_b, S_all_b):
    P = k.P
    kT, b_kT = kT32_b
    S_all, b_S = S_all_b
    qT = k.sb(st, [128, 16, n], F32, "pq"); b_qT = P.buf()
    for hp in range(16):
        def ev(_j, p_t, b_p, hp=hp):
            P.op("act", lambda e: e.copy(out=qT[:, hp, :], in_=p_t[:, 0:n]), reads=[b_p], writes=[b_qT])
        gemm_feat_one(k, st, h32_b, n, w_q[hp], ev, wdt=F32)
    pS = [k.ps(st, [128, 16, 128], F32, "pS") for _ in range(1)]; b_pS = [P.buf()]
    for ti in range(tg):
        for hp in range(16):
            P.op("pe", lambda e, hp=hp, ti=ti: e.matmul(pS[0][:, hp, :], lhsT=qT[:, hp, ti * 128:(ti + 1) * 128], rhs=kT[:, hp, :],
                                                       start=True, stop=True), reads=[b_qT, b_kT], writes=[b_pS[0]], skip_same=True)
        P.op("act", lambda e, ti=ti: e.copy(out=S_all[:, ti, :, :, :].rearrange("p h t k -> p (h t) k"), in_=pS[0][:]),
             reads=[b_pS[0]], writes=[b_S])


def peer_group_A(k, idb_b, hT_b, tg, tok0, S_all_b, UT, GAT):
    P = k.P
    idb, b_id = idb_b
    hT, b_hT = hT_b
    S_all, b_S = S_all_b
    n = tg * 128
    with ExitStack() as keep:
        G = [k.sb(keep, [128, NE], BF16, "pG") for _ in range(tg)]; bG = [P.buf() for _ in range(tg)]
        with ExitStack() as st:
            rt = k.sb(st, [128, 8, 4], F32, "prt"); b_rt = P.buf()
            nb = k.sb(st, [128, 8, 4], F32, "pnb"); b_nb = P.buf()
            ex = [k.sb(st, [128, 16, 128], F32, "pex") for _ in range(2)]; bex = [P.buf() for _ in range(2)]
            acc = [k.sb(st, [128, 16, 128], F32, "pacc") for _ in range(2)]; bacc = [P.buf() for _ in range(2)]
            tmp = [k.sb(st, [128, 16, 128], F32, "ptmp") for _ in range(2)]; btmp = [P.buf() for _ in range(2)]
            for ti in range(tg):
                S_t = S_all[:, ti, :, :, :]
                sv, b_sv = peer_route(k, st, S_t, b_S, rt, b_rt, 8)
                P.op("dve", lambda e: e.tensor_scalar(out=nb[:, :, 0:1], in0=rt[:, :, 1:2], scalar1=-1.0, scalar2=None, op0=ALU.mult),
                     reads=[b_rt], writes=[b_nb])
                P.op("dve", lambda e: e.reciprocal(out=nb[:, :, 2:3], in_=rt[:, :, 2:3]), reads=[b_rt], writes=[b_nb])
                for q in range(8):
                    a_t, b_a, t_t, b_t, x_t, b_x = acc[q % 2], bacc[q % 2], tmp[q % 2], btmp[q % 2], ex[q % 2], bex[q % 2]
                    for h in range(8):
                        s2b = S_t[:, h, 1, :].unsqueeze(1).to_broadcast([128, 16, 128])
                        s1b = S_t[:, h, 0, q * 16:(q + 1) * 16].unsqueeze(2).to_broadcast([128, 16, 128])
                        P.op("pool", lambda e, t_t=t_t, s2b=s2b, s1b=s1b: e.tensor_tensor(out=t_t[:], in0=s2b, in1=s1b, op=ALU.add),
                             reads=[b_S], writes=[b_t])
                        P.op("act", lambda e, t_t=t_t, x_t=x_t, h=h: e.activation(out=x_t[:], in_=t_t[:], func=AF.Exp, bias=nb[:, h, 0:1], scale=1.0),
                             reads=[b_t, b_nb], writes=[b_x])
                        P.op("dve", lambda e, t_t=t_t, x_t=x_t, h=h: e.scalar_tensor_tensor(out=x_t[:], in0=t_t[:], scalar=rt[:, h, 0:1], in1=x_t[:],
                                                                                           op0=ALU.is_ge, op1=ALU.mult),
                             reads=[b_t, b_x, b_rt], writes=[b_x])
                        if h == 0:
                            P.op("dve", lambda e, a_t=a_t, x_t=x_t, h=h: e.tensor_scalar(out=a_t[:], in0=x_t[:], scalar1=nb[:, h, 2:3], scalar2=None,
                                                                                        op0=ALU.mult), reads=[b_x, b_nb], writes=[b_a])
                        else:
                            P.op("dve", lambda e, a_t=a_t, x_t=x_t, h=h: e.scalar_tensor_tensor(out=a_t[:], in0=x_t[:], scalar=nb[:, h, 2:3], in1=a_t[:],
                                                                                               op0=ALU.mult, op1=ALU.add),
                                 reads=[b_x, b_nb, b_a], writes=[b_a])
                    P.op("pool", lambda e, a_t=a_t, ti=ti, q=q: e.tensor_copy(out=G[ti][:, q * 2048:(q + 1) * 2048],
                                                                             in_=a_t[:].rearrange("p a b -> p (a b)")),
                         reads=[b_a], writes=[bG[ti]])
            k.end_stage()
        with ExitStack() as st:
            ut = [k.sb(st, [128, KC, 512], BF16, "aut") for _ in range(2)]; but = [P.buf() for _ in range(2)]
            pA = [k.ps(st, [128, 512], F32, "apA") for _ in range(2)]; bpA = [P.buf() for _ in range(2)]
            pT = [k.ps(st, [128, 4, 128], BF16, "apT") for _ in range(2)]; bpT = [P.buf() for _ in range(2)]
            act = [k.sb(st, [128, 512], F32, "aact") for _ in range(2)]; bact = [P.buf() for _ in range(2)]
            ga = [k.sb(st, [128, 512], BF16, "aga") for _ in range(2)]; bga = [P.buf() for _ in range(2)]
            gaT = [k.sb(st, [128, 4, n], BF16, "agaT") for _ in range(2)]; bgaT = [P.buf() for _ in range(2)]
            it = 0
            pending = [None]

            def make_tail(i, ti, g_t, b_g, eb, last):
                def tail():
                    for cc in range(4):
                        P.op("pe", lambda e, cc=cc: e.transpose(out=pT[i][:, cc, :], in_=ga[i][:, cc * 128:(cc + 1) * 128], identity=idb[:]),
                             reads=[bga[i], b_id], writes=[bpT[i]], skip_same=True)
                    P.op("act", lambda e: e.copy(out=g_t[:, :, ti * 128:(ti + 1) * 128], in_=pT[i][:]), reads=[bpT[i]], writes=[b_g])
                    if last:
                        P.dma("sp", lambda e: e.dma_start(out=GAT[eb * 4:(eb + 1) * 4, :, tok0:tok0 + n].rearrange("c p t -> p c t"), in_=g_t[:]), reads=[b_g])
                return tail

            for eb in range(NE // 512):
                u_t, b_u = ut[eb % 2], but[eb % 2]
                g_t, b_g = gaT[eb % 2], bgaT[eb % 2]
                P.dma("sp", lambda e, u_t=u_t, eb=eb: e.dma_start(out=u_t[:], in_=UT[eb]), writes=[b_u])
                for ti in range(tg):
                    i = it % 2
                    it += 1
                    for c in range(KC):
                        P.op("pe", lambda e, i=i, c=c, ti=ti, u_t=u_t: e.matmul(pA[i][:], lhsT=hT[:, c, ti * 128:(ti + 1) * 128], rhs=u_t[:, c, :],
                                                                              start=(c == 0), stop=(c == KC - 1)),
                             reads=[b_hT, b_u], writes=[bpA[i]], skip_same=True)
                    if pending[0] is not None:
                        pending[0]()
                    P.op("act", lambda e, i=i: e.activation(out=act[i][:], in_=pA[i][:], func=AF.Gelu), reads=[bpA[i]], writes=[bact[i]])
                    P.op("dve", lambda e, i=i, ti=ti, eb=eb: e.tensor_tensor(out=ga[i][:], in0=act[i][:], in1=G[ti][:, eb * 512:(eb + 1) * 512],
                                                                            op=ALU.mult), reads=[bact[i], bG[ti]], writes=[bga[i]])
                    pending[0] = make_tail(i, ti, g_t, b_g, eb, ti == tg - 1)
            pending[0]()
            k.end_stage()


def peer_out(k, V, GAT, T, x_src, x_dst, gate_vec):
    P = k.P
    SB = min(8, T // 128)
    with ExitStack() as st:
        gate_t, b_gate = load_bcast(k, st, gate_vec, "ogate")
        vt = [k.sb(st, [128, 512], BF16, "ovt") for _ in range(4)]; bvt = [P.buf() for _ in range(4)]
        gt = [k.sb(st, [128, SB * 128], BF16, "ogt") for _ in range(4)]; bgt = [P.buf() for _ in range(4)]
        pO = [k.ps(st, [128, 512], F32, "opO") for _ in range(SB)]; bpO = [P.buf() for _ in range(SB)]
        it = 0
        evac = make_residual_evac(k, st, x_src, x_dst, 0, (gate_t, b_gate), SB * 128)
        for s0 in range(0, T, SB * 128):
            evac.base[0] = s0
            for db in range(D // 512):
                for ch in range(NCH):
                    i = it % 4
                    it += 1
                    P.dma("pool", lambda e, i=i, ch=ch, db=db: e.dma_start(out=vt[i][:], in_=V[ch * 128:(ch + 1) * 128, db * 512:(db + 1) * 512]),
                          writes=[bvt[i]])
                    P.dma("sp", lambda e, i=i, ch=ch, s0=s0: e.dma_start(out=gt[i][:], in_=GAT[ch, :, s0:s0 + SB * 128]), writes=[bgt[i]])
                    for ti in range(SB):
                        P.op("pe", lambda e, i=i, ti=ti, ch=ch: e.matmul(pO[ti][:], lhsT=gt[i][:, ti * 128:(ti + 1) * 128], rhs=vt[i][:],
                                                                        start=(ch == 0), stop=(ch == NCH - 1)),
                             reads=[bgt[i], bvt[i]], writes=[bpO[ti]], skip_same=True)
                for ti in range(SB):
                    evac(ti, db * 512, 512, pO[ti], bpO[ti])
        k.end_stage()


def peer_layer(k, idb_b, idf_b, x_src, x_dst, T, tg, nw, sc, sh, gate, w_q, keysT, U, V, UT, GAT, do_prep=True):
    P = k.P
    if do_prep:
        peer_prep_UT(k, idb_b, U, UT)
    n = tg * 128
    with ExitStack() as lay:
        kT = k.sb(lay, [128, 16, 128], F32, "lkT"); b_kT = P.buf()
        P.dma("sp", lambda e: e.dma_start(out=kT[:], in_=keysT[:, :, :]), writes=[b_kT])
        for g0 in range(0, T, n):
            with ExitStack() as keep:
                hT = k.sb(keep, [128, KC, n], BF16, "lhT"); b_hT = P.buf()
                S_all = k.sb(keep, [128, tg, 8, 2, 128], F32, "lS"); b_S = P.buf()
                with ExitStack() as keep2:
                    h32 = k.sb(keep2, [128, KC, n], F32, "lh32"); b_h32 = P.buf()
                    with ExitStack() as st:
                        g_t, b_g = load_bcast(k, st, nw, "lg"); s_t, b_s = load_bcast(k, st, sc, "ls"); h_t, b_h = load_bcast(k, st, sh, "lh")
                        P.op("dve", lambda e: e.scalar_tensor_tensor(out=g_t[:], in0=s_t[:], scalar=1.0, in1=g_t[:], op0=ALU.add, op1=ALU.mult),
                             reads=[b_s, b_g], writes=[b_g])
                        stage_norm_T(k, st, idb_b, x_src, g0, n, (g_t, b_g), (h_t, b_h), (hT, b_hT), hT32=(h32, b_h32), ident_f=idf_b)
                        k.end_stage()
                    with ExitStack() as st:
                        peer_group_scores(k, st, (h32, b_h32), n, tg, w_q, (kT, b_kT), (S_all, b_S))
                        k.end_stage()
                peer_group_A(k, idb_b, (hT, b_hT), tg, g0, (S_all, b_S), UT, GAT)
        k.end_stage()
    peer_out(k, V, GAT, T, x_src, x_dst, gate)


MLA_H, QK_NOPE, QK_ROPE, QK_HEAD, V_HEAD, Q_LORA, KV_LORA = 16, 128, 64, 192, 128, 768, 512
MLA_IN = Q_LORA + KV_LORA + QK_ROPE
PI = 3.141592653589793


def _rstd(P, eng_tile, b, src_col, dst_col, inv_n, eps):
    t = eng_tile
    P.op("dve", lambda e: e.tensor_scalar(out=dst_col, in0=src_col, scalar1=inv_n, scalar2=eps, op0=ALU.mult, op1=ALU.add),
         reads=[b], writes=[b])
    P.op("act", lambda e: e.activation(out=dst_col, in_=dst_col, func=AF.Sqrt), reads=[b], writes=[b])
    P.op("dve", lambda e: e.reciprocal(out=dst_col, in_=dst_col), reads=[b], writes=[b])


def mla_prep(k, idb_b, consts, Pm, col0, pos, S, t_base, q_norm_w, w_uq, kv_norm_w, w_ukv, qk_q_w, qk_k_w, QTn, QTr, KTn, KTr, Vh):
    P = k.P
    idb, b_id = idb_b
    with ExitStack() as st:
        wq = k.sb(st, [128, 6, 3072], BF16, "mwq"); b_wq = P.buf()
        wkv = k.sb(st, [128, 4, 4096], BF16, "mwkv"); b_wkv = P.buf()
        P.dma("pool", lambda e: e.dma_start(out=wq[:], in_=w_uq.rearrange("(c p) n -> p c n", p=128)), writes=[b_wq])
        P.dma("pool", lambda e: e.dma_start(out=wkv[:], in_=w_ukv.rearrange("(c p) n -> p c n", p=128)), writes=[b_wkv])
        qnw, b_qnw = load_bcast(k, st, q_norm_w, "mqnw")
        kvnw, b_kvnw = load_bcast(k, st, kv_norm_w, "mkvnw")
        gq, b_gq = load_bcast(k, st, qk_q_w, "mgq")
        gk, b_gk = load_bcast(k, st, qk_k_w, "mgk")
        fr = k.sb(st, [128, 32], F32, "mfr"); b_fr = P.buf()
        P.dma("sp", lambda e: e.dma_start(out=fr[:], in_=consts[:, 256:288]), writes=[b_fr])
        P.op("dve", lambda e: e.tensor_scalar(out=gq[:], in0=gq[:], scalar1=float(QK_HEAD) ** -0.5, scalar2=None, op0=ALU.mult),
             reads=[b_gq], writes=[b_gq])
        ca = k.sb(st, [128, MLA_IN], F32, "mca"); b_ca = P.buf()
        scr = k.sb(st, [128, 4096], F32, "mscr"); b_scr = P.buf()
        cb = k.sb(st, [128, Q_LORA + KV_LORA], BF16, "mcb"); b_cb = P.buf()
        cT = k.sb(st, [128, 10, 128], BF16, "mcT"); b_cT = P.buf()
        stt = k.sb(st, [128, 64], F32, "mst"); b_st = P.buf()
        pi = k.sb(st, [128, 2], mybir.dt.int32, "mpi"); b_pi = P.buf()
        cs = k.sb(st, [128, 4, 32], F32, "mcs"); b_cs = P.buf()
        ni = k.sb(st, [128, 2, 32], mybir.dt.int32, "mni"); b_ni = P.buf()
        q_sb = k.sb(st, [128, 16, 192], F32, "mq"); b_q = P.buf()
        kv_sb = k.sb(st, [128, 16, 256], F32, "mkv"); b_kv = P.buf()
        kp = k.sb(st, [128, 16, 64], F32, "mkp"); b_kp = P.buf()
        rtmp = k.sb(st, [128, 16, 64], F32, "mrt"); b_rtmp = P.buf()
        qb = k.sb(st, [128, 16, 192], BF16, "mqb"); b_qb = P.buf()
        kb = k.sb(st, [128, 16, 192], BF16, "mkb"); b_kb = P.buf()
        vb = k.sb(st, [128, 16, 128], BF16, "mvb"); b_vb = P.buf()
        oTn = [k.sb(st, [128, 16, 128], BF16, "moTn") for _ in range(2)]; b_oTn = [P.buf() for _ in range(2)]
        oTr = [k.sb(st, [64, 16, 128], BF16, "moTr") for _ in range(2)]; b_oTr = [P.buf() for _ in range(2)]
        pT = [k.ps(st, [128, 8, 128], BF16, "mpT") for _ in range(2)]; bpT = [P.buf() for _ in range(2)]
        pM = [k.ps(st, [128, 512], F32, "mpM") for _ in range(2)]; bpM = [P.buf() for _ in range(2)]
        npt = [0]

        def transpose_to(src_ap_fn, nblk, rows, dst_fn, b_src, b_dst):
            for g0 in range(0, nblk, 8):
                i = npt[0] % 2
                npt[0] += 1
                nb_ = min(8, nblk - g0)
                for j in range(nb_):
                    P.op("pe", lambda e, i=i, j=j, g0=g0: e.transpose(out=pT[i][0:rows, j, :], in_=src_ap_fn(g0 + j), identity=idb[:]),
                         reads=[b_src, b_id], writes=[bpT[i]], skip_same=True)
                if i == 0:
                    P.op("act", lambda e, i=i, g0=g0, nb_=nb_: e.copy(out=dst_fn(g0, nb_), in_=pT[i][0:rows, 0:nb_, :]), reads=[bpT[i]], writes=[b_dst])
                else:
                    P.op("dve", lambda e, i=i, g0=g0, nb_=nb_: e.tensor_copy(out=dst_fn(g0, nb_), in_=pT[i][0:rows, 0:nb_, :]), reads=[bpT[i]], writes=[b_dst])

        for ti in range(S // 128):
            r0 = t_base + ti * 128
            P.dma("sp", lambda e, r0=r0: e.dma_start(out=ca[:], in_=Pm[r0:r0 + 128, col0:col0 + MLA_IN]), writes=[b_ca])
            P.dma("sp", lambda e, r0=r0: e.dma_start(out=pi[:, 0:1], in_=pos[r0:r0 + 128, :]), writes=[b_pi])
            P.op("dve", lambda e: e.memset(stt[:], 0.0), writes=[b_st])
            P.op("act", lambda e: e.activation(out=scr[:, 0:Q_LORA], in_=ca[:, 0:Q_LORA], func=AF.Square, accum_out=stt[:, 0:1]),
                 reads=[b_ca], writes=[b_scr, b_st])
            P.op("act", lambda e: e.activation(out=scr[:, 0:KV_LORA], in_=ca[:, Q_LORA:Q_LORA + KV_LORA], func=AF.Square, accum_out=stt[:, 1:2]),
                 reads=[b_ca], writes=[b_scr, b_st])
            P.op("act", lambda e: e.activation(out=scr[:, 0:QK_ROPE], in_=ca[:, Q_LORA + KV_LORA:MLA_IN], func=AF.Square, accum_out=stt[:, 2:3]),
                 reads=[b_ca], writes=[b_scr, b_st])
            _rstd(P, stt, b_st, stt[:, 0:1], stt[:, 4:5], 1.0 / Q_LORA, 1e-6)
            _rstd(P, stt, b_st, stt[:, 1:2], stt[:, 5:6], 1.0 / KV_LORA, 1e-6)
            P.op("dve", lambda e: e.scalar_tensor_tensor(out=cb[:, 0:Q_LORA], in0=ca[:, 0:Q_LORA], scalar=stt[:, 4:5], in1=qnw[:],
                                                         op0=ALU.mult, op1=ALU.mult), reads=[b_ca, b_st, b_qnw], writes=[b_cb])
            P.op("dve", lambda e: e.scalar_tensor_tensor(out=cb[:, Q_LORA:], in0=ca[:, Q_LORA:Q_LORA + KV_LORA], scalar=stt[:, 5:6], in1=kvnw[:],
                                                         op0=ALU.mult, op1=ALU.mult), reads=[b_ca, b_st, b_kvnw], writes=[b_cb])
            transpose_to(lambda c: cb[:, c * 128:(c + 1) * 128], 10, 128, lambda g0, nb_: cT[:, g0:g0 + nb_, :], b_cb, b_cT)
            nm = 0
            for cbk in range(6):
                i = nm % 2; nm += 1
                for c in range(6):
                    P.op("pe", lambda e, i=i, c=c, cbk=cbk: e.matmul(pM[i][:], lhsT=cT[:, c, :], rhs=wq[:, c, cbk * 512:(cbk + 1) * 512],
                                                                    start=(c == 0), stop=(c == 5)), reads=[b_cT, b_wq], writes=[bpM[i]], skip_same=True)
                P.op("act", lambda e, i=i, cbk=cbk: e.copy(out=q_sb[:].rearrange("p h d -> p (h d)")[:, cbk * 512:(cbk + 1) * 512], in_=pM[i][:]),
                     reads=[bpM[i]], writes=[b_q])
            for cbk in range(8):
                i = nm % 2; nm += 1
                for c in range(4):
                    P.op("pe", lambda e, i=i, c=c, cbk=cbk: e.matmul(pM[i][:], lhsT=cT[:, 6 + c, :], rhs=wkv[:, c, cbk * 512:(cbk + 1) * 512],
                                                                    start=(c == 0), stop=(c == 3)), reads=[b_cT, b_wkv], writes=[bpM[i]], skip_same=True)
                P.op("dve", lambda e, i=i, cbk=cbk: e.tensor_copy(out=kv_sb[:].rearrange("p h d -> p (h d)")[:, cbk * 512:(cbk + 1) * 512], in_=pM[i][:]),
                     reads=[bpM[i]], writes=[b_kv])
            P.op("act", lambda e: e.activation(out=scr[:, 0:3072], in_=q_sb[:].rearrange("p h d -> p (h d)"), func=AF.Square),
                 reads=[b_q], writes=[b_scr])
            P.op("dve", lambda e: e.reduce_sum(out=stt[:, 8:24], in_=scr[:, 0:3072].rearrange("p (h d) -> p h d", d=192), axis=AX.X),
                 reads=[b_scr], writes=[b_st])
            P.op("act", lambda e: e.activation(out=scr[:, 0:2048].rearrange("p (h d) -> p h d", d=128), in_=kv_sb[:, :, 0:128], func=AF.Square),
                 reads=[b_kv], writes=[b_scr])
            P.op("dve", lambda e: e.reduce_sum(out=stt[:, 24:40], in_=scr[:, 0:2048].rearrange("p (h d) -> p h d", d=128), axis=AX.X),
                 reads=[b_scr], writes=[b_st])
            P.op("dve", lambda e: e.tensor_scalar(out=stt[:, 24:40], in0=stt[:, 24:40], scalar1=stt[:, 2:3], scalar2=None, op0=ALU.add),
                 reads=[b_st], writes=[b_st])
            _rstd(P, stt, b_st, stt[:, 8:24], stt[:, 8:24], 1.0 / QK_HEAD, 1e-6)
            _rstd(P, stt, b_st, stt[:, 24:40], stt[:, 24:40], 1.0 / QK_HEAD, 1e-6)
            rq = stt[:, 8:24].unsqueeze(2)
            rk = stt[:, 24:40].unsqueeze(2)
            P.op("dve", lambda e: e.tensor_tensor(out=q_sb[:], in0=q_sb[:], in1=rq.to_broadcast([128, 16, 192]), op=ALU.mult),
                 reads=[b_q, b_st], writes=[b_q])
            P.op("dve", lambda e: e.tensor_tensor(out=q_sb[:], in0=q_sb[:], in1=gq[:].unsqueeze(1).to_broadcast([128, 16, 192]), op=ALU.mult),
                 reads=[b_q, b_gq], writes=[b_q])
            P.op("pool", lambda e: e.tensor_tensor(out=kv_sb[:, :, 0:128], in0=kv_sb[:, :, 0:128], in1=rk.to_broadcast([128, 16, 128]), op=ALU.mult),
                 reads=[b_kv, b_st], writes=[b_kv])
            P.op("pool", lambda e: e.tensor_tensor(out=kv_sb[:, :, 0:128], in0=kv_sb[:, :, 0:128],
                                                   in1=gk[:, 0:128].unsqueeze(1).to_broadcast([128, 16, 128]), op=ALU.mult),
                 reads=[b_kv, b_gk], writes=[b_kv])
            P.op("pool", lambda e: e.tensor_tensor(out=kp[:], in0=ca[:, Q_LORA + KV_LORA:MLA_IN].unsqueeze(1).to_broadcast([128, 16, 64]),
                                                   in1=rk.to_broadcast([128, 16, 64]), op=ALU.mult), reads=[b_ca, b_st], writes=[b_kp])
            P.op("pool", lambda e: e.tensor_tensor(out=kp[:], in0=kp[:], in1=gk[:, 128:192].unsqueeze(1).to_broadcast([128, 16, 64]), op=ALU.mult),
                 reads=[b_kp, b_gk], writes=[b_kp])
            P.op("dve", lambda e: e.tensor_copy(out=stt[:, 40:41], in_=pi[:, 0:1]), reads=[b_pi], writes=[b_st])
            P.op("dve", lambda e: e.tensor_scalar(out=cs[:, 0, :], in0=fr[:], scalar1=stt[:, 40:41], scalar2=None, op0=ALU.mult),
                 reads=[b_fr, b_st], writes=[b_cs])
            P.op("dve", lambda e: e.tensor_scalar(out=cs[:, 1, :], in0=cs[:, 0, :], scalar1=0.5 * PI, scalar2=None, op0=ALU.add),
                 reads=[b_cs], writes=[b_cs])
            ph, wk = cs[:, 0:2, :], cs[:, 2:4, :]
            P.op("dve", lambda e: e.tensor_scalar(out=wk, in0=ph, scalar1=1.0 / (2 * PI), scalar2=None, op0=ALU.mult), reads=[b_cs], writes=[b_cs])
            P.op("dve", lambda e: e.tensor_copy(out=ni[:], in_=wk), reads=[b_cs], writes=[b_ni])
            P.op("dve", lambda e: e.tensor_copy(out=wk, in_=ni[:]), reads=[b_ni], writes=[b_cs])
            P.op("dve", lambda e: e.scalar_tensor_tensor(out=ph, in0=wk, scalar=-2 * PI, in1=ph, op0=ALU.mult, op1=ALU.add), reads=[b_cs], writes=[b_cs])
            P.op("dve", lambda e: e.tensor_scalar(out=wk, in0=ph, scalar1=PI, scalar2=-2 * PI, op0=ALU.is_gt, op1=ALU.mult), reads=[b_cs], writes=[b_cs])
            P.op("dve", lambda e: e.tensor_tensor(out=ph, in0=ph, in1=wk, op=ALU.add), reads=[b_cs], writes=[b_cs])
            P.op("dve", lambda e: e.tensor_scalar(out=wk, in0=ph, scalar1=-PI, scalar2=2 * PI, op0=ALU.is_lt, op1=ALU.mult), reads=[b_cs], writes=[b_cs])
            P.op("dve", lambda e: e.tensor_tensor(out=ph, in0=ph, in1=wk, op=ALU.add), reads=[b_cs], writes=[b_cs])
            P.op("act", lambda e: e.activation(out=cs[:, 0:2, :], in_=cs[:, 0:2, :], func=AF.Sin), reads=[b_cs], writes=[b_cs])
            sinb = cs[:, 0, :].unsqueeze(1).to_broadcast([128, 16, 32])
            cosb = cs[:, 1, :].unsqueeze(1).to_broadcast([128, 16, 32])
            for (src, b_src, lo, dstb, b_dstb) in ((q_sb, b_q, 128, qb, b_qb), (kp, b_kp, 0, kb, b_kb)):
                x1, x2 = src[:, :, lo:lo + 32], src[:, :, lo + 32:lo + 64]
                P.op("dve", lambda e, x1=x1: e.tensor_tensor(out=rtmp[:, :, 0:32], in0=x1, in1=cosb, op=ALU.mult), reads=[b_src, b_cs], writes=[b_rtmp])
                P.op("dve", lambda e, x2=x2: e.tensor_tensor(out=rtmp[:, :, 32:64], in0=x2, in1=sinb, op=ALU.mult), reads=[b_src, b_cs], writes=[b_rtmp])
                P.op("dve", lambda e, dstb=dstb: e.tensor_tensor(out=dstb[:, :, 128:160], in0=rtmp[:, :, 0:32], in1=rtmp[:, :, 32:64], op=ALU.subtract),
                     reads=[b_rtmp], writes=[b_dstb])
                P.op("dve", lambda e, x2=x2: e.tensor_tensor(out=rtmp[:, :, 0:32], in0=x2, in1=cosb, op=ALU.mult), reads=[b_src, b_cs, b_dstb], writes=[b_rtmp])
                P.op("dve", lambda e, x1=x1: e.tensor_tensor(out=rtmp[:, :, 32:64], in0=x1, in1=sinb, op=ALU.mult), reads=[b_src, b_cs], writes=[b_rtmp])
                P.op("dve", lambda e, dstb=dstb: e.tensor_tensor(out=dstb[:, :, 160:192], in0=rtmp[:, :, 0:32], in1=rtmp[:, :, 32:64], op=ALU.add),
                     reads=[b_rtmp], writes=[b_dstb])
            P.op("act", lambda e: e.copy(out=qb[:, :, 0:128], in_=q_sb[:, :, 0:128]), reads=[b_q], writes=[b_qb])
            P.op("act", lambda e: e.copy(out=kb[:, :, 0:128], in_=kv_sb[:, :, 0:128]), reads=[b_kv], writes=[b_kb])
            P.op("pool", lambda e: e.tensor_copy(out=vb[:], in_=kv_sb[:, :, 128:256]), reads=[b_kv], writes=[b_vb])
            P.dma("sp", lambda e, ti=ti: e.dma_start(out=Vh[:, ti * 128:(ti + 1) * 128, :].rearrange("h t d -> t h d"), in_=vb[:]), reads=[b_vb])
            for (srcb, b_srcb, Dn, Dr) in ((qb, b_qb, QTn, QTr), (kb, b_kb, KTn, KTr)):
                j = ti % 2
                transpose_to(lambda h, srcb=srcb: srcb[:, h, 0:128], 16, 128, lambda g0, nb_, j=j: oTn[j][:, g0:g0 + nb_, :], b_srcb, b_oTn[j])
                transpose_to(lambda h, srcb=srcb: srcb[:, h, 128:192], 16, 64, lambda g0, nb_, j=j: oTr[j][:, g0:g0 + nb_, :], b_srcb, b_oTr[j])
                P.dma("sp", lambda e, j=j, Dn=Dn, ti=ti: e.dma_start(out=Dn[:, :, ti * 128:(ti + 1) * 128].rearrange("h d t -> d h t"), in_=oTn[j][:]),
                      reads=[b_oTn[j]])
                P.dma("sp", lambda e, j=j, Dr=Dr, ti=ti: e.dma_start(out=Dr[:, :, ti * 128:(ti + 1) * 128].rearrange("h d t -> d h t"), in_=oTr[j][:]),
                      reads=[b_oTr[j]])
        k.end_stage()


def mla_attn(k, consts, qk_q_w, qk_k_w, QTn, QTr, KTn, KTr, Vh, S, Y, y_row0, y_col0):
    P = k.P
    nb = S // 128
    with ExitStack() as st:
        mk = k.sb(st, [128, 128], F32, "amk"); b_mk = P.buf()
        P.dma("sp", lambda e: e.dma_start(out=mk[:], in_=consts[:, 128:256]), writes=[b_mk])
        mkb = k.sb(st, [128, 128], BF16, "amkb"); b_mkb = P.buf()
        P.op("dve", lambda e: e.tensor_copy(out=mkb[:], in_=mk[:]), reads=[b_mk], writes=[b_mkb])
        gq, b_gq = load_bcast(k, st, qk_q_w, "agq"); gk, b_gk = load_bcast(k, st, qk_k_w, "agk")
        bd = k.sb(st, [128, 4], F32, "abd"); b_bd = P.buf()
        P.op("dve", lambda e: e.reduce_max(out=bd[:, 0:1], in_=gq[:], axis=AX.X, apply_absolute_value=True), reads=[b_gq], writes=[b_bd])
        P.op("dve", lambda e: e.reduce_max(out=bd[:, 1:2], in_=gk[:], axis=AX.X, apply_absolute_value=True), reads=[b_gk], writes=[b_bd])
        P.op("dve", lambda e: e.tensor_tensor(out=bd[:, 2:3], in0=bd[:, 0:1], in1=bd[:, 1:2], op=ALU.mult), reads=[b_bd], writes=[b_bd])
        P.op("dve", lambda e: e.tensor_scalar(out=bd[:, 2:3], in0=bd[:, 2:3], scalar1=-(float(QK_HEAD) ** 0.5), scalar2=None, op0=ALU.mult),
             reads=[b_bd], writes=[b_bd])
        kn = [k.sb(st, [128, S], BF16, "akn") for _ in range(2)]; kr = [k.sb(st, [64, S], BF16, "akr") for _ in range(2)]
        qn = [k.sb(st, [128, S], BF16, "aqn") for _ in range(2)]; qr = [k.sb(st, [64, S], BF16, "aqr") for _ in range(2)]
        vv = [k.sb(st, [128, nb, 132], BF16, "avv") for _ in range(2)]
        bh = [P.buf() for _ in range(2)]
        pS = [k.ps(st, [128, 512], F32, "apS") for _ in range(2)]; bpS = [P.buf() for _ in range(2)]
        pO = [k.ps(st, [128, 512], F32, "apO") for _ in range(4)]; bpO = [P.buf() for _ in range(4)]
        pt = [k.sb(st, [128, 512], BF16, "apt") for _ in range(3)]; bpt = [P.buf() for _ in range(3)]
        ob = [k.sb(st, [128, 132], F32, "aob") for _ in range(2)]; bob = [P.buf() for _ in range(2)]
        nS = nP = nO = 0
        for h in range(MLA_H):
            j = h % 2
            for (dst, src) in ((kn[j], KTn[h]), (kr[j], KTr[h]), (qn[j], QTn[h]), (qr[j], QTr[h])):
                P.dma("sp", lambda e, dst=dst, src=src: e.dma_start(out=dst[:], in_=src), writes=[bh[j]])
            P.dma("sp", lambda e, j=j, h=h: e.dma_start(out=vv[j][:, :, 0:128], in_=Vh[h].rearrange("(b p) d -> p b d", p=128)), writes=[bh[j]])
            P.op("pool", lambda e, j=j: e.memset(vv[j][:, :, 128:129], 1.0), writes=[bh[j]])
            for g0 in range(0, nb, 4):
                ng = min(4, nb - g0)
                for kb_ in range(g0 + ng):
                    lo = max(kb_, g0)
                    c0, c1 = (lo - g0) * 128, ng * 128
                    si = nS % 2; nS += 1
                    P.op("pe", lambda e, si=si, j=j, kb_=kb_, g0=g0, c0=c0, c1=c1: e.matmul(
                        pS[si][:, c0:c1], lhsT=kn[j][:, kb_ * 128:(kb_ + 1) * 128], rhs=qn[j][:, g0 * 128 + c0:g0 * 128 + c1], start=True, stop=False),
                        reads=[bh[j]], writes=[bpS[si]], skip_same=True)
                    P.op("pe", lambda e, si=si, j=j, kb_=kb_, g0=g0, c0=c0, c1=c1: e.matmul(
                        pS[si][:, c0:c1], lhsT=kr[j][:, kb_ * 128:(kb_ + 1) * 128], rhs=qr[j][:, g0 * 128 + c0:g0 * 128 + c1], start=False, stop=True),
                        reads=[bh[j]], writes=[bpS[si]], skip_same=True)
                    pi_ = nP % 3; nP += 1
                    P.op("act", lambda e, si=si, pi_=pi_, c0=c0, c1=c1: e.activation(out=pt[pi_][:, c0:c1], in_=pS[si][:, c0:c1], func=AF.Exp,
                                                                                    bias=bd[:, 2:3], scale=1.0),
                         reads=[bpS[si], b_bd], writes=[bpt[pi_]])
                    if kb_ >= g0:
                        P.op("dve", lambda e, pi_=pi_, c0=c0: e.tensor_tensor(out=pt[pi_][:, c0:c0 + 128], in0=pt[pi_][:, c0:c0 + 128], in1=mkb[:], op=ALU.mult),
                             reads=[bpt[pi_], b_mkb], writes=[bpt[pi_]])
                    for qi in range(lo, g0 + ng):
                        a = qi - g0
                        P.op("pe", lambda e, pi_=pi_, a=a, j=j, kb_=kb_, qi=qi: e.matmul(
                            pO[a][:, 0:129], lhsT=pt[pi_][:, a * 128:(a + 1) * 128], rhs=vv[j][:, kb_, 0:129], start=(kb_ == 0), stop=(kb_ == qi)),
                            reads=[bpt[pi_], bh[j]], writes=[bpO[a]], skip_same=True)
                for a in range(ng):
                    oi = nO % 2; nO += 1
                    qi = g0 + a
                    P.op("dve", lambda e, oi=oi, a=a: e.reciprocal(out=ob[oi][:, 129:130], in_=pO[a][:, 128:129]), reads=[bpO[a]], writes=[bob[oi]])
                    P.op("dve", lambda e, oi=oi, a=a: e.tensor_scalar(out=ob[oi][:, 0:128], in0=pO[a][:, 0:128], scalar1=ob[oi][:, 129:130], scalar2=None,
                                                                     op0=ALU.mult), reads=[bpO[a], bob[oi]], writes=[bob[oi]])
                    P.dma("sp", lambda e, oi=oi, qi=qi, h=h: e.dma_start(out=Y[y_row0 + qi * 128:y_row0 + (qi + 1) * 128, y_col0 + h * 128:y_col0 + (h + 1) * 128],
                                                                        in_=ob[oi][:, 0:128]), reads=[bob[oi]])
        k.end_stage()


RW_H, RW_N, RW_C = 32, 64, 64
RW_W = RW_H * RW_N


def rwkv_scan(k, consts, AT, BT, KT_, RT, GC, Vtm, Btm, Ktm, bonus, Gt, lnw, lnb, S, Y, y_row0, y_col0, HB=4, CB=8):
    P = k.P
    C, N = RW_C, RW_N
    nch = S // C
    with ExitStack() as st:
        ms = k.sb(st, [64, 4, 64], F32, "smask"); b_ms = P.buf()
        P.dma("sp", lambda e: e.dma_start(out=ms[:, 0, :], in_=consts[0:64, 0:64]), writes=[b_ms])
        P.dma("sp", lambda e: e.dma_start(out=ms[:, 1, :], in_=consts[0:64, 128:192]), writes=[b_ms])
        P.dma("sp", lambda e: e.dma_start(out=ms[:, 2, :], in_=consts[0:64, 288:352]), writes=[b_ms])
        P.dma("sp", lambda e: e.dma_start(out=ms[:, 3, :], in_=consts[0:64, 352:416]), writes=[b_ms])
        mI = ms[:, 0, :].unsqueeze(1).to_broadcast([64, HB, 64]); mIU = ms[:, 1, :].unsqueeze(1).to_broadcast([64, HB, 64])
        mSU = ms[:, 2, :].unsqueeze(1).to_broadcast([64, HB, 64]); mSL = ms[:, 3, :].unsqueeze(1).to_broadcast([64, HB, 64])
        fm = [[k.sb(st, [64, HB, CB * C], F32, "sfm") for _ in range(4)] for _ in range(2)]; b_fm = [P.buf() for _ in range(2)]
        gc = [k.sb(st, [64, HB, CB], F32, "sgc") for _ in range(2)]
        tm = [[k.sb(st, [64, CB, HB * N], F32, "stm") for _ in range(4)] for _ in range(2)]; b_tm = [P.buf() for _ in range(2)]
        bn = [k.sb(st, [64, CB, HB], F32, "sbn") for _ in range(2)]
        lw_t = k.sb(st, [64, HB * N], F32, "slw"); lb_t = k.sb(st, [64, HB * N], F32, "slb"); b_ln = P.buf()
        Z = k.sb(st, [64, HB, N], F32, "sZ"); b_Z = P.buf()
        names = ("Nm", "Lm", "LkT", "MbT", "MkT", "Pw", "Pt", "X", "Xt", "RHS", "U", "yt", "yc", "sq")
        W = {n: k.sb(st, [64, HB, 64], F32, "s" + n) for n in names}; bW = {n: P.buf() for n in names}
        stt = k.sb(st, [64, HB, 4], F32, "sst"); b_st = P.buf()
        ot = [k.sb(st, [64, HB, 64], F32, "sot") for _ in range(2)]; b_ot = [P.buf() for _ in range(2)]
        pp = [k.ps(st, [64, HB, 64], F32, "spp") for _ in range(7)]; bpp = [P.buf() for _ in range(7)]

        def mm_heads(pi, lhs_fn, rhs_fn, reads):
            mm_acc(pi, [(lhs_fn, rhs_fn)], reads)

        def mm_acc(pi, terms, reads):
            nt_ = len(terms)
            for h in range(HB):
                for ti_, (lhs_fn, rhs_fn) in enumerate(terms):
                    l_ap, r_ap = lhs_fn(h), rhs_fn(h)
                    P.op("pe", lambda e, h=h, l_ap=l_ap, r_ap=r_ap, ti_=ti_: e.matmul(
                        pp[pi][:, h, :], lhsT=l_ap, rhs=r_ap, start=(ti_ == 0), stop=(ti_ == nt_ - 1)),
                        reads=reads, writes=[bpp[pi]], skip_same=True)

        for h0 in range(0, RW_H, HB):
            r0, r1 = h0 * N, (h0 + HB) * N
            P.dma("sp", lambda e, r0=r0, r1=r1: e.dma_start(out=lw_t[:], in_=lnw[r0:r1].partition_broadcast(64)), writes=[b_ln])
            P.dma("sp", lambda e, r0=r0, r1=r1: e.dma_start(out=lb_t[:], in_=lnb[r0:r1].partition_broadcast(64)), writes=[b_ln])
            P.op("dve", lambda e: e.memset(Z[:], 0.0), writes=[b_Z])
            for cb in range(nch // CB):
                j = cb % 2
                t0 = cb * CB * C
                for i, src in enumerate((AT, BT, KT_, RT)):
                    P.dma("sp", lambda e, i=i, src=src, j=j, t0=t0, r0=r0, r1=r1: e.dma_start(
                        out=fm[j][i][:], in_=src[r0:r1, t0:t0 + CB * C].rearrange("(h j) t -> j h t", j=64)), writes=[b_fm[j]])
                P.dma("sp", lambda e, j=j, cb=cb, r0=r0, r1=r1: e.dma_start(out=gc[j][:], in_=GC[r0:r1, cb * CB:(cb + 1) * CB].rearrange("(h j) c -> j h c", j=64)),
                      writes=[b_fm[j]])
                for i, src in enumerate((Vtm, Btm, Ktm, Gt)):
                    P.dma("sp", lambda e, i=i, src=src, j=j, t0=t0, r0=r0, r1=r1: e.dma_start(
                        out=tm[j][i][:], in_=src[t0:t0 + CB * C, r0:r1].rearrange("(c t) f -> t c f", t=64)), writes=[b_tm[j]])
                P.dma("sp", lambda e, j=j, t0=t0, h0=h0: e.dma_start(out=bn[j][:], in_=bonus[t0:t0 + CB * C, h0:h0 + HB].rearrange("(c t) h -> t c h", t=64)),
                      writes=[b_tm[j]])
                A_, B_, K_, R_ = fm[j]
                V_, Bm_, Km_, G_ = tm[j]
                for ci in range(CB):
                    cs_ = slice(ci * C, (ci + 1) * C)
                    fr_ = [b_fm[j]]
                    mm_heads(0, lambda h: B_[:, h, cs_], lambda h: A_[:, h, cs_], fr_)
                    mm_heads(1, lambda h: A_[:, h, cs_], lambda h: B_[:, h, cs_], fr_)
                    mm_heads(2, lambda h: K_[:, h, cs_], lambda h: A_[:, h, cs_], fr_)
                    mm_heads(3, lambda h: B_[:, h, cs_], lambda h: R_[:, h, cs_], fr_)
                    mm_heads(4, lambda h: K_[:, h, cs_], lambda h: R_[:, h, cs_], fr_)
                    for (pi, nm, mk) in ((0, "Nm", mSU), (1, "Lm", mSL), (2, "LkT", mSU), (3, "MbT", mIU), (4, "MkT", mIU)):
                        P.op("dve", lambda e, pi=pi, nm=nm, mk=mk: e.tensor_tensor(out=W[nm][:], in0=pp[pi][:], in1=mk, op=ALU.mult),
                             reads=[bpp[pi], b_ms], writes=[bW[nm]])
                    P.op("pool", lambda e: e.tensor_copy(out=W["Pw"][:], in_=W["Nm"][:]), reads=[bW["Nm"]], writes=[bW["Pw"]])
                    P.op("pool", lambda e: e.tensor_copy(out=W["Pt"][:], in_=W["Lm"][:]), reads=[bW["Lm"]], writes=[bW["Pt"]])
                    P.op("dve", lambda e: e.tensor_tensor(out=W["X"][:], in0=W["Nm"][:], in1=mI, op=ALU.add), reads=[bW["Nm"], b_ms], writes=[bW["X"]])
                    P.op("dve", lambda e: e.tensor_tensor(out=W["Xt"][:], in0=W["Lm"][:], in1=mI, op=ALU.add), reads=[bW["Lm"], b_ms], writes=[bW["Xt"]])
                    for rd in range(5):
                        last = rd == 4
                        mm_heads(5, lambda h: W["Pt"][:, h, :], lambda h: W["Pw"][:, h, :], [bW["Pt"], bW["Pw"]])
                        if not last:
                            mm_heads(6, lambda h: W["Pw"][:, h, :], lambda h: W["Pt"][:, h, :], [bW["Pt"], bW["Pw"]])
                        P.op("act", lambda e: e.copy(out=W["Pw"][:], in_=pp[5][:]), reads=[bpp[5]], writes=[bW["Pw"]])
                        if not last:
                            P.op("act", lambda e: e.copy(out=W["Pt"][:], in_=pp[6][:]), reads=[bpp[6]], writes=[bW["Pt"]])
                        mm_heads(5, lambda h: W["Xt"][:, h, :], lambda h: W["Pw"][:, h, :], [bW["Xt"], bW["Pw"]])
                        if not last:
                            mm_heads(6, lambda h: W["Pw"][:, h, :], lambda h: W["Xt"][:, h, :], [bW["Xt"], bW["Pw"]])
                        P.op("dve", lambda e: e.tensor_tensor(out=W["X"][:], in0=W["X"][:], in1=pp[5][:], op=ALU.add), reads=[bW["X"], bpp[5]], writes=[bW["X"]])
                        if not last:
                            P.op("dve", lambda e: e.tensor_tensor(out=W["Xt"][:], in0=W["Xt"][:], in1=pp[6][:], op=ALU.add), reads=[bW["Xt"], bpp[6]], writes=[bW["Xt"]])
                    Vc = lambda h: V_[:, ci, h * N:(h + 1) * N]
                    mm_acc(0, [(lambda h: A_[:, h, cs_], lambda h: Z[:, h, :]), (lambda h: W["LkT"][:, h, :], Vc)], fr_ + [b_Z, bW["LkT"], b_tm[j]])
                    P.op("act", lambda e: e.copy(out=W["RHS"][:], in_=pp[0][:]), reads=[bpp[0]], writes=[bW["RHS"]])
                    mm_heads(1, lambda h: W["X"][:, h, :], lambda h: W["RHS"][:, h, :], [bW["X"], bW["RHS"]])
                    P.op("act", lambda e: e.copy(out=W["U"][:], in_=pp[1][:]), reads=[bpp[1]], writes=[bW["U"]])
                    mm_acc(2, [(lambda h: R_[:, h, cs_], lambda h: Z[:, h, :]), (lambda h: W["MbT"][:, h, :], lambda h: W["U"][:, h, :]),
                               (lambda h: W["MkT"][:, h, :], Vc)], fr_ + [b_Z, bW["MbT"], bW["U"], bW["MkT"], b_tm[j]])
                    P.op("act", lambda e: e.copy(out=W["yt"][:], in_=pp[2][:]), reads=[bpp[2]], writes=[bW["yt"]])
                    mm_acc(3, [(lambda h: Bm_[:, ci, h * N:(h + 1) * N], lambda h: W["U"][:, h, :]), (lambda h: Km_[:, ci, h * N:(h + 1) * N], Vc)],
                           [b_tm[j], bW["U"]])
                    P.op("dve", lambda e: e.tensor_tensor(out=Z[:], in0=Z[:], in1=pp[3][:], op=ALU.add), reads=[b_Z, bpp[3]], writes=[b_Z])
                    gcb = gc[j][:, :, ci:ci + 1].to_broadcast([64, HB, N])
                    P.op("dve", lambda e, gcb=gcb: e.tensor_tensor(out=Z[:], in0=Z[:], in1=gcb, op=ALU.mult),
                         reads=[b_Z, b_fm[j]], writes=[b_Z])
                    y_, yc, sq = W["yt"], W["yc"], W["sq"]
                    P.op("dve", lambda e: e.reduce_sum(out=stt[:, :, 0:1], in_=y_[:], axis=AX.X), reads=[bW["yt"]], writes=[b_st])
                    P.op("dve", lambda e: e.tensor_scalar(out=stt[:, :, 0:1], in0=stt[:, :, 0:1], scalar1=-1.0 / N, scalar2=None, op0=ALU.mult),
                         reads=[b_st], writes=[b_st])
                    P.op("dve", lambda e: e.tensor_tensor(out=yc[:], in0=y_[:], in1=stt[:, :, 0:1].to_broadcast([64, HB, N]), op=ALU.add),
                         reads=[bW["yt"], b_st], writes=[bW["yc"]])
                    P.op("act", lambda e: e.activation(out=sq[:], in_=yc[:], func=AF.Square), reads=[bW["yc"]], writes=[bW["sq"]])
                    P.op("dve", lambda e: e.reduce_sum(out=stt[:, :, 1:2], in_=sq[:], axis=AX.X), reads=[bW["sq"]], writes=[b_st])
                    P.op("dve", lambda e: e.tensor_scalar(out=stt[:, :, 1:2], in0=stt[:, :, 1:2], scalar1=1.0 / N, scalar2=64e-5, op0=ALU.mult, op1=ALU.add),
                         reads=[b_st], writes=[b_st])
                    P.op("act", lambda e: e.activation(out=stt[:, :, 1:2], in_=stt[:, :, 1:2], func=AF.Sqrt), reads=[b_st], writes=[b_st])
                    P.op("dve", lambda e: e.reciprocal(out=stt[:, :, 1:2], in_=stt[:, :, 1:2]), reads=[b_st], writes=[b_st])
                    P.op("dve", lambda e: e.tensor_tensor(out=yc[:], in0=yc[:], in1=stt[:, :, 1:2].to_broadcast([64, HB, N]), op=ALU.mult),
                         reads=[bW["yc"], b_st], writes=[bW["yc"]])
                    P.op("dve", lambda e: e.tensor_tensor(out=yc[:], in0=yc[:], in1=lw_t[:].rearrange("p (h n) -> p h n", n=N), op=ALU.mult),
                         reads=[bW["yc"], b_ln], writes=[bW["yc"]])
                    P.op("pool", lambda e: e.tensor_tensor(out=yc[:], in0=yc[:], in1=lb_t[:].rearrange("p (h n) -> p h n", n=N), op=ALU.add),
                         reads=[bW["yc"], b_ln], writes=[bW["yc"]])
                    Vc3 = V_[:, ci, :].rearrange("p (h n) -> p h n", n=N)
                    bnb = bn[j][:, ci, :].unsqueeze(2).to_broadcast([64, HB, N])
                    P.op("pool", lambda e, bnb=bnb, Vc3=Vc3: e.tensor_tensor(out=sq[:], in0=Vc3, in1=bnb, op=ALU.mult),
                         reads=[b_tm[j], bW["sq"]], writes=[bW["sq"]])
                    P.op("pool", lambda e: e.tensor_tensor(out=yc[:], in0=yc[:], in1=sq[:], op=ALU.add), reads=[bW["yc"], bW["sq"]], writes=[bW["yc"]])
                    oi = ci % 2
                    g3 = G_[:, ci, :].rearrange("p (h n) -> p h n", n=N)
                    P.op("dve", lambda e, oi=oi, g3=g3: e.tensor_tensor(out=ot[oi][:], in0=yc[:], in1=g3, op=ALU.mult),
                         reads=[bW["yc"], b_tm[j]], writes=[b_ot[oi]])
                    tr = y_row0 + t0 + ci * C
                    P.dma("sp", lambda e, oi=oi, tr=tr, r0=r0, r1=r1: e.dma_start(out=Y[tr:tr + C, y_col0 + r0:y_col0 + r1], in_=ot[oi][:].rearrange("p h n -> p (h n)")),
                          reads=[b_ot[oi]])
        k.end_stage()


RW_IN = 3 * RW_W + 64 + 64 + 128
C_BONES, C_SEL, C_RMASK, C_W = 512, 640, 768, 1280


def rwkv_prep(k, consts, PT, t_base, S, rwp, w2, a2, g2, AT, BT, KT_, RT, GC, Vtm, Btm, Ktm, bonus, Gt, TB=512):
    P = k.P
    n = TB
    nsl = n // 128
    with ExitStack() as st:
        def tile(shape, name, dt=F32):
            return k.sb(st, shape, dt, name), P.buf()
        idf, b_idf = tile([128, 128], "ridf"); bones, b_bones = tile([128, 128], "rbo"); sel, b_sel = tile([128, 2], "rsel")
        rmask, b_rm = tile([128, n], "rrm"); prm, b_prm = tile([128, 132], "rprm"); omk, b_omk = tile([128, 50 + 16], "romk")
        wl, b_wl = tile([128, RW_W], "rwl"); g2s, b_g2 = tile([128, RW_W], "rg2")
        P.dma("sp", lambda e: e.dma_start(out=idf[:], in_=consts[:, 0:128]), writes=[b_idf])
        P.dma("sp", lambda e: e.dma_start(out=bones[:], in_=consts[:, C_BONES:C_BONES + 128]), writes=[b_bones])
        P.dma("sp", lambda e: e.dma_start(out=sel[:], in_=consts[:, C_SEL:C_SEL + 2]), writes=[b_sel])
        P.dma("sp", lambda e: e.dma_start(out=rmask[:], in_=consts[:, C_RMASK:C_RMASK + n]), writes=[b_rm])
        P.dma("sp", lambda e: e.dma_start(out=prm[:, 0:130], in_=rwp[:, :]), writes=[b_prm])
        P.dma("sp", lambda e: e.dma_start(out=wl[0:64, :], in_=w2[:, :]), writes=[b_wl])
        P.dma("sp", lambda e: e.dma_start(out=wl[64:128, :], in_=a2[:, :]), writes=[b_wl])
        P.dma("sp", lambda e: e.dma_start(out=g2s[:], in_=g2[:, :]), writes=[b_g2])
        MU, W0, A0, KK_, KA, RK = 0, 50, 66, 82, 98, 114
        P.op("dve", lambda e: e.tensor_scalar(out=omk[:, 0:50], in0=prm[:, MU:MU + 50], scalar1=-1.0, scalar2=1.0, op0=ALU.mult, op1=ALU.add),
             reads=[b_prm], writes=[b_omk])
        P.op("dve", lambda e: e.tensor_scalar(out=omk[:, 50:66], in0=prm[:, KA:KA + 16], scalar1=-1.0, scalar2=1.0, op0=ALU.mult, op1=ALU.add),
             reads=[b_prm], writes=[b_omk])
        raw = [tile([128, 1 + n], "rraw") for _ in range(4)]
        la, b_la = tile([128, n], "rla"); sg, b_sg = tile([128, n], "rsg")
        rm, b_r = tile([128, n], "rr"); km, b_k = tile([128, n], "rk"); vm, b_v = tile([128, n], "rv")
        lw, b_lw = tile([128, n], "rlw"); aa, b_a = tile([128, n], "ra"); kk, b_kk = tile([128, n], "rkk"); kp, b_kp = tile([128, n], "rkp")
        cl, b_cl = tile([128, n], "rcl"); ep, b_ep = tile([128, n], "rep"); en, b_en = tile([128, n], "ren"); ev, b_ev = tile([128, n], "rev")
        t1, b_t1 = tile([128, n], "rt1"); t2, b_t2 = tile([128, n], "rt2")
        oA, b_oA = tile([128, n], "roA"); oB, b_oB = tile([128, n], "roB"); oK, b_oK = tile([128, n], "roK"); oR, b_oR = tile([128, n], "roR")
        gcs, b_gcs = tile([128, n // 64], "rgcs"); bns, b_bns = tile([128, nsl, 2], "rbns")
        tmo = [tile([128, 3, 128], "rtmo") for _ in range(2)]
        gto = [tile([128, 512], "rgto") for _ in range(2)]
        pM = [k.ps(st, [128, 512], F32, "rpM") for _ in range(3)]; bpM = [P.buf() for _ in range(3)]
        pT = [k.ps(st, [128, 3, 128], F32, "rpT") for _ in range(2)]; bpT = [P.buf() for _ in range(2)]
        pB = k.ps(st, [128, 8], F32, "rpB"); b_pB = P.buf()
        nraw = [0]

        def load_mixed(row0, mu_col, dst, b_dst, t0, parts=slice(0, 128)):
            (rw_, b_rw) = raw[nraw[0] % 4]; nraw[0] += 1
            c0 = t_base + t0
            if t0 == 0:
                P.op("pool", lambda e, rw_=rw_: e.memset(rw_[:, 0:1], 0.0), writes=[b_rw])
                P.dma("sp", lambda e, rw_=rw_, c0=c0: e.dma_start(out=rw_[:, 1:1 + n], in_=PT[row0:row0 + 128, c0:c0 + n]), writes=[b_rw])
            else:
                P.dma("sp", lambda e, rw_=rw_, c0=c0: e.dma_start(out=rw_[:, 0:1 + n], in_=PT[row0:row0 + 128, c0 - 1:c0 + n]), writes=[b_rw])
            P.op("dve", lambda e, rw_=rw_: e.tensor_scalar(out=dst[:], in0=rw_[:, 1:1 + n], scalar1=omk[:, mu_col:mu_col + 1], scalar2=None, op0=ALU.mult),
                 reads=[b_rw, b_omk], writes=[b_dst])
            P.op("dve", lambda e, rw_=rw_: e.scalar_tensor_tensor(out=dst[:], in0=rw_[:, 0:n], scalar=prm[:, MU + mu_col:MU + mu_col + 1], in1=dst[:],
                                                                 op0=ALU.mult, op1=ALU.add), reads=[b_rw, b_prm, b_dst], writes=[b_dst])

        ntm = ngt = 0
        for t0 in range(0, S, n):
            load_mixed(6144, 48, la, b_la, t0)
            P.op("act", lambda e: e.activation(out=la[0:64, :], in_=la[0:64, :], func=AF.Tanh), reads=[b_la], writes=[b_la])
            load_mixed(6272, 49, sg, b_sg, t0)
            P.op("act", lambda e: e.activation(out=sg[:], in_=sg[:], func=AF.Sigmoid), reads=[b_sg], writes=[b_sg])
            for sl_ in range(nsl):
                for cbk in range(4):
                    i = ngt % 2; ngt += 1
                    (go, b_go) = gto[i]
                    pi = 2
                    P.op("pe", lambda e, sl_=sl_, cbk=cbk: e.matmul(pM[2][:], lhsT=sg[:, sl_ * 128:(sl_ + 1) * 128], rhs=g2s[:, cbk * 512:(cbk + 1) * 512],
                                                                   start=True, stop=True), reads=[b_sg, b_g2], writes=[bpM[2]], skip_same=True)
                    P.op("act", lambda e, go=go: e.copy(out=go[:], in_=pM[2][:]), reads=[bpM[2]], writes=[b_go])
                    tr = t_base * 0 + t0 + sl_ * 128
                    P.dma("sp", lambda e, go=go, tr=tr, cbk=cbk: e.dma_start(out=Gt[tr:tr + 128, cbk * 512:(cbk + 1) * 512], in_=go[:]), reads=[b_go])
            for c in range(16):
                load_mixed(c * 128, c, rm, b_r, t0)
                load_mixed(RW_W + c * 128, 16 + c, km, b_k, t0)
                load_mixed(2 * RW_W + c * 128, 32 + c, vm, b_v, t0)
                fc = slice(c * 128, (c + 1) * 128)
                P.op("pe", lambda e, fc=fc: e.matmul(pM[0][:, 0:n], lhsT=wl[0:64, fc], rhs=la[0:64, :], start=True, stop=True),
                     reads=[b_wl, b_la], writes=[bpM[0]], skip_same=True)
                P.op("act", lambda e, c=c: e.activation(out=lw[:], in_=pM[0][:, 0:n], func=AF.Sigmoid, bias=prm[:, W0 + c:W0 + c + 1], scale=1.0),
                     reads=[bpM[0], b_prm], writes=[b_lw])
                P.op("dve", lambda e: e.tensor_scalar(out=lw[:], in0=lw[:], scalar1=-0.6065306597126334, scalar2=None, op0=ALU.mult), reads=[b_lw], writes=[b_lw])
                P.op("pe", lambda e, fc=fc: e.matmul(pM[1][:, 0:n], lhsT=wl[64:128, fc], rhs=la[64:128, :], start=True, stop=True),
                     reads=[b_wl, b_la], writes=[bpM[1]], skip_same=True)
                P.op("act", lambda e, c=c: e.activation(out=aa[:], in_=pM[1][:, 0:n], func=AF.Sigmoid, bias=prm[:, A0 + c:A0 + c + 1], scale=1.0),
                     reads=[bpM[1], b_prm], writes=[b_a])
                P.op("dve", lambda e, c=c: e.tensor_scalar(out=kk[:], in0=km[:], scalar1=prm[:, KK_ + c:KK_ + c + 1], scalar2=None, op0=ALU.mult),
                     reads=[b_k, b_prm], writes=[b_kk])
                P.op("act", lambda e: e.activation(out=t1[:], in_=kk[:], func=AF.Square), reads=[b_kk], writes=[b_t1])
                P.op("pe", lambda e: e.matmul(pM[0][:, 0:n], lhsT=bones[:], rhs=t1[:], start=True, stop=True), reads=[b_bones, b_t1], writes=[bpM[0]], skip_same=True)
                P.op("act", lambda e: e.activation(out=t2[:], in_=pM[0][:, 0:n], func=AF.Sqrt), reads=[bpM[0]], writes=[b_t2])
                P.op("dve", lambda e: e.tensor_scalar(out=t2[:], in0=t2[:], scalar1=1e-12, scalar2=None, op0=ALU.max), reads=[b_t2], writes=[b_t2])
                P.op("dve", lambda e: e.reciprocal(out=t2[:], in_=t2[:]), reads=[b_t2], writes=[b_t2])
                P.op("dve", lambda e: e.tensor_tensor(out=kk[:], in0=kk[:], in1=t2[:], op=ALU.mult), reads=[b_kk, b_t2], writes=[b_kk])
                P.op("dve", lambda e, c=c: e.tensor_scalar(out=kp[:], in0=aa[:], scalar1=prm[:, KA + c:KA + c + 1], scalar2=omk[:, 50 + c:51 + c],
                                                          op0=ALU.mult, op1=ALU.add), reads=[b_a, b_prm, b_omk], writes=[b_kp])
                P.op("dve", lambda e: e.tensor_tensor(out=kp[:], in0=kp[:], in1=km[:], op=ALU.mult), reads=[b_kp, b_k], writes=[b_kp])
                P.op("dve", lambda e: e.tensor_tensor_scan(out=cl[:], data0=rmask[:], data1=lw[:], initial=0.0, op0=ALU.mult, op1=ALU.add),
                     reads=[b_rm, b_lw], writes=[b_cl])
                P.op("act", lambda e: e.activation(out=ep[:], in_=cl[:], func=AF.Exp), reads=[b_cl], writes=[b_ep])
                P.op("act", lambda e: e.activation(out=en[:], in_=cl[:], func=AF.Exp, scale=-1.0), reads=[b_cl], writes=[b_en])
                P.op("pool", lambda e: e.tensor_tensor(out=t1[:], in0=cl[:], in1=lw[:], op=ALU.subtract), reads=[b_cl, b_lw, b_t1], writes=[b_t1])
                P.op("act", lambda e: e.activation(out=ev[:], in_=t1[:], func=AF.Exp), reads=[b_t1], writes=[b_ev])
                P.op("dve", lambda e: e.scalar_tensor_tensor(out=oA[:], in0=kk[:], scalar=-1.0, in1=ev[:], op0=ALU.mult, op1=ALU.mult),
                     reads=[b_kk, b_ev], writes=[b_oA])
                P.op("pool", lambda e: e.tensor_tensor(out=oB[:], in0=kk[:], in1=aa[:], op=ALU.mult), reads=[b_kk, b_a], writes=[b_oB])
                P.op("pool", lambda e: e.tensor_tensor(out=oB[:], in0=oB[:], in1=en[:], op=ALU.mult), reads=[b_oB, b_en], writes=[b_oB])
                P.op("dve", lambda e: e.tensor_tensor(out=oK[:], in0=kp[:], in1=en[:], op=ALU.mult), reads=[b_kp, b_en], writes=[b_oK])
                P.op("pool", lambda e: e.tensor_tensor(out=oR[:], in0=rm[:], in1=ep[:], op=ALU.mult), reads=[b_r, b_ep], writes=[b_oR])
                P.op("dve", lambda e: e.tensor_copy(out=gcs[:], in_=ep[:, 63::64]), reads=[b_ep], writes=[b_gcs])
                for (src, dstD) in ((oA, AT), (oB, BT), (oK, KT_), (oR, RT)):
                    bsrc = {id(oA): b_oA, id(oB): b_oB, id(oK): b_oK, id(oR): b_oR}[id(src)]
                    P.dma("sp", lambda e, src=src, dstD=dstD, fc=fc, t0=t0: e.dma_start(out=dstD[fc, t0:t0 + n], in_=src[:]), reads=[bsrc])
                P.dma("sp", lambda e, fc=fc, t0=t0: e.dma_start(out=GC[fc, t0 // 64:(t0 + n) // 64], in_=gcs[:]), reads=[b_gcs])
                P.op("dve", lambda e, c=c: e.scalar_tensor_tensor(out=t2[:], in0=rm[:], scalar=prm[:, RK + c:RK + c + 1], in1=kp[:], op0=ALU.mult, op1=ALU.mult),
                     reads=[b_r, b_prm, b_kp, b_t2], writes=[b_t2])
                for sl_ in range(nsl):
                    ts_ = slice(sl_ * 128, (sl_ + 1) * 128)
                    P.op("pe", lambda e, ts_=ts_, sl_=sl_: e.matmul(pB[:, sl_ * 2:sl_ * 2 + 2], lhsT=t2[:, ts_], rhs=sel[:], start=True, stop=True),
                         reads=[b_t2, b_sel], writes=[b_pB], skip_same=True)
                P.op("act", lambda e: e.copy(out=bns[:].rearrange("p s h -> p (s h)"), in_=pB[:, 0:2 * nsl]), reads=[b_pB], writes=[b_bns])
                P.dma("sp", lambda e, c=c, t0=t0: e.dma_start(out=bonus[t0:t0 + n, 2 * c:2 * c + 2].rearrange("(s p) h -> p s h", p=128), in_=bns[:]), reads=[b_bns])
                for sl_ in range(nsl):
                    ts_ = slice(sl_ * 128, (sl_ + 1) * 128)
                    i = ntm % 2; ntm += 1
                    (to, b_to) = tmo[i]
                    for q_, (src, bsrc) in enumerate(((vm, b_v), (oB, b_oB), (oK, b_oK))):
                        P.op("pe", lambda e, i=i, q_=q_, src=src, ts_=ts_: e.transpose(out=pT[i][:, q_, :], in_=src[:, ts_], identity=idf[:]),
                             reads=[bsrc, b_idf], writes=[bpT[i]], skip_same=True)
                    P.op("act", lambda e, i=i, to=to: e.copy(out=to[:], in_=pT[i][:]), reads=[bpT[i]], writes=[b_to])
                    tr = t0 + sl_ * 128
                    for q_, dstD in enumerate((Vtm, Btm, Ktm)):
                        P.dma("sp", lambda e, to=to, q_=q_, dstD=dstD, tr=tr, fc=fc: e.dma_start(out=dstD[tr:tr + 128, fc], in_=to[:, q_, :]), reads=[b_to])
        k.end_stage()


def load_bcast(k, st, vec_ap, name):
    P = k.P
    t = k.sb(st, [128, vec_ap.shape[0]], F32, name)
    b = P.buf()
    P.dma("sp", lambda e: e.dma_start(out=t[:], in_=vec_ap.partition_broadcast(128)), writes=[b])
    return t, b


def stage_plain_T(k, st, ident_bf, src, r0, nt, yT):
    P = k.P
    (idb, b_id), (yT_t, b_yT) = ident_bf, yT
    xt = [k.sb(st, [128, D], F32, "tx") for _ in range(2)]; bx = [P.buf() for _ in range(2)]
    xb = k.sb(st, [128, D], BF16, "txb"); b_xb = P.buf()
    pT = [k.ps(st, [128, 8, 128], BF16, "tpT") for _ in range(2)]; bpT = [P.buf() for _ in range(2)]
    for i in range(nt // 128):
        x_t, b_x = xt[i % 2], bx[i % 2]
        rr = r0 + i * 128
        P.dma("sp", lambda e, x_t=x_t, rr=rr: e.dma_start(out=x_t[:], in_=src[rr:rr + 128, :]), writes=[b_x])
        P.op("act", lambda e, x_t=x_t: e.copy(out=xb[:], in_=x_t[:]), reads=[b_x], writes=[b_xb])
        for g in range(KC // 8):
            p_t, b_p = pT[g % 2], bpT[g % 2]
            for j in range(8):
                c = g * 8 + j
                P.op("pe", lambda e, p_t=p_t, j=j, c=c: e.transpose(out=p_t[:, j, :], in_=xb[:, c * 128:(c + 1) * 128], identity=idb[:]),
                     reads=[b_xb, b_id], writes=[b_p], skip_same=True)
            if g % 2 == 0:
                P.op("act", lambda e, p_t=p_t, g=g, i=i: e.copy(out=yT_t[:, g * 8:(g + 1) * 8, i * 128:(i + 1) * 128], in_=p_t[:]), reads=[b_p], writes=[b_yT])
            else:
                P.op("dve", lambda e, p_t=p_t, g=g, i=i: e.tensor_copy(out=yT_t[:, g * 8:(g + 1) * 8, i * 128:(i + 1) * 128], in_=p_t[:]), reads=[b_p], writes=[b_yT])


def load_norm_bcast(k, st, nw, sc, sh):
    P = k.P
    g_t, b_g = load_bcast(k, st, nw, "nbg"); s_t, b_s = load_bcast(k, st, sc, "nbs"); h_t, b_h = load_bcast(k, st, sh, "nbh")
    P.op("dve", lambda e: e.scalar_tensor_tensor(out=g_t[:], in0=s_t[:], scalar=1.0, in1=g_t[:], op0=ALU.add, op1=ALU.mult),
         reads=[b_s, b_g], writes=[b_g])
    return (g_t, b_g), (h_t, b_h)


INPUT_SHAPES = lambda B, T: dict(
    x=[T, D], cT=[128, KC, B], pos=[T, 1], consts=[128, C_W], ada_w=[2, 6 * D // 512, 128, KC, 512], ada_b=[2, 6 * D], norm_mix_w=[2, D], norm_ffn_w=[2, D],
    hyb_w_mla=[3, 128, KC, 512], hyb_w_rw=[RW_IN // 128, 128, KC, 128], mla_q_norm_w=[Q_LORA], mla_w_uq=[Q_LORA, MLA_H * QK_HEAD], mla_kv_norm_w=[KV_LORA],
    mla_w_ukv=[KV_LORA, MLA_H * (QK_NOPE + V_HEAD)], mla_qk_q_w=[QK_HEAD], mla_qk_k_w=[QK_HEAD],
    rwp=[128, 130], rwkv_w2=[64, RW_W], rwkv_a2=[64, RW_W], rwkv_g2=[128, RW_W], rwkv_lnx_w=[RW_W], rwkv_lnx_b=[RW_W], hyb_w_out=[D // 512, 128, KC, 512],
    conv_w_in=[3 * KC, 128, KC, 128], conv_w3=[128, KC, 3], conv_w_out=[D // 512, 128, KC, 512], peer_w_q=[2, 16, 128, KC, 128], keysT=[2, 128, 16, 128], peer_u=[2, NE, D], peer_v=[2, NE, D])


def build(B, S):
    T = B * S
    nc = bass.Bass("TRN2", target_bir_lowering=False)
    A = {}
    for name, shp in INPUT_SHAPES(B, T).items():
        A[name] = nc.dram_tensor(name, list(shp), mybir.dt.int32 if name == "pos" else F32, kind="ExternalInput").ap()
    out = nc.dram_tensor("out", [T, D], F32, kind="ExternalOutput").ap()
    NB = min(512, S)
    with ExitStack() as st0:
        P = Prog(nc, st0)
        k = K(nc, P, st0)
        mod = k.dram([2, B, 6 * D], F32, "mod")
        Pm = k.dram([T, MLA_IN], F32, "Pm"); PT = k.dram([RW_IN, T], F32, "PT"); Ycat = k.dram([T, D], F32, "Ycat")
        x1 = k.dram([T, D], F32, "x1"); x2 = k.dram([T, D], F32, "x2"); x3 = k.dram([T, D], F32, "x3")
        QTn = k.dram([16, 128, S], BF16, "QTn"); QTr = k.dram([16, 64, S], BF16, "QTr")
        KTn = k.dram([16, 128, S], BF16, "KTn"); KTr = k.dram([16, 64, S], BF16, "KTr"); Vh = k.dram([16, S, 128], BF16, "Vh")
        rs = {n: k.dram([RW_W, S], F32, "r" + n) for n in ("AT", "BT", "KT", "RT")}
        rs["GC"] = k.dram([RW_W, S // 64], F32, "rGC")
        for n in ("Vtm", "Btm", "Ktm", "Gt"):
            rs[n] = k.dram([S, RW_W], F32, "r" + n)
        rs["bonus"] = k.dram([S, 32], F32, "rbonus")
        UT = k.dram([NE // 512, 128, KC, 512], BF16, "UT"); GAT = k.dram([NCH, 128, T], BF16, "GAT")
        with ExitStack() as keep0:
            idb = k.sb(keep0, [128, 128], BF16, "idb"); b_idb = P.buf()
            idf = k.sb(keep0, [128, 128], F32, "idf"); b_idf = P.buf()
            ID, IDF = (idb, b_idb), (idf, b_idf)
            with ExitStack() as st:
                P.dma("sp", lambda e: e.dma_start(out=idf[:], in_=A["consts"][:, 0:128]), writes=[b_idf])
                P.op("dve", lambda e: e.tensor_copy(out=idb[:], in_=idf[:]), reads=[b_idf], writes=[b_idb])
                k.end_stage()
            for l in range(2):
                with ExitStack() as st:
                    stage_ada(k, st, A["cT"], A["ada_w"][l], A["ada_b"][l], mod[l], 6 * D, B)
                    k.end_stage()
            mv = lambda l, b, i: mod[l, b, i * D:(i + 1) * D]
            for b in range(B):
                for t0 in range(0, S, NB):
                    row0 = b * S + t0
                    with ExitStack() as keep:
                        hT = k.sb(keep, [128, KC, NB], BF16, "ihT"); b_hT = P.buf()
                        with ExitStack() as st:
                            gb_, sb_ = load_norm_bcast(k, st, A["norm_mix_w"][0], mv(0, b, 1), mv(0, b, 0))
                            stage_norm_T(k, st, ID, A["x"], row0, NB, gb_, sb_, (hT, b_hT))
                            k.end_stage()
                        with ExitStack() as st:
                            ot = [k.sb(st, [128, 512], F32, "iot") for _ in range(2)]; bo = [P.buf() for _ in range(2)]
                            cnt = [0]

                            def evac(ti, c0, w, p_t, b_p, row0=row0, ot=ot, bo=bo, cnt=cnt):
                                i = cnt[0] % 2; cnt[0] += 1
                                P.op("act", lambda e: e.copy(out=ot[i][:, 0:w], in_=p_t[:, 0:w]), reads=[b_p], writes=[bo[i]])
                                rr = row0 + ti * 128
                                P.dma("sp", lambda e: e.dma_start(out=Pm[rr:rr + 128, c0:c0 + w], in_=ot[i][:, 0:w]), reads=[bo[i]])
                            gemm_tok(k, st, (hT, b_hT), KC, NB, A["hyb_w_mla"], MLA_IN, evac)
                            k.end_stage()
                        with ExitStack() as st:
                            ft = [k.sb(st, [128, 512], F32, "ift") for _ in range(2)]; bf_ = [P.buf() for _ in range(2)]
                            for j in range(RW_IN // 128):
                                def ev(_j, p_t, b_p, j=j, row0=row0, ft=ft, bf_=bf_):
                                    i = j % 2
                                    P.op("act", lambda e: e.copy(out=ft[i][:, 0:NB], in_=p_t[:, 0:NB]), reads=[b_p], writes=[bf_[i]])
                                    P.dma("sp", lambda e: e.dma_start(out=PT[j * 128:(j + 1) * 128, row0:row0 + NB], in_=ft[i][:, 0:NB]), reads=[bf_[i]])
                                gemm_feat_one(k, st, (hT, b_hT), NB, A["hyb_w_rw"][j], ev)
                            k.end_stage()
            for b in range(B):
                mla_prep(k, ID, A["consts"], Pm, 0, A["pos"], S, b * S, A["mla_q_norm_w"], A["mla_w_uq"], A["mla_kv_norm_w"], A["mla_w_ukv"],
                         A["mla_qk_q_w"], A["mla_qk_k_w"], QTn, QTr, KTn, KTr, Vh)
                mla_attn(k, A["consts"], A["mla_qk_q_w"], A["mla_qk_k_w"], QTn, QTr, KTn, KTr, Vh, S, Ycat, b * S, 0)
                rwkv_prep(k, A["consts"], PT, b * S, S, A["rwp"], A["rwkv_w2"], A["rwkv_a2"], A["rwkv_g2"], rs["AT"], rs["BT"], rs["KT"], rs["RT"], rs["GC"],
                          rs["Vtm"], rs["Btm"], rs["Ktm"], rs["bonus"], rs["Gt"], TB=NB)
                rwkv_scan(k, A["consts"], rs["AT"], rs["BT"], rs["KT"], rs["RT"], rs["GC"], rs["Vtm"], rs["Btm"], rs["Ktm"], rs["bonus"], rs["Gt"],
                          A["rwkv_lnx_w"], A["rwkv_lnx_b"], S, Ycat, b * S, MLA_H * V_HEAD, HB=8, CB=min(4, S // 64))
            for b in range(B):
                for t0 in range(0, S, NB):
                    row0 = b * S + t0
                    with ExitStack() as keep:
                        yT = k.sb(keep, [128, KC, NB], BF16, "oyT"); b_yT = P.buf()
                        with ExitStack() as st:
                            stage_plain_T(k, st, ID, Ycat, row0, NB, (yT, b_yT))
                            k.end_stage()
                        with ExitStack() as st:
                            gate_b = load_bcast(k, st, mv(0, b, 2), "ogt")
                            evac = make_residual_evac(k, st, A["x"], x1, row0, gate_b, NB)
                            gemm_tok(k, st, (yT, b_yT), KC, NB, A["hyb_w_out"], D, evac)
                            k.end_stage()
            for b in range(B):
                rsl = slice(b * S, (b + 1) * S)
                peer_layer(k, ID, IDF, x1[rsl, :], x2[rsl, :], S, 2, A["norm_ffn_w"][0], mv(0, b, 4), mv(0, b, 3), mv(0, b, 5),
                           A["peer_w_q"][0], A["keysT"][0], A["peer_u"][0], A["peer_v"][0], UT, GAT[:, :, rsl], do_prep=(b == 0))
            NTc = 256
            for b in range(B):
                with ExitStack() as keepb:
                    hp = k.sb(keepb, [128, KC, 2], BF16, "chp"); b_hp = P.buf()
                    for t0 in range(0, S, NTc):
                        row0 = b * S + t0
                        with ExitStack() as keep:
                            hT = k.sb(keep, [128, KC, 2 + NTc], BF16, "chT"); b_hT = P.buf()
                            gT = k.sb(keep, [128, KC, NTc], BF16, "cgT"); b_gT = P.buf()
                            with ExitStack() as st:
                                gb_, sb_ = load_norm_bcast(k, st, A["norm_mix_w"][1], mv(1, b, 1), mv(1, b, 0))
                                if t0 == 0:
                                    P.op("pool", lambda e, hT=hT: e.memset(hT[:, :, 0:2], 0.0), writes=[b_hT])
                                else:
                                    P.op("pool", lambda e, hT=hT: e.tensor_copy(out=hT[:, :, 0:2], in_=hp[:]), reads=[b_hp], writes=[b_hT])
                                stage_norm_T(k, st, ID, x2, row0, NTc, gb_, sb_, (hT[:, :, 2:2 + NTc], b_hT))
                                P.op("pool", lambda e, hT=hT: e.tensor_copy(out=hp[:], in_=hT[:, :, NTc:NTc + 2]), reads=[b_hT], writes=[b_hp])
                                k.end_stage()
                            with ExitStack() as st:
                                stage_conv_core(k, st, (hT, b_hT), NTc, 2, A["conv_w_in"], A["conv_w3"], KC, (gT, b_gT))
                                k.end_stage()
                            with ExitStack() as st:
                                gate_b = load_bcast(k, st, mv(1, b, 2), "cgt")
                                evac = make_residual_evac(k, st, x2, x3, row0, gate_b, NTc)
                                gemm_tok(k, st, (gT, b_gT), KC, NTc, A["conv_w_out"], D, evac)
                                k.end_stage()
                    k.end_stage()
            for b in range(B):
                rsl = slice(b * S, (b + 1) * S)
                peer_layer(k, ID, IDF, x3[rsl, :], out[rsl, :], S, 2, A["norm_ffn_w"][1], mv(1, b, 4), mv(1, b, 3), mv(1, b, 5),
                           A["peer_w_q"][1], A["keysT"][1], A["peer_u"][1], A["peer_v"][1], UT, GAT[:, :, rsl], do_prep=(b == 0))
            k.end_stage()
    return nc


def make_consts():
    cv = np.zeros((128, C_W), np.float32)
    cv[:, 0:128] = np.eye(128)
    cv[:, 128:256] = (np.arange(128)[:, None] <= np.arange(128)[None, :])
    cv[:, 256:288] = (10000.0 ** (-np.arange(32, dtype=np.float32) / 32)).astype(np.float32)[None, :]
    cv[:64, 288:352] = (np.arange(64)[:, None] < np.arange(64)[None, :])
    cv[:64, 352:416] = (np.arange(64)[:, None] > np.arange(64)[None, :])
    cv[:64, C_BONES:C_BONES + 64] = 1
    cv[64:, C_BONES + 64:C_BONES + 128] = 1
    cv[:64, C_SEL] = 1
    cv[64:, C_SEL + 1] = 1
    cv[:, C_RMASK:C_RMASK + 512] = 1
    cv[:, C_RMASK:C_RMASK + 512:64] = 0
    return cv


def host_layout(inp):
    f = lambda a: np.ascontiguousarray(np.asarray(a), dtype=np.float32)
    B, S, _ = inp["x"].shape
    pc = lambda v: np.asarray(v, dtype=np.float32).reshape(-1, 128).T

    def blk(W, width):
        W = np.asarray(W, dtype=np.float32)
        K_, n_ = W.shape
        nb_ = -(-n_ // width)
        if nb_ * width != n_:
            W = np.concatenate([W, np.zeros((K_, nb_ * width - n_), np.float32)], 1)
        return np.ascontiguousarray(W.reshape(K_ // 128, 128, nb_, width).transpose(2, 1, 0, 3))

    m = dict(
        x=f(inp["x"]).reshape(B * S, D), cT=f(np.asarray(inp["c"]).reshape(B, KC, 128).transpose(2, 1, 0)),
        pos=np.ascontiguousarray(np.asarray(inp["positions"]).reshape(B * S, 1).astype(np.int32)), consts=make_consts(),
        ada_w=np.stack([blk(inp["ada_w"][l], 512) for l in range(2)]), ada_b=f(inp["ada_b"]), norm_mix_w=f(inp["norm_mix_w"]), norm_ffn_w=f(inp["norm_ffn_w"]),
        hyb_w_mla=blk(np.asarray(inp["hyb_w_in"][0])[:, :MLA_IN], 512), hyb_w_rw=blk(np.asarray(inp["hyb_w_in"][0])[:, MLA_IN:], 128), mla_q_norm_w=f(inp["mla_q_norm_w"][0]), mla_w_uq=f(inp["mla_w_uq"][0]),
        mla_kv_norm_w=f(inp["mla_kv_norm_w"][0]), mla_w_ukv=f(inp["mla_w_ukv"][0]), mla_qk_q_w=f(inp["mla_qk_q_w"][0]),
        mla_qk_k_w=f(inp["mla_qk_k_w"][0]),
        rwp=f(np.concatenate([pc(inp["rwkv_mu"][0]), pc(inp["rwkv_w0"][0]), pc(inp["rwkv_a0"][0]), pc(inp["rwkv_k_k"][0]), pc(inp["rwkv_k_a"][0]),
                              pc(np.asarray(inp["rwkv_r_k"][0]).reshape(-1))], 1)),
        rwkv_w2=f(inp["rwkv_w2"][0]), rwkv_a2=f(inp["rwkv_a2"][0]), rwkv_g2=f(inp["rwkv_g2"][0]), rwkv_lnx_w=f(inp["rwkv_lnx_w"][0]),
        rwkv_lnx_b=f(inp["rwkv_lnx_b"][0]), hyb_w_out=blk(inp["hyb_w_out"][0], 512),
        conv_w_in=blk(inp["conv_w_in"][0], 128), conv_w3=f(np.asarray(inp["conv_w"][0]).reshape(3, KC, 128).transpose(2, 1, 0)), conv_w_out=blk(inp["conv_w_out"][0], 512),
        peer_w_q=np.stack([blk(inp["peer_w_q"][l], 128) for l in range(2)]), keysT=f(np.asarray(inp["peer_keys"]).reshape(2, 16, 128, 128).transpose(0, 3, 1, 2)),
        peer_u=f(inp["peer_u"]), peer_v=f(inp["peer_v"]))
    return m, B, S


def kernel(**inputs):
    m, B, S = host_layout(inputs)
    nc = build(B, S)
    res = run_bass_kernel_spmd(nc, [m], core_ids=[0])
    return np.asarray(res.results[0]["out"], dtype=np.float32).reshape(B, S, D)
```
